# Optimizing a Trainium2 kernel written in Bass

```python
import math
import jax
import jax.numpy as jnp
from jax import lax
import numpy as np

D_MODEL = 2048
BATCH = 4
SEQ = 2048
DEPTH = 2

CHUNK = 64
N_META = 16
MIX_WIDTH = D_MODEL
HGRN_DK = 128
HGRN_WIDTH = MIX_WIDTH // 4
HGRN_HEADS = HGRN_WIDTH // HGRN_DK
HGRN_BLOCK = 64
S5_CH = 16
S5_STATE = 64
S5_WIDTH = MIX_WIDTH // 4
S5_GROUPS = S5_WIDTH // S5_CH
DIFF_DH = 128
DIFF_WIDTH = MIX_WIDTH - HGRN_WIDTH - S5_WIDTH
DIFF_HEADS = DIFF_WIDTH // (2 * DIFF_DH)
Q_BLOCK = 128
N_GROUPS = 8
EXPERTS_PER_GROUP = 8
N_EXPERTS = N_GROUPS * EXPERTS_PER_GROUP
TOP_K = 2
D_EXPERT = D_MODEL // 4
MOE_BLOCK = 128
RMS_EPS = 1e-6
IN_SPLITS = (HGRN_WIDTH, HGRN_WIDTH, HGRN_WIDTH, HGRN_WIDTH, S5_WIDTH, DIFF_WIDTH, DIFF_WIDTH, DIFF_WIDTH)
IN_WIDTH = 4 * HGRN_WIDTH + S5_WIDTH + 3 * DIFF_WIDTH

kernel_name = 'hymba_hgrn2_s5_diffattn_hiermoe'


def rms_norm(x, w):
    xf = x.astype(jnp.float32)
    y = xf * lax.rsqrt(jnp.mean(xf * xf, axis=-1, keepdims=True) + RMS_EPS)
    return (y * w.astype(jnp.float32)).astype(x.dtype)


def chunk_ids(length):
    pos = jnp.arange(length, dtype=jnp.int32)
    return jnp.where(pos < N_META, 0, (pos - N_META) // CHUNK + 1)


def chunk_end(p, length):
    end = N_META if p < N_META else N_META + ((p - N_META) // CHUNK + 1) * CHUNK
    return min(end, length)


def split_columns(p):
    outs, start = [], 0
    for width in IN_SPLITS:
        outs.append(p[..., start:start + width])
        start += width
    return outs


def hgrn2_mixer(q, f_pre, v, g, lower_bound, norm_w):
    f32 = jnp.float32
    bsz, length, _ = q.shape
    nblk = -(-length // HGRN_BLOCK)
    pad = nblk * HGRN_BLOCK - length
    lb = lower_bound.astype(f32)
    log_f = jnp.logaddexp(jnp.log(lb), jnp.log1p(-lb) + jax.nn.log_sigmoid(f_pre.astype(f32)))
    k = -jnp.expm1(log_f)

    def blocks(t):
        t = jnp.pad(t.astype(f32), ((0, 0), (0, pad), (0, 0)))
        t = t.reshape(bsz, nblk, HGRN_BLOCK, HGRN_HEADS, HGRN_DK)
        return t.transpose(1, 0, 3, 2, 4)

    causal = jnp.tril(jnp.ones((HGRN_BLOCK, HGRN_BLOCK), bool))[:, :, None]

    def step(state, blk):
        q_c, k_c, v_c, lf_c = blk
        b = jnp.cumsum(lf_c, axis=2)
        rel = jnp.where(causal, b[:, :, :, None, :] - b[:, :, None, :, :], -jnp.inf)
        scores = jnp.einsum('bhtk,bhsk,bhtsk->bhts', q_c, k_c, jnp.exp(rel))
        out = (jnp.einsum('bhts,bhsv->bhtv', scores, v_c)
               + jnp.einsum('bhtk,bhkv->bhtv', q_c * jnp.exp(b), state))
        b_end = b[:, :, -1:, :]
        state = (jnp.exp(b_end[:, :, 0, :, None]) * state
                 + jnp.einsum('bhsk,bhsv->bhkv', k_c * jnp.exp(b_end - b), v_c))
        return state, out

    s0 = jnp.zeros((bsz, HGRN_HEADS, HGRN_DK, HGRN_DK), f32)
    _, o = lax.scan(step, s0, (blocks(q), blocks(k), blocks(v), blocks(log_f)))
    o = o.transpose(1, 0, 3, 2, 4).reshape(bsz, nblk * HGRN_BLOCK, HGRN_HEADS, HGRN_DK)[:, :length]
    gate = g.astype(f32).reshape(bsz, length, HGRN_HEADS, HGRN_DK)
    o = rms_norm(o, norm_w) * jax.nn.silu(gate)
    return o.reshape(bsz, length, HGRN_WIDTH)


def s5_mixer(u, a_re, a_im, b_re, b_im, c_re, c_im, d_skip, log_dt, w_glu, b_glu):
    f32 = jnp.float32
    bsz, length, _ = u.shape
    uf = u.astype(f32).reshape(bsz, length, S5_GROUPS, S5_CH)
    a_re, a_im = a_re.astype(f32), a_im.astype(f32)
    dt = jnp.exp(log_dt.astype(f32))[:, None]
    mag = jnp.exp(a_re * dt)
    ab_re, ab_im = mag * jnp.cos(a_im * dt), mag * jnp.sin(a_im * dt)
    den = a_re * a_re + a_im * a_im
    z_re = ((ab_re - 1.0) * a_re + ab_im * a_im) / den
    z_im = (ab_im * a_re - (ab_re - 1.0) * a_im) / den
    b_re, b_im = b_re.astype(f32), b_im.astype(f32)
    bb_re = z_re[..., None] * b_re - z_im[..., None] * b_im
    bb_im = z_re[..., None] * b_im + z_im[..., None] * b_re
    bu_re = jnp.einsum('blgc,gpc->blgp', uf, bb_re)
    bu_im = jnp.einsum('blgc,gpc->blgp', uf, bb_im)
    a_seq_re = jnp.broadcast_to(ab_re, bu_re.shape)
    a_seq_im = jnp.broadcast_to(ab_im, bu_im.shape)

    def combine(e1, e2):
        a1r, a1i, b1r, b1i = e1
        a2r, a2i, b2r, b2i = e2
        return (a2r * a1r - a2i * a1i, a2r * a1i + a2i * a1r,
                a2r * b1r - a2i * b1i + b2r, a2r * b1i + a2i * b1r + b2i)

    _, _, x_re, x_im = lax.associative_scan(combine, (a_seq_re, a_seq_im, bu_re, bu_im), axis=1)
    y = (jnp.einsum('blgp,gcp->blgc', x_re, c_re.astype(f32))
         - jnp.einsum('blgp,gcp->blgc', x_im, c_im.astype(f32)))
    y = y + d_skip.astype(f32).reshape(S5_GROUPS, S5_CH) * uf
    y = jax.nn.gelu(y.reshape(bsz, length, S5_WIDTH))
    val, gate = jnp.split(y @ w_glu.astype(f32) + b_glu.astype(f32), 2, axis=-1)
    return val * jax.nn.sigmoid(gate)


def diff_attention_mixer(q, k, v, lq1, lk1, lq2, lk2, subln_w, lambda_init):
    f32 = jnp.float32
    bsz, length, _ = q.shape
    q = q.reshape(bsz, length, DIFF_HEADS, 2, DIFF_DH).transpose(0, 2, 3, 1, 4)
    k = k.reshape(bsz, length, DIFF_HEADS, 2, DIFF_DH).transpose(0, 2, 3, 1, 4)
    v = v.reshape(bsz, length, DIFF_HEADS, 2 * DIFF_DH).transpose(0, 2, 1, 3).astype(f32)
    lam = (jnp.exp(jnp.sum(lq1.astype(f32) * lk1.astype(f32)))
           - jnp.exp(jnp.sum(lq2.astype(f32) * lk2.astype(f32))) + lambda_init)
    ids = chunk_ids(length)
    scale = DIFF_DH ** -0.5
    outs = []
    for q0 in range(0, length, Q_BLOCK):
        q1 = min(q0 + Q_BLOCK, length)
        kend = chunk_end(q1 - 1, length)
        s = jnp.einsum('bhiqd,bhikd->bhiqk', q[:, :, :, q0:q1], k[:, :, :, :kend]).astype(f32) * scale
        mask = ids[None, :kend] <= ids[q0:q1, None]
        p = jax.nn.softmax(jnp.where(mask, s, -jnp.inf), axis=-1)
        w = p[:, :, 0] - lam * p[:, :, 1]
        outs.append(jnp.einsum('bhqk,bhkd->bhqd', w, v[:, :, :kend]))
    o = jnp.concatenate(outs, axis=2)
    o = rms_norm(o, subln_w) * (1.0 - lambda_init)
    return o.transpose(0, 2, 1, 3).reshape(bsz, length, DIFF_WIDTH)


def hierarchical_moe(h, w_rg, b_rg, w_re, b_re, w1, w3, w2):
    f32 = jnp.float32
    bsz, length, dim = h.shape
    xt = h.reshape(-1, dim)
    n_tok = xt.shape[0]
    g_logits = (xt @ w_rg).astype(f32) + b_rg.astype(f32)
    g_prob = jax.nn.softmax(g_logits, axis=-1)
    g_sel = jnp.argmax(g_logits, axis=-1).astype(jnp.int32)
    p_group = jnp.take_along_axis(g_prob, g_sel[:, None], axis=1)
    e_logits = ((xt @ w_re).astype(f32) + b_re.astype(f32)).reshape(n_tok, N_GROUPS, EXPERTS_PER_GROUP)
    e_logits = jnp.take_along_axis(e_logits, g_sel[:, None, None], axis=1)[:, 0]
    top_v, top_j = lax.top_k(e_logits, TOP_K)
    gate = jax.nn.softmax(top_v, axis=-1) * p_group
    expert = g_sel[:, None] * EXPERTS_PER_GROUP + top_j.astype(jnp.int32)

    n_assign = n_tok * TOP_K
    flat_e = expert.reshape(-1)
    order = jnp.argsort(flat_e)
    sorted_e = flat_e[order]
    counts = jnp.zeros((N_EXPERTS,), jnp.int32).at[flat_e].add(1)
    padded = (counts + MOE_BLOCK - 1) // MOE_BLOCK * MOE_BLOCK
    pad_end = jnp.cumsum(padded)
    pad_start = pad_end - padded
    start = jnp.cumsum(counts) - counts
    dest = pad_start[sorted_e] + jnp.arange(n_assign, dtype=jnp.int32) - start[sorted_e]
    n_blocks = -(-(n_assign + N_EXPERTS * (MOE_BLOCK - 1)) // MOE_BLOCK)
    n_rows = n_blocks * MOE_BLOCK
    row_token = jnp.full((n_rows,), n_tok, jnp.int32).at[dest].set(order // TOP_K)
    x_ext = jnp.concatenate([xt, jnp.zeros((1, dim), xt.dtype)], axis=0)
    xb = x_ext[row_token].reshape(n_blocks, MOE_BLOCK, dim)
    block_start = jnp.arange(n_blocks, dtype=jnp.int32) * MOE_BLOCK
    block_expert = jnp.minimum(jnp.searchsorted(pad_end, block_start, side='right'), N_EXPERTS - 1)

    def expert_block(args):
        rows, e = args
        hid = jax.nn.silu(rows @ w1[e]) * (rows @ w3[e])
        return hid @ w2[e]

    y_rows = lax.map(expert_block, (xb, block_expert)).reshape(n_rows, dim)
    contrib = y_rows[dest].astype(f32) * gate.reshape(-1)[order][:, None]
    out = jnp.zeros((n_tok, dim), f32).at[order // TOP_K].add(contrib)
    return out.astype(h.dtype).reshape(bsz, length, dim)


def setup_inputs(seed: int = 0) -> dict:
    key = jax.random.key(seed)
    keys = iter(jax.random.split(key, 40))
    f32 = jnp.float32

    def nrm(shape, scale):
        return scale * jax.random.normal(next(keys), shape, f32)

    def gain(shape):
        return 1.0 + 0.02 * jax.random.normal(next(keys), shape, f32)

    n_idx = jnp.arange(S5_STATE, dtype=f32)
    return {
        'x': nrm((BATCH, SEQ, D_MODEL), 1.0),
        'meta_tokens': nrm((N_META, D_MODEL), 1.0),
        'ln1_w': gain((DEPTH, D_MODEL)),
        'w_in': nrm((DEPTH, D_MODEL, IN_WIDTH), D_MODEL ** -0.5),
        'hgrn_lower_bounds': nrm((DEPTH, HGRN_WIDTH), 1.0),
        'hgrn_norm_w': gain((DEPTH, HGRN_DK)),
        's5_a_re': -0.5 + nrm((DEPTH, S5_GROUPS, S5_STATE), 0.01),
        's5_a_im': math.pi * n_idx + nrm((DEPTH, S5_GROUPS, S5_STATE), 0.01),
        's5_b_re': nrm((DEPTH, S5_GROUPS, S5_STATE, S5_CH), (2 * S5_CH) ** -0.5),
        's5_b_im': nrm((DEPTH, S5_GROUPS, S5_STATE, S5_CH), (2 * S5_CH) ** -0.5),
        's5_c_re': nrm((DEPTH, S5_GROUPS, S5_CH, S5_STATE), S5_STATE ** -0.5),
        's5_c_im': nrm((DEPTH, S5_GROUPS, S5_CH, S5_STATE), S5_STATE ** -0.5),
        's5_d': nrm((DEPTH, S5_WIDTH), 1.0),
        's5_log_dt': jax.random.uniform(next(keys), (DEPTH, S5_GROUPS), f32, math.log(1e-3), math.log(1e-1)),
        's5_w_glu': nrm((DEPTH, S5_WIDTH, 2 * S5_WIDTH), S5_WIDTH ** -0.5),
        's5_b_glu': nrm((DEPTH, 2 * S5_WIDTH), 0.01),
        'diff_lambda_q1': nrm((DEPTH, DIFF_DH), 0.1),
        'diff_lambda_k1': nrm((DEPTH, DIFF_DH), 0.1),
        'diff_lambda_q2': nrm((DEPTH, DIFF_DH), 0.1),
        'diff_lambda_k2': nrm((DEPTH, DIFF_DH), 0.1),
        'diff_subln_w': gain((DEPTH, 2 * DIFF_DH)),
        'w_out': nrm((DEPTH, MIX_WIDTH, D_MODEL), MIX_WIDTH ** -0.5),
        'ln2_w': gain((DEPTH, D_MODEL)),
        'router_group_w': nrm((DEPTH, D_MODEL, N_GROUPS), D_MODEL ** -0.5),
        'router_group_b': nrm((DEPTH, N_GROUPS), 0.01),
        'router_expert_w': nrm((DEPTH, D_MODEL, N_EXPERTS), D_MODEL ** -0.5),
        'router_expert_b': nrm((DEPTH, N_EXPERTS), 0.01),
        'expert_w1': nrm((DEPTH, N_EXPERTS, D_MODEL, D_EXPERT), D_MODEL ** -0.5),
        'expert_w3': nrm((DEPTH, N_EXPERTS, D_MODEL, D_EXPERT), D_MODEL ** -0.5),
        'expert_w2': nrm((DEPTH, N_EXPERTS, D_EXPERT, D_MODEL), D_EXPERT ** -0.5),
        'final_norm_w': gain((D_MODEL,)),
    }


def reference(x, meta_tokens, ln1_w, w_in, hgrn_lower_bounds, hgrn_norm_w, s5_a_re, s5_a_im,
              s5_b_re, s5_b_im, s5_c_re, s5_c_im, s5_d, s5_log_dt, s5_w_glu, s5_b_glu,
              diff_lambda_q1, diff_lambda_k1, diff_lambda_q2, diff_lambda_k2, diff_subln_w,
              w_out, ln2_w, router_group_w, router_group_b, router_expert_w, router_expert_b,
              expert_w1, expert_w3, expert_w2, final_norm_w):
    bsz = x.shape[0]
    meta = jnp.broadcast_to(meta_tokens.astype(x.dtype)[None], (bsz, N_META, D_MODEL))
    z = jnp.concatenate([meta, x], axis=1)
    lb_all = jnp.cumsum(jax.nn.softmax(hgrn_lower_bounds.astype(jnp.float32), axis=0), axis=0)
    lb_all = lb_all - lb_all[0]
    for layer in range(DEPTH):
        hn = rms_norm(z, ln1_w[layer])
        proj = hn @ w_in[layer]
        hq, hf, hi, hg, su, dq, dk, dv = split_columns(proj)
        o_a = hgrn2_mixer(hq, hf, hi, hg, lb_all[layer], hgrn_norm_w[layer])
        o_b = s5_mixer(su, s5_a_re[layer], s5_a_im[layer], s5_b_re[layer], s5_b_im[layer],
                       s5_c_re[layer], s5_c_im[layer], s5_d[layer], s5_log_dt[layer],
                       s5_w_glu[layer], s5_b_glu[layer])
        lambda_init = 0.8 - 0.6 * math.exp(-0.3 * layer)
        o_c = diff_attention_mixer(dq, dk, dv, diff_lambda_q1[layer], diff_lambda_k1[layer],
                                   diff_lambda_q2[layer], diff_lambda_k2[layer],
                                   diff_subln_w[layer], lambda_init)
        mixed = jnp.concatenate([o_a.astype(z.dtype), o_b.astype(z.dtype), o_c.astype(z.dtype)], axis=-1)
        z = z + mixed @ w_out[layer]
        z = z + hierarchical_moe(rms_norm(z, ln2_w[layer]), router_group_w[layer], router_group_b[layer],
                                 router_expert_w[layer], router_expert_b[layer],
                                 expert_w1[layer], expert_w3[layer], expert_w2[layer])
    return rms_norm(z, final_norm_w)[:, N_META:]
```

```python
import numpy as np
import concourse.bass as bass
import concourse.mybir as mybir
from concourse.bass_utils import run_bass_kernel_spmd

F32 = mybir.dt.float32
BF16 = mybir.dt.bfloat16
AF = mybir.ActivationFunctionType
ALU = mybir.AluOpType
AX = mybir.AxisListType


class Res:
    __slots__ = ("w", "r", "dsem", "dcnt", "t", "name", "cm")

    def __init__(self, name=""):
        self.w = {}
        self.r = {}
        self.dsem = None
        self.dcnt = 0
        self.t = None
        self.name = name


class T(Res):
    def __getitem__(self, idx):
        return self.t[idx]


class P:
    def __init__(self, nc, same_engine_sync=True):
        self.nc = nc
        self.eng = {"pe": nc.tensor, "dve": nc.vector, "act": nc.scalar, "pool": nc.gpsimd, "sp": nc.sync}
        self.sem = {}
        self.cnt = {}
        for e in ("pe", "dve", "act", "pool"):
            self.sem[e] = nc.semaphore("sem_" + e).__enter__()
            self.cnt[e] = 0
        self.semid = {id(s): e for e, s in self.sem.items()}
        self.waited = {e: {} for e in self.eng}
        self.same_engine_sync = same_engine_sync
        self.out_tokens = []
        self.ntile = 0
        self.tiles = []

    def sb(self, shape, dt=F32, name=None):
        self.ntile += 1
        name = name or f"t{self.ntile}"
        t = T(name)
        t.cm = self.nc.sbuf_tensor(name, list(shape), dt)
        t.t = t.cm.__enter__()
        self.tiles.append(t)
        return t

    def ps(self, shape, dt=F32, name=None):
        self.ntile += 1
        name = name or f"p{self.ntile}"
        t = T(name)
        t.t = self.nc.psum_tensor(name, list(shape), dt).__enter__()
        return t

    def _wait(self, e, deps, skip_own=False):
        eng = self.eng[e]
        own = self.sem.get(e)
        for sem, val in deps.items():
            if sem is own and (skip_own or not self.same_engine_sync):
                continue
            k = id(sem)
            if self.waited[e].get(k, 0) >= val:
                continue
            eng.wait_ge(sem, val)
            self.waited[e][k] = val

    @staticmethod
    def _merge(d, s):
        for k, v in s.items():
            if d.get(k, 0) < v:
                d[k] = v

    def op(self, e, fn, reads=(), writes=(), accum=False):
        deps = {}
        for r in reads:
            self._merge(deps, r.w)
        for w in writes:
            self._merge(deps, w.w)
            self._merge(deps, w.r)
        self._wait(e, deps, skip_own=(e == "pe"))
        inst = fn(self.eng[e])
        self.cnt[e] += 1
        inst.then_inc(self.sem[e], 1)
        tok = {self.sem[e]: self.cnt[e]}
        for r in reads:
            self._merge(r.r, tok)
        for w in writes:
            if accum:
                self._merge(w.w, tok)
            else:
                w.w = dict(tok)
                w.r = {}
        return inst

    def dma(self, q, out, in_, reads=(), writes=(), semres=None, concurrent=False, **kw):
        if semres is None:
            semres = (list(writes) + list(reads))[0]
        if semres.dsem is None:
            semres.dsem = self.nc.semaphore("d_" + semres.name).__enter__()
        deps = {}
        for r in reads:
            self._merge(deps, r.w)
        for w in writes:
            if concurrent:
                ww = {k: v for k, v in w.w.items() if k is not w.dsem}
                self._merge(deps, ww)
            else:
                self._merge(deps, w.w)
            self._merge(deps, w.r)
        self._wait(q, deps)
        inst = self.eng[q].dma_start(out=out, in_=in_, **kw)
        semres.dcnt += 16
        inst.then_inc(semres.dsem, 16)
        tok = {semres.dsem: semres.dcnt}
        for r in reads:
            self._merge(r.r, tok)
        for w in writes:
            w.w = dict(tok)
            w.r = {}
        return tok

    def barrier(self):
        deps = {}
        for e in ("pe", "dve", "act", "pool"):
            if self.cnt[e] > 0:
                deps[self.sem[e]] = self.cnt[e]
        for t in self.tiles:
            if t.dsem is not None and t.dcnt > 0:
                deps[t.dsem] = t.dcnt
        for e in self.eng:
            self._wait(e, deps, skip_own=True)

    def free(self, tiles):
        for t in reversed(tiles):
            t.cm.__exit__(None, None, None)
            self.tiles.remove(t)

    def finish(self, out_res):
        deps = {}
        for r in out_res:
            self._merge(deps, r.r)
            self._merge(deps, r.w)
        self._wait("sp", deps)


D = 2048
NB = 4
SEQ = 2048
NMETA = 16
L = SEQ + NMETA
NT = L // 2
NCH = 3
TCH = NT // NCH
INW = 5632
KC = D // 128
EPS = 1e-6


class Rot:
    def __init__(self, tiles):
        self.tiles = tiles
        self.i = 0

    def next(self):
        t = self.tiles[self.i % len(self.tiles)]
        self.i += 1
        return t


def emit_rmsnorm(p, z, lnw, hn, ones_bf, epsc, sq_rot, ps_rot, rstd, hn32=None, nt=NT, tch=TCH):
    nch = nt // tch
    pss = [ps_rot.next() for _ in range(nch)]
    for c in range(KC):
        sq = sq_rot.next()
        p.op("act", lambda e: e.activation(out=sq[:, 0:nt], in_=z[:, c, 0:nt], func=AF.Square), reads=[z], writes=[sq])
        for n in range(nch):
            p.op("pe", lambda e: e.matmul(pss[n][:, 0:tch], lhsT=ones_bf[:], rhs=sq[:, n * tch:(n + 1) * tch],
                                          start=(c == 0), stop=(c == KC - 1)),
                 reads=[sq, ones_bf], writes=[pss[n]], accum=(c > 0))
    for n in range(nch):
        sl = slice(n * tch, (n + 1) * tch)
        p.op("act", lambda e: e.activation(out=rstd[:, sl], in_=pss[n][:, 0:tch], func=AF.Sqrt, bias=epsc[:, 0:1]),
             reads=[pss[n], epsc], writes=[rstd], accum=(n > 0))
    p.op("dve", lambda e: e.reciprocal(out=rstd[:, 0:nt], in_=rstd[:, 0:nt]), reads=[rstd], writes=[rstd])
    for c in range(KC):
        if hn is not None:
            p.op("dve", lambda e: e.scalar_tensor_tensor(out=hn[:, c, 0:nt], in0=z[:, c, 0:nt], scalar=lnw[:, c:c + 1], in1=rstd[:, 0:nt],
                                                         op0=ALU.mult, op1=ALU.mult), reads=[z, lnw, rstd], writes=[hn])
        if hn32 is not None:
            p.op("dve", lambda e: e.scalar_tensor_tensor(out=hn32[:, c, 0:nt], in0=z[:, c, 0:nt], scalar=lnw[:, c:c + 1], in1=rstd[:, 0:nt],
                                                         op0=ALU.mult, op1=ALU.mult), reads=[z, lnw, rstd], writes=[hn32])


def emit_inproj(p, hn, w_in, projT, wb_rot, ps_rot, st_rot, evac):
    GW = 512
    w_v = w_in.rearrange("(c p) n -> p c n", p=128)
    for g in range(INW // GW):
        wb = wb_rot.next()
        p.dma("pool", wb[:], w_v[:, :, g * GW:(g + 1) * GW], writes=[wb])
        for j in range(GW // 128):
            st = st_rot.next()
            for n in range(NCH):
                ps = ps_rot.next()
                for c in range(KC):
                    p.op("pe", lambda e: e.matmul(ps[:, 0:TCH], lhsT=wb[:, c, j * 128:(j + 1) * 128],
                                                  rhs=hn[:, c, n * TCH:(n + 1) * TCH], start=(c == 0), stop=(c == KC - 1)),
                         reads=[wb, hn], writes=[ps], accum=(c > 0))
                evac(st[:, n * TCH:(n + 1) * TCH], ps[:, 0:TCH], [ps], [st], first=(n == 0))
            r0 = g * GW + j * 128
            p.dma("sp", projT[r0:r0 + 128, :], st[:, 0:NT], reads=[st])


class Evac:
    def __init__(self, p):
        self.p = p
        self.i = 0

    def __call__(self, out, in_, reads, writes, first=True):
        self.i += 1
        p = self.p
        if self.i % 2 == 0:
            p.op("act", lambda e: e.activation(out=out, in_=in_, func=AF.Copy), reads=reads, writes=writes, accum=not first)
        else:
            p.op("dve", lambda e: e.tensor_copy(out=out, in_=in_), reads=reads, writes=writes, accum=not first)


def build_A():
    nc = bass.Bass("TRN2", target_bir_lowering=False)
    p = P(nc)
    zT = nc.dram_tensor("zT", [D, NT], F32, kind="ExternalInput").ap()
    lnw_d = nc.dram_tensor("lnw", [128, KC], F32, kind="ExternalInput").ap()
    w_in = nc.dram_tensor("w_in", [D, INW], F32, kind="ExternalInput").ap()
    projT = nc.dram_tensor("projT", [INW, NT], F32, kind="ExternalOutput").ap()
    z = p.sb([128, KC, NT], F32, "z")
    hn = p.sb([128, KC, NT], BF16, "hn")
    lnw = p.sb([128, KC], F32, "lnw_s")
    rstd = p.sb([128, NT], F32, "rstd")
    ones_bf = p.sb([128, 128], BF16, "ones")
    sq_rot = Rot([p.sb([128, NT], BF16, f"sq{i}") for i in range(2)])
    ps_rot = Rot([p.ps([128, 512], F32, f"ps{i}") for i in range(8)])
    wb_rot = Rot([p.sb([128, KC, 512], BF16, f"wb{i}") for i in range(2)])
    st_rot = Rot([p.sb([128, NT], F32, f"st{i}") for i in range(3)])
    evac = Evac(p)
    zv = zT.rearrange("(c p) t -> p c t", p=128)
    for q in range(4):
        p.dma("sp", z[:, 4 * q:4 * q + 4, :], zv[:, 4 * q:4 * q + 4, :], writes=[z], concurrent=True)
    p.dma("sp", lnw[:], lnw_d[:, :], writes=[lnw])
    p.op("pool", lambda e: e.memset(ones_bf[:], 1.0 / D), writes=[ones_bf])
    epsc = p.sb([128, 1], F32, "epsc")
    p.op("pool", lambda e: e.memset(epsc[:], EPS), writes=[epsc])
    emit_rmsnorm(p, z, lnw, hn, ones_bf, epsc, sq_rot, ps_rot, rstd)
    emit_inproj(p, hn, w_in, projT, wb_rot, ps_rot, st_rot, evac)
    p.finish(st_rot.tiles)
    return nc


HC = 32
LP = 2176
NCK = LP // HC


def build_B1():
    nc = bass.Bass("TRN2", target_bir_lowering=False)
    p = P(nc)
    hqT = nc.dram_tensor("hqT", [2, 128, LP], F32, kind="ExternalInput").ap()
    hfT = nc.dram_tensor("hfT", [2, 128, LP], F32, kind="ExternalInput").ap()
    hv = nc.dram_tensor("hv", [2, HC, NCK, 128], F32, kind="ExternalInput").ap()
    hg = nc.dram_tensor("hg", [2, 128, LP // 128, 128], F32, kind="ExternalInput").ap()
    lbraw_d = nc.dram_tensor("lbraw", [128, 2, 2], F32, kind="ExternalInput").ap()
    lsel_d = nc.dram_tensor("lsel", [128, 1], F32, kind="ExternalInput").ap()
    nw_d = nc.dram_tensor("nw", [128, 128], F32, kind="ExternalInput").ap()
    oa = nc.dram_tensor("oa", [2, 128, LP // 128, 128], F32, kind="ExternalOutput").ap()

    lb = p.sb([128, 2], F32, "lb_s")
    oml = p.sb([128, 2], F32, "oml")
    nw = p.sb([128, 128], F32, "nw_s")
    epsc = p.sb([128, 1], F32, "epsc")
    maskU = p.sb([HC, HC], F32, "maskU")
    mm = p.sb([128, LP], F32, "mm")
    ident = p.sb([128, 128], BF16, "ident")
    identf = p.sb([128, 128], F32, "identf")
    lbraw = p.sb([128, 2, 2], F32, "lbraw_s")
    lsel = p.sb([128, 1], F32, "lsel_s")
    p.dma("sp", lbraw[:], lbraw_d[:, :, :], writes=[lbraw])
    p.dma("sp", lsel[:], lsel_d[:, :], writes=[lsel])
    p.op("dve", lambda e: e.tensor_tensor(out=lb[:], in0=lbraw[:, :, 1], in1=lbraw[:, :, 0], op=ALU.subtract), reads=[lbraw], writes=[lb])
    p.op("act", lambda e: e.activation(out=lb[:], in_=lb[:], func=AF.Sigmoid), reads=[lb], writes=[lb])
    p.op("dve", lambda e: e.tensor_scalar(out=lb[:], in0=lb[:], scalar1=lsel[:, 0:1], scalar2=None, op0=ALU.mult), reads=[lb, lsel], writes=[lb])
    p.dma("sp", nw[:], nw_d[:, :], writes=[nw])
    p.op("pool", lambda e: e.memset(epsc[:], EPS), writes=[epsc])
    p.op("pool", lambda e: e.memset(maskU[:], 1.0), writes=[maskU])
    p.op("pool", lambda e: e.affine_select(out=maskU[:], in_=maskU[:], pattern=[[1, HC]], compare_op=ALU.is_ge, fill=0.0,
                                           base=0, channel_multiplier=-1), reads=[maskU], writes=[maskU])
    p.op("pool", lambda e: e.memset(identf[:], 0.0), writes=[identf])
    p.op("pool", lambda e: e.affine_select(out=identf[:], in_=identf[:], pattern=[[-1, 128]], compare_op=ALU.not_equal,
                                           fill=1.0, base=0, channel_multiplier=1), reads=[identf], writes=[identf])
    p.op("pool", lambda e: e.tensor_copy(out=ident[:], in_=identf[:]), reads=[identf], writes=[ident])
    p.op("pool", lambda e: e.memset(mm[:], 1.0), writes=[mm])
    p.op("pool", lambda e: e.memset(mm[:].rearrange("p (n c) -> p n c", c=HC)[:, :, 0:1], 0.0), reads=[mm], writes=[mm])
    p.op("dve", lambda e: e.tensor_scalar(out=oml[:], in0=lb[:], scalar1=-1.0, scalar2=1.0, op0=ALU.mult, op1=ALU.add),
         reads=[lb], writes=[oml])

    H = []
    shared = {nm: p.sb([128, LP], F32, "sh_" + nm) for nm in ("q", "f", "b", "e")}
    for h in range(2):
        d = {}
        for nm in ("q", "f", "b", "e"):
            d[nm] = shared[nm]
        d["qs"] = p.sb([128, LP], BF16, f"qs{h}")
        d["ks"] = p.sb([128, LP], BF16, f"ks{h}")
        d["kh"] = p.sb([128, LP], BF16, f"kh{h}")
        d["ebend"] = p.sb([128, NCK], F32, f"ebend{h}")
        d["v"] = p.sb([HC, NCK, 128], BF16, f"v{h}")
        d["g"] = p.sb([128, LP // 128, 128], F32, f"g{h}")
        d["o"] = p.sb([128, LP // 128, 128], F32, f"o{h}")
        d["S"] = p.sb([128, 128], F32, f"S{h}")
        d["Sb"] = p.sb([128, 128], BF16, f"Sb{h}")
        d["khT"] = Rot([p.sb([HC, 128], BF16, f"khT{h}_{i}") for i in range(3)])
        d["ATm"] = Rot([p.sb([HC, HC], BF16, f"ATm{h}_{i}") for i in range(2)])
        d["ms"] = p.sb([128, LP // 128], F32, f"ms{h}")
        H.append(d)
        p.dma("pool", d["v"][:], hv[h], writes=[d["v"]])
        p.dma("sp", d["g"][:], hg[h], writes=[d["g"]])
    psA = Rot([p.ps([128, 512], F32, f"psA{i}") for i in range(2)])
    psT = Rot([p.ps([128, 512], BF16, f"psT{i}") for i in range(2)])
    psO = Rot([p.ps([128, 512], F32, f"psO{i}") for i in range(2)])
    psS = Rot([p.ps([128, 512], F32, f"psS{i}") for i in range(2)])

    for h in range(2):
        d = H[h]
        q, f, b, e_ = d["q"], d["f"], d["b"], d["e"]
        p.dma("sp", q[:], hqT[h], writes=[q])
        p.dma("sp", f[:], hfT[h], writes=[f])
        p.op("act", lambda e: e.activation(out=f[:], in_=f[:], func=AF.Sigmoid), reads=[f], writes=[f])
        p.op("dve", lambda e: e.tensor_scalar(out=f[:], in0=f[:], scalar1=oml[:, h:h + 1], scalar2=lb[:, h:h + 1],
                                              op0=ALU.mult, op1=ALU.add), reads=[f, oml, lb], writes=[f])
        p.op("act", lambda e: e.activation(out=e_[:], in_=f[:], func=AF.Ln), reads=[f], writes=[e_])
        p.op("dve", lambda e: e.tensor_tensor_scan(out=b[:], data0=mm[:], data1=e_[:], initial=0.0, op0=ALU.mult, op1=ALU.add),
             reads=[mm, e_], writes=[b])
        p.op("dve", lambda e: e.tensor_scalar(out=f[:], in0=f[:], scalar1=-1.0, scalar2=1.0, op0=ALU.mult, op1=ALU.add),
             reads=[f], writes=[f])
        p.op("act", lambda e: e.activation(out=e_[:], in_=b[:], func=AF.Exp), reads=[b], writes=[e_])
        p.op("dve", lambda e: e.tensor_tensor(out=d["qs"][:], in0=q[:], in1=e_[:], op=ALU.mult), reads=[q, e_], writes=[d["qs"]])
        p.op("act", lambda e: e.activation(out=e_[:], in_=b[:], func=AF.Exp, scale=-1.0), reads=[b], writes=[e_])
        p.op("dve", lambda e: e.tensor_tensor(out=d["ks"][:], in0=f[:], in1=e_[:], op=ALU.mult), reads=[f, e_], writes=[d["ks"]])
        b3 = b[:].rearrange("p (n c) -> p n c", c=HC)
        p.op("act", lambda e: e.activation(out=d["ebend"][:], in_=b3[:, :, HC - 1], func=AF.Exp), reads=[b], writes=[d["ebend"]])
        e3 = e_[:].rearrange("p (n c) -> p n c", c=HC)
        p.op("dve", lambda e: e.tensor_tensor(out=e3, in0=b3[:, :, HC - 1:HC].to_broadcast([128, NCK, HC]), in1=b3, op=ALU.subtract),
             reads=[b], writes=[e_])
        p.op("act", lambda e: e.activation(out=e_[:], in_=e_[:], func=AF.Exp), reads=[e_], writes=[e_])
        p.op("dve", lambda e: e.tensor_tensor(out=d["kh"][:], in0=f[:], in1=e_[:], op=ALU.mult), reads=[f, e_], writes=[d["kh"]])
        p.op("pool", lambda e: e.memset(d["S"][:], 0.0), writes=[d["S"]])
        p.op("pool", lambda e: e.memset(d["Sb"][:], 0.0), writes=[d["Sb"]])

    for n in range(NCK):
        for h in range(2):
            d = H[h]
            sl = slice(n * HC, (n + 1) * HC)
            qs, ks, kh, v, S, Sb = d["qs"], d["ks"], d["kh"], d["v"], d["S"], d["Sb"]
            pa = psA.next()
            p.op("pe", lambda e: e.matmul(pa[0:HC, 0:HC], lhsT=ks[:, sl], rhs=qs[:, sl], start=True, stop=True),
                 reads=[ks, qs], writes=[pa])
            atm = d["ATm"].next()
            p.op("dve", lambda e: e.tensor_tensor(out=atm[:], in0=pa[0:HC, 0:HC], in1=maskU[:], op=ALU.mult),
                 reads=[pa, maskU], writes=[atm])
            pt = psT.next()
            p.op("pe", lambda e: e.transpose(out=pt[0:HC, 0:128], in_=kh[:, sl], identity=ident[:]), reads=[kh, ident], writes=[pt])
            kht = d["khT"].next()
            p.op("act", lambda e: e.activation(out=kht[:], in_=pt[0:HC, 0:128], func=AF.Copy), reads=[pt], writes=[kht])
            po = psO.next()
            p.op("pe", lambda e: e.matmul(po[0:HC, 0:128], lhsT=atm[:], rhs=v[:, n, :], start=True, stop=False),
                 reads=[atm, v], writes=[po])
            p.op("pe", lambda e: e.matmul(po[0:HC, 0:128], lhsT=qs[:, sl], rhs=Sb[:], start=False, stop=True),
                 reads=[qs, Sb], writes=[po], accum=True)
            pj = HC * (n % 4)
            p.op("act", lambda e: e.activation(out=d["o"][pj:pj + HC, n // 4, :], in_=po[0:HC, 0:128], func=AF.Copy), reads=[po],
                 writes=[d["o"]], accum=(n > 0))
            pS = psS.next()
            p.op("pe", lambda e: e.matmul(pS[:, 0:128], lhsT=kht[:], rhs=v[:, n, :], start=True, stop=True),
                 reads=[kht, v], writes=[pS])
            p.op("dve", lambda e: e.scalar_tensor_tensor(out=S[:], in0=S[:], scalar=d["ebend"][:, n:n + 1], in1=pS[:, 0:128],
                                                         op0=ALU.mult, op1=ALU.add), reads=[S, d["ebend"], pS], writes=[S])
            p.op("act", lambda e: e.activation(out=Sb[:], in_=S[:], func=AF.Copy), reads=[S], writes=[Sb])

    for h in range(2):
        d = H[h]
        o, g, ms = d["o"], d["g"], d["ms"]
        sq = shared["q"]
        NT_ = LP // 128
        sq3 = sq[:].rearrange("p (n c) -> p n c", c=128)
        p.op("pool", lambda e: e.tensor_tensor(out=sq3, in0=o[:], in1=o[:], op=ALU.mult), reads=[o], writes=[sq])
        p.op("dve", lambda e: e.tensor_reduce(out=ms[:], in_=sq3, axis=AX.X, op=ALU.add), reads=[sq], writes=[ms])
        p.op("act", lambda e: e.activation(out=ms[:], in_=ms[:], func=AF.Sqrt, bias=epsc[:, 0:1], scale=1.0 / 128),
             reads=[ms, epsc], writes=[ms])
        p.op("dve", lambda e: e.reciprocal(out=ms[:], in_=ms[:]), reads=[ms], writes=[ms])
        p.op("dve", lambda e: e.tensor_tensor(out=o[:], in0=o[:], in1=ms[:].unsqueeze(2).to_broadcast([128, NT_, 128]), op=ALU.mult),
             reads=[o, ms], writes=[o])
        p.op("pool", lambda e: e.tensor_tensor(out=o[:], in0=o[:], in1=nw[:].unsqueeze(1).to_broadcast([128, NT_, 128]), op=ALU.mult),
             reads=[o, nw], writes=[o])
        p.op("act", lambda e: e.activation(out=g[:], in_=g[:], func=AF.Silu), reads=[g], writes=[g])
        p.op("dve", lambda e: e.tensor_tensor(out=o[:], in0=o[:], in1=g[:], op=ALU.mult), reads=[o, g], writes=[o])
        p.dma("sp", oa[h], o[:], reads=[o])
    p.finish([H[0]["o"], H[1]["o"]])
    return nc


S5N = 6
S5C = L // S5N
PI = 3.14159265358979


def build_B2():
    nc = bass.Bass("TRN2", target_bir_lowering=False)
    p = P(nc)
    uT = nc.dram_tensor("uT", [2, 128, L], F32, kind="ExternalInput").ap()
    are_d = nc.dram_tensor("a_re", [128, 8], F32, kind="ExternalInput").ap()
    aim_d = nc.dram_tensor("a_im", [128, 8], F32, kind="ExternalInput").ap()
    ldt_d = nc.dram_tensor("ldt", [128, 8], F32, kind="ExternalInput").ap()
    bre_d = nc.dram_tensor("b_re", [128, 8, 16], F32, kind="ExternalInput").ap()
    bim_d = nc.dram_tensor("b_im", [128, 8, 16], F32, kind="ExternalInput").ap()
    cre_d = nc.dram_tensor("cT_re", [128, 8, 16], F32, kind="ExternalInput").ap()
    cim_d = nc.dram_tensor("cT_im", [128, 8, 16], F32, kind="ExternalInput").ap()
    dsk_d = nc.dram_tensor("dsk", [128, 2], F32, kind="ExternalInput").ap()
    yT = nc.dram_tensor("yT", [2, 128, L], F32, kind="ExternalOutput").ap()

    def small(name, shape=(128, 8)):
        return p.sb(list(shape), F32, name)

    a_re, a_im, ldt = small("are_s"), small("aim_s"), small("ldt_s")
    b_re, b_im = small("bre_s", (128, 8, 16)), small("bim_s", (128, 8, 16))
    c_re, c_im = small("cre_s", (128, 8, 16)), small("cim_s", (128, 8, 16))
    dsk = small("dsk_s", (128, 2))
    for t_, d_ in ((a_re, are_d), (a_im, aim_d), (ldt, ldt_d), (dsk, dsk_d)):
        p.dma("sp", t_[:], d_[:, :], writes=[t_])
    for t_, d_ in ((b_re, bre_d), (b_im, bim_d), (c_re, cre_d), (c_im, cim_d)):
        p.dma("sp", t_[:], d_[:, :, :], writes=[t_])
    u = p.sb([128, 2, L], F32, "u")
    ub = p.sb([128, 2, L], BF16, "ub")
    p.dma("sp", u[:], uT.rearrange("k p t -> p k t"), writes=[u])
    p.dma("pool", ub[:], uT.rearrange("k p t -> p k t"), writes=[ub])
    pic = small("pic", (128, 1))
    p.op("pool", lambda e: e.memset(pic[:], PI), writes=[pic])
    identf = p.sb([128, 128], F32, "identf")
    p.op("pool", lambda e: e.memset(identf[:], 0.0), writes=[identf])
    p.op("pool", lambda e: e.affine_select(out=identf[:], in_=identf[:], pattern=[[-1, 128]], compare_op=ALU.not_equal,
                                           fill=1.0, base=0, channel_multiplier=1), reads=[identf], writes=[identf])
    iot = p.sb([128, L], F32, "iot")
    p.op("pool", lambda e: e.iota(iot[:], pattern=[[1, L]], base=0, channel_multiplier=0, allow_small_or_imprecise_dtypes=True), writes=[iot])

    def ts(out, in0, s1, s2, op0, op1=None, eng="dve"):
        rd = [in0] + [x for x in (s1, s2) if isinstance(x, T)]
        a1 = s1[:] if isinstance(s1, T) else s1
        a2 = s2[:] if isinstance(s2, T) else s2
        if op1 is None:
            p.op(eng, lambda e: e.tensor_scalar(out=out[:], in0=in0[:], scalar1=a1, scalar2=None, op0=op0), reads=rd, writes=[out])
        else:
            p.op(eng, lambda e: e.tensor_scalar(out=out[:], in0=in0[:], scalar1=a1, scalar2=a2, op0=op0, op1=op1), reads=rd, writes=[out])

    def tt(out, in0, in1, op, eng="dve"):
        p.op(eng, lambda e: e.tensor_tensor(out=out[:], in0=in0[:], in1=in1[:], op=op), reads=[in0, in1], writes=[out])

    I32 = mybir.dt.int32

    def wrap_sin(out, x, tf, ti, shift=0.0):
        if shift != 0.0:
            ts(tf, x, shift, None, ALU.add, eng="pool")
            x = tf
        p.op("dve", lambda e: e.tensor_scalar(out=ti[:], in0=x[:], scalar1=1.0 / (2 * PI), scalar2=None, op0=ALU.mult), reads=[x], writes=[ti])
        kf = out
        p.op("pool", lambda e: e.tensor_copy(out=kf[:], in_=ti[:]), reads=[ti], writes=[kf])
        p.op("dve", lambda e: e.scalar_tensor_tensor(out=tf[:], in0=kf[:], scalar=-2 * PI, in1=x[:], op0=ALU.mult, op1=ALU.add),
             reads=[kf, x], writes=[tf])
        ts(tf, tf, PI, -PI, ALU.min, ALU.max, eng="pool")
        p.op("act", lambda e: e.activation(out=out[:], in_=tf[:], func=AF.Sin), reads=[tf], writes=[out])

    dt, th, mag, tmp, tmp2 = small("dt"), small("th"), small("mag"), small("tmp"), small("tmp2")
    s1, c1, abr, abi, den, zre, zim = (small(n) for n in ("s1", "c1", "abr", "abi", "den", "zre", "zim"))
    p.op("act", lambda e: e.activation(out=dt[:], in_=ldt[:], func=AF.Exp), reads=[ldt], writes=[dt])
    tt(tmp, a_re, dt, ALU.mult)
    p.op("act", lambda e: e.activation(out=mag[:], in_=tmp[:], func=AF.Exp), reads=[tmp], writes=[mag])
    tt(th, a_im, dt, ALU.mult)
    smi = p.sb([128, 8], I32, "smi")
    p.op("dve", lambda e: e.tensor_scalar(out=smi[:], in0=th[:], scalar1=1.0 / (2 * PI), scalar2=None, op0=ALU.mult), reads=[th], writes=[smi])
    p.op("dve", lambda e: e.tensor_copy(out=tmp[:], in_=smi[:]), reads=[smi], writes=[tmp])
    p.op("dve", lambda e: e.scalar_tensor_tensor(out=th[:], in0=tmp[:], scalar=-2 * PI, in1=th[:], op0=ALU.mult, op1=ALU.add),
         reads=[tmp, th], writes=[th])
    wrap_sin(s1, th, tmp, smi)
    wrap_sin(c1, th, tmp, smi, shift=PI / 2)
    tt(abr, mag, c1, ALU.mult)
    tt(abi, mag, s1, ALU.mult)
    tt(den, a_re, a_re, ALU.mult)
    tt(tmp, a_im, a_im, ALU.mult)
    tt(den, den, tmp, ALU.add)
    p.op("dve", lambda e: e.reciprocal(out=den[:], in_=den[:]), reads=[den], writes=[den])
    ts(tmp, abr, -1.0, None, ALU.add)
    tt(zre, tmp, a_re, ALU.mult)
    tt(tmp2, abi, a_im, ALU.mult)
    tt(zre, zre, tmp2, ALU.add)
    tt(zre, zre, den, ALU.mult)
    tt(zim, abi, a_re, ALU.mult)
    tt(tmp2, tmp, a_im, ALU.mult)
    tt(zim, zim, tmp2, ALU.subtract)
    tt(zim, zim, den, ALU.mult)
    bbr, bbi, t3a, t3b = (small(n, (128, 8, 16)) for n in ("bbr", "bbi", "t3a", "t3b"))

    def bc(x):
        return x[:].unsqueeze(2).to_broadcast([128, 8, 16])

    p.op("dve", lambda e: e.tensor_tensor(out=t3a[:], in0=b_re[:], in1=bc(zre), op=ALU.mult), reads=[b_re, zre], writes=[t3a])
    p.op("dve", lambda e: e.tensor_tensor(out=t3b[:], in0=b_im[:], in1=bc(zim), op=ALU.mult), reads=[b_im, zim], writes=[t3b])
    tt(bbr, t3a, t3b, ALU.subtract)
    p.op("dve", lambda e: e.tensor_tensor(out=t3a[:], in0=b_im[:], in1=bc(zre), op=ALU.mult), reads=[b_im, zre], writes=[t3a])
    p.op("dve", lambda e: e.tensor_tensor(out=t3b[:], in0=b_re[:], in1=bc(zim), op=ALU.mult), reads=[b_re, zim], writes=[t3b])
    tt(bbi, t3a, t3b, ALU.add)
    ts(c_im, c_im, -1.0, None, ALU.mult)

    ps_rot = Rot([p.ps([128, 512], F32, f"ps{i}") for i in range(6)])
    WB = [[p.sb([128, 128], BF16, f"WB{ri}_{j}") for j in range(8)] for ri in range(2)]
    WC = [[p.sb([128, 128], BF16, f"WC{ri}_{j}") for j in range(8)] for ri in range(2)]
    stg = Rot([p.sb([128, 128], F32, f"stg{i}") for i in range(2)])
    for j in range(8):
        j4 = j % 4
        for ri, src in enumerate((bbr, bbi)):
            st = stg.next()
            p.op("pool", lambda e: e.memset(st[:], 0.0), writes=[st])
            p.op("pool", lambda e: e.tensor_copy(out=st[0:64, 32 * j4:32 * j4 + 16], in_=src[0:64, j, :]), reads=[src], writes=[st], accum=True)
            p.op("pool", lambda e: e.tensor_copy(out=st[64:128, 32 * j4 + 16:32 * j4 + 32], in_=src[64:128, j, :]), reads=[src], writes=[st], accum=True)
            ps = ps_rot.next()
            p.op("pe", lambda e: e.transpose(out=ps[:, 0:128], in_=st[:], identity=identf[:]), reads=[st, identf], writes=[ps])
            p.op("act", lambda e: e.activation(out=WB[ri][j][:], in_=ps[:, 0:128], func=AF.Copy), reads=[ps], writes=[WB[ri][j]])
        for ri, src in enumerate((c_re, c_im)):
            w = WC[ri][j]
            p.op("pool", lambda e: e.memset(w[:], 0.0), writes=[w])
            p.op("pool", lambda e: e.tensor_copy(out=w[0:64, 32 * j4:32 * j4 + 16], in_=src[0:64, j, :]), reads=[src], writes=[w], accum=True)
            p.op("pool", lambda e: e.tensor_copy(out=w[64:128, 32 * j4 + 16:32 * j4 + 32], in_=src[64:128, j, :]), reads=[src], writes=[w], accum=True)

    big = lambda n, dt_=F32: p.sb([128, L], dt_, n)
    sinT, cosT, arg, rB = big("sinT"), big("cosT"), big("arg"), big("rB")
    argi = p.sb([128, L], mybir.dt.int32, "argi")
    bur, bui, wr, wi, ta, tb = big("bur"), big("bui"), big("wr"), big("wi"), big("ta"), big("tb")
    xs = [[big(f"x{ri}_{j4}", BF16) for j4 in range(4)] for ri in range(2)]
    yo = Rot([big(f"yo{i}") for i in range(2)])
    evac = Evac(p)
    for j in range(8):
        k, j4 = j // 4, j % 4
        p.op("dve", lambda e: e.tensor_scalar(out=arg[:], in0=iot[:], scalar1=th[:, j:j + 1], scalar2=None, op0=ALU.mult),
             reads=[iot, th], writes=[arg])
        wrap_sin(sinT, arg, ta, argi)
        wrap_sin(cosT, arg, ta, argi, shift=PI / 2)
        p.op("pool", lambda e: e.memset(rB[:], 1.0), writes=[rB])
        p.op("pool", lambda e: e.tensor_scalar(out=rB[:], in0=rB[:], scalar1=mag[:, j:j + 1], scalar2=None, op0=ALU.mult), reads=[rB, mag], writes=[rB])
        for n in range(S5N):
            sl = slice(n * S5C, (n + 1) * S5C)
            for ri, dst in enumerate((bur, bui)):
                ps = ps_rot.next()
                p.op("pe", lambda e: e.matmul(ps[:, 0:S5C], lhsT=WB[ri][j][:], rhs=ub[:, k, sl], start=True, stop=True),
                     reads=[WB[ri][j], ub], writes=[ps])
                evac(dst[:, sl], ps[:, 0:S5C], [ps], [dst], first=(n == 0))
        tt(ta, cosT, bur, ALU.mult)
        tt(tb, sinT, bui, ALU.mult, eng="pool")
        tt(wr, ta, tb, ALU.add)
        tt(ta, cosT, bui, ALU.mult)
        tt(tb, sinT, bur, ALU.mult, eng="pool")
        tt(wi, ta, tb, ALU.subtract)
        p.op("dve", lambda e: e.tensor_tensor_scan(out=bur[:], data0=rB[:], data1=wr[:], initial=0.0, op0=ALU.mult, op1=ALU.add),
             reads=[rB, wr], writes=[bur])
        p.op("dve", lambda e: e.tensor_tensor_scan(out=bui[:], data0=rB[:], data1=wi[:], initial=0.0, op0=ALU.mult, op1=ALU.add),
             reads=[rB, wi], writes=[bui])
        tt(ta, cosT, bur, ALU.mult)
        tt(tb, sinT, bui, ALU.mult, eng="pool")
        tt(xs[0][j4], ta, tb, ALU.subtract)
        tt(wr, cosT, bui, ALU.mult)
        tt(wi, sinT, bur, ALU.mult, eng="pool")
        tt(xs[1][j4], wr, wi, ALU.add)
        if j4 == 3:
            y = yo.next()
            for n in range(S5N):
                sl = slice(n * S5C, (n + 1) * S5C)
                ps = ps_rot.next()
                i = 0
                for jj in range(4):
                    for ri in range(2):
                        p.op("pe", lambda e: e.matmul(ps[:, 0:S5C], lhsT=WC[ri][4 * k + jj][:], rhs=xs[ri][jj][:, sl], start=(i == 0), stop=(i == 7)),
                             reads=[WC[ri][4 * k + jj], xs[ri][jj]], writes=[ps], accum=(i > 0))
                        i += 1
                p.op("dve", lambda e: e.scalar_tensor_tensor(out=y[:, sl], in0=u[:, k, sl], scalar=dsk[:, k:k + 1], in1=ps[:, 0:S5C],
                                                             op0=ALU.mult, op1=ALU.add), reads=[u, dsk, ps], writes=[y], accum=(n > 0))
            tt(ta, y, y, ALU.mult)
            tt(ta, ta, y, ALU.mult)
            p.op("dve", lambda e: e.scalar_tensor_tensor(out=ta[:], in0=ta[:], scalar=0.044715, in1=y[:], op0=ALU.mult, op1=ALU.add),
                 reads=[ta, y], writes=[ta])
            p.op("act", lambda e: e.activation(out=ta[:], in_=ta[:], func=AF.Sigmoid, scale=1.5957691216), reads=[ta], writes=[ta])
            tt(y, y, ta, ALU.mult)
            p.dma("sp", yT[k], y[:], reads=[y])
    p.finish(yo.tiles)
    return nc


NQT = 17
DH = 128
DV = 256


def build_B3():
    nc = bass.Bass("TRN2", target_bir_lowering=False)
    p = P(nc)
    qT_d = nc.dram_tensor("qT", [4, 128, L], F32, kind="ExternalInput").ap()
    kT_d = nc.dram_tensor("kT", [4, 128, L], F32, kind="ExternalInput").ap()
    v_d = nc.dram_tensor("v", [128, 2, NQT, DV], F32, kind="ExternalInput").ap()
    lam_d = nc.dram_tensor("lamv", [128, 4, DH], F32, kind="ExternalInput").ap()
    sw_d = nc.dram_tensor("subw", [128, DV], F32, kind="ExternalInput").ap()
    li_d = nc.dram_tensor("linit", [128, 2], F32, kind="ExternalInput").ap()
    oc = nc.dram_tensor("oc", [128, 2, NQT, DV], F32, kind="ExternalOutput").ap()

    qT = p.sb([128, 4, L], BF16, "qT_s")
    kT = p.sb([128, 4, L], BF16, "kT_s")
    V = p.sb([128, 2, NQT, DV + 1], BF16, "V")
    lamv = p.sb([128, 4, DH], F32, "lamv_s")
    subw = p.sb([128, DV], F32, "subw_s")
    linit = p.sb([128, 2], F32, "linit_s")
    epsc = p.sb([128, 1], F32, "epsc")
    p.op("pool", lambda e: e.memset(epsc[:], EPS), writes=[epsc])
    p.op("pool", lambda e: e.memset(V[:], 1.0), writes=[V])
    p.dma("pool", V[:, :, :, 0:DV], v_d[:, :, :, :], writes=[V])
    for i in range(4):
        p.dma("pool", qT[:, i, :], qT_d[i], writes=[qT], concurrent=True)
        p.dma("pool", kT[:, i, :], kT_d[i], writes=[kT], concurrent=True)
    p.dma("sp", lamv[:], lam_d[:, :, :], writes=[lamv])
    p.dma("sp", subw[:], sw_d[:, :], writes=[subw])
    p.dma("sp", linit[:], li_d[:, :], writes=[linit])
    lt = p.sb([128, 2, DH], F32, "lt")
    ls = p.sb([128, 2], F32, "ls")
    nlam = p.sb([128, 1], F32, "nlam")
    p.op("dve", lambda e: e.tensor_tensor(out=lt[:, 0, :], in0=lamv[:, 0, :], in1=lamv[:, 1, :], op=ALU.mult), reads=[lamv], writes=[lt])
    p.op("dve", lambda e: e.tensor_tensor(out=lt[:, 1, :], in0=lamv[:, 2, :], in1=lamv[:, 3, :], op=ALU.mult), reads=[lamv], writes=[lt], accum=True)
    p.op("dve", lambda e: e.tensor_reduce(out=ls[:], in_=lt[:], axis=AX.X, op=ALU.add), reads=[lt], writes=[ls])
    p.op("act", lambda e: e.activation(out=ls[:], in_=ls[:], func=AF.Exp), reads=[ls], writes=[ls])
    p.op("dve", lambda e: e.tensor_tensor(out=nlam[:], in0=ls[:, 1:2], in1=ls[:, 0:1], op=ALU.subtract), reads=[ls], writes=[nlam])
    p.op("dve", lambda e: e.tensor_tensor(out=nlam[:], in0=nlam[:], in1=linit[:, 0:1], op=ALU.subtract), reads=[nlam, linit], writes=[nlam])

    acc = [p.sb([128, NQT, DV], F32, f"acc{h}") for h in range(2)]
    psS = Rot([p.ps([128, 512], F32, f"psS{i}") for i in range(3)])
    psO = [p.ps([128, 512], F32, f"psO{i}") for i in range(4)]
    PT = Rot([p.sb([128, 512], BF16, f"PT{i}") for i in range(3)])
    rl = Rot([p.sb([128, 1], F32, f"rl{i}") for i in range(4)])
    on = Rot([p.sb([128, DV], F32, f"on{i}") for i in range(2)])
    scale = DH ** -0.5

    def tok0(i):
        return (0, 16) if i == 0 else (16 + 128 * (i - 1), 128)

    def finish_tile(h, s, i, po, nq):
        r = rl.next()
        p.op("dve", lambda e: e.reciprocal(out=r[0:nq, :], in_=po[0:nq, DV:DV + 1]), reads=[po], writes=[r])
        if s == 0:
            p.op("act", lambda e: e.activation(out=acc[h][0:nq, i, :], in_=po[0:nq, 0:DV], func=AF.Copy, scale=r[0:nq, 0:1]),
                 reads=[po, r], writes=[acc[h]], accum=True)
        else:
            o_ = on.next()
            p.op("act", lambda e: e.activation(out=o_[0:nq, :], in_=po[0:nq, 0:DV], func=AF.Copy, scale=r[0:nq, 0:1]),
                 reads=[po, r], writes=[o_])
            p.op("dve", lambda e: e.scalar_tensor_tensor(out=acc[h][0:nq, i, :], in0=o_[0:nq, :], scalar=nlam[0:nq, 0:1],
                                                         in1=acc[h][0:nq, i, :], op0=ALU.mult, op1=ALU.add),
                 reads=[o_, nlam, acc[h]], writes=[acc[h]])

    for h in range(2):
        p.op("pool", lambda e: e.memset(acc[h][:], 0.0), writes=[acc[h]])
    for h in range(2):
        for s in range(2):
            hs = 2 * h + s
            ps = psS.next()
            p.op("pe", lambda e: e.matmul(ps[0:16, 0:16], lhsT=kT[:, hs, 0:16], rhs=qT[:, hs, 0:16], start=True, stop=True),
                 reads=[kT, qT], writes=[ps])
            pt = PT.next()
            p.op("act", lambda e: e.activation(out=pt[0:16, 0:16], in_=ps[0:16, 0:16], func=AF.Exp, scale=scale), reads=[ps], writes=[pt])
            po = psO[0]
            p.op("pe", lambda e: e.matmul(po[0:16, 0:DV + 1], lhsT=pt[0:16, 0:16], rhs=V[0:16, h, 0, :], start=True, stop=True),
                 reads=[pt, V], writes=[po])
            finish_tile(h, s, 0, po, 16)
            for g in range(4):
                q0 = 16 + 512 * g
                tiles = [4 * g + 1 + a for a in range(4)]
                for j in range(0, 4 * g + 5):
                    k0, nk = tok0(j)
                    ps = psS.next()
                    p.op("pe", lambda e: e.matmul(ps[0:nk, 0:512], lhsT=kT[:, hs, k0:k0 + nk], rhs=qT[:, hs, q0:q0 + 512], start=True, stop=True),
                         reads=[kT, qT], writes=[ps])
                    pt = PT.next()
                    p.op("act", lambda e: e.activation(out=pt[0:nk, :], in_=ps[0:nk, 0:512], func=AF.Exp, scale=scale), reads=[ps], writes=[pt])
                    if j in tiles:
                        a = tiles.index(j)
                        p.op("pool", lambda e: e.memset(pt[64:128, 128 * a:128 * a + 64], 0.0), reads=[pt], writes=[pt])
                    for a, i in enumerate(tiles):
                        if i < j:
                            continue
                        p.op("pe", lambda e: e.matmul(psO[a][:, 0:DV + 1], lhsT=pt[0:nk, 128 * a:128 * (a + 1)], rhs=V[0:nk, h, j, :],
                                                      start=(j == 0), stop=(j == i)), reads=[pt, V], writes=[psO[a]], accum=(j > 0))
                        if j == i:
                            finish_tile(h, s, i, psO[a], 128)
    sq = p.sb([128, NQT, DV], F32, "sq")
    ms = p.sb([128, NQT], F32, "ms")
    for h in range(2):
        a = acc[h]
        p.op("pool", lambda e: e.tensor_tensor(out=sq[:], in0=a[:], in1=a[:], op=ALU.mult), reads=[a], writes=[sq])
        p.op("dve", lambda e: e.tensor_reduce(out=ms[:], in_=sq[:], axis=AX.X, op=ALU.add), reads=[sq], writes=[ms])
        p.op("act", lambda e: e.activation(out=ms[:], in_=ms[:], func=AF.Sqrt, bias=epsc[:, 0:1], scale=1.0 / DV), reads=[ms, epsc], writes=[ms])
        p.op("dve", lambda e: e.reciprocal(out=ms[:], in_=ms[:]), reads=[ms], writes=[ms])
        p.op("dve", lambda e: e.tensor_scalar(out=ms[:], in0=ms[:], scalar1=linit[:, 1:2], scalar2=None, op0=ALU.mult), reads=[ms, linit], writes=[ms])
        p.op("dve", lambda e: e.tensor_tensor(out=a[:], in0=a[:], in1=ms[:].unsqueeze(2).to_broadcast([128, NQT, DV]), op=ALU.mult),
             reads=[a, ms], writes=[a])
        p.op("pool", lambda e: e.tensor_tensor(out=a[:], in0=a[:], in1=subw[:].unsqueeze(1).to_broadcast([128, NQT, DV]), op=ALU.mult),
             reads=[a, subw], writes=[a])
        p.dma("sp", oc[:, h, :, :], a[:], reads=[a])
    p.finish(acc)
    return nc


NTP = 1152
NTT = NTP // 128
TCP = NTP // 3
NE = 64
NR = 72
BIGNEG = -1.0e30


def build_C1():
    nc = bass.Bass("TRN2", target_bir_lowering=False)
    p = P(nc)
    zT = nc.dram_tensor("zT", [D, NT], F32, kind="ExternalInput").ap()
    oaT = nc.dram_tensor("oaT", [512, NT], F32, kind="ExternalInput").ap()
    ysT = nc.dram_tensor("ysT", [512, NT], F32, kind="ExternalInput").ap()
    ocT = nc.dram_tensor("ocT", [1024, NT], F32, kind="ExternalInput").ap()
    wglu_d = nc.dram_tensor("w_glu", [512, 1024], F32, kind="ExternalInput").ap()
    bglu_d = nc.dram_tensor("b_glu", [128, 8], F32, kind="ExternalInput").ap()
    wout_d = nc.dram_tensor("w_out", [D, D], F32, kind="ExternalInput").ap()
    ln2_d = nc.dram_tensor("ln2", [128, KC], F32, kind="ExternalInput").ap()
    wr_d = nc.dram_tensor("w_r", [D, NR], F32, kind="ExternalInput").ap()
    br_d = nc.dram_tensor("b_r", [128, NR], F32, kind="ExternalInput").ap()
    z1T = nc.dram_tensor("z1T", [D, NT], F32, kind="ExternalOutput").ap()
    xtm_d = nc.dram_tensor("xtm", [128, NTT, D], BF16, kind="ExternalOutput").ap()
    rankp_d = nc.dram_tensor("rankp", [128, NTT, NE], F32, kind="ExternalOutput").ap()
    slotT_d = nc.dram_tensor("slotT", [NE, NTP], F32, kind="ExternalOutput").ap()
    gtT_d = nc.dram_tensor("gtT", [NE, NTP], F32, kind="ExternalOutput").ap()

    z = p.sb([128, KC, NTP], F32, "z")
    mx = p.sb([128, KC, NTP], BF16, "mx")
    bglu = p.sb([128, 8], F32, "bglu_s")
    ln2 = p.sb([128, KC], F32, "ln2_s")
    wr = p.sb([128, KC, NR], F32, "wr_s")
    br = p.sb([128, NR], F32, "br_s")
    rstd = p.sb([128, NTP], F32, "rstd")
    ones_bf = p.sb([128, 128], BF16, "ones_bf")
    onesD = p.sb([128, 128], BF16, "onesD")
    epsc = p.sb([128, 1], F32, "epsc")
    identf = p.sb([128, 128], F32, "identf")
    identb = p.sb([128, 128], BF16, "identb")
    ustr = p.sb([128, 128], BF16, "ustr")
    vmask = p.sb([128, NTT], F32, "vmask")
    ps_rot = Rot([p.ps([128, 512], F32, f"ps{i}") for i in range(6)])
    psb_rot = Rot([p.ps([128, 512], BF16, f"psb{i}") for i in range(2)])
    evac = Evac(p)
    ys = p.sb([128, 4, NTP], BF16, "ys")
    wg = p.sb([128, 4, 1024], BF16, "wg")
    sg_rot = Rot([p.sb([128, TCP], F32, f"sg{i}") for i in range(2)])
    wb_rot = Rot([p.sb([128, KC, 512], BF16, f"wb{i}") for i in range(2)])

    p.op("pool", lambda e: e.memset(z[:, :, NT:NTP], 0.0), writes=[z])
    p.op("pool", lambda e: e.memset(mx[:, :, NT:NTP], 0.0), writes=[mx])
    p.op("pool", lambda e: e.memset(ys[:, :, NT:NTP], 0.0), writes=[ys])
    zv = zT.rearrange("(c p) t -> p c t", p=128)
    for q in range(4):
        p.dma("sp", z[:, 4 * q:4 * q + 4, 0:NT], zv[:, 4 * q:4 * q + 4, :], writes=[z], concurrent=True)
    p.dma("pool", ys[:, :, 0:NT], ysT.rearrange("(c p) t -> p c t", p=128), writes=[ys])
    p.dma("pool", wg[:], wglu_d.rearrange("(c p) n -> p c n", p=128), writes=[wg])
    p.dma("pool", mx[:, 0:4, 0:NT], oaT.rearrange("(c p) t -> p c t", p=128), writes=[mx], concurrent=True)
    p.dma("pool", mx[:, 8:16, 0:NT], ocT.rearrange("(c p) t -> p c t", p=128), writes=[mx], concurrent=True)
    for t_, d_ in ((bglu, bglu_d), (ln2, ln2_d), (br, br_d)):
        p.dma("sp", t_[:], d_[:, :], writes=[t_])
    p.dma("sp", wr[:], wr_d.rearrange("(c p) n -> p c n", p=128), writes=[wr])
    p.op("pool", lambda e: e.memset(ones_bf[:], 1.0), writes=[ones_bf])
    p.op("pool", lambda e: e.memset(onesD[:], 1.0 / D), writes=[onesD])
    p.op("pool", lambda e: e.memset(epsc[:], EPS), writes=[epsc])
    p.op("pool", lambda e: e.memset(identf[:], 0.0), writes=[identf])
    p.op("pool", lambda e: e.affine_select(out=identf[:], in_=identf[:], pattern=[[-1, 128]], compare_op=ALU.not_equal,
                                           fill=1.0, base=0, channel_multiplier=1), reads=[identf], writes=[identf])
    p.op("pool", lambda e: e.tensor_copy(out=identb[:], in_=identf[:]), reads=[identf], writes=[identb])
    p.op("pool", lambda e: e.memset(ustr[:], 1.0), writes=[ustr])
    p.op("pool", lambda e: e.affine_select(out=ustr[:], in_=ustr[:], pattern=[[1, 128]], compare_op=ALU.is_ge, fill=0.0,
                                           base=-1, channel_multiplier=-1), reads=[ustr], writes=[ustr])
    p.op("pool", lambda e: e.memset(vmask[:], 1.0), writes=[vmask])
    p.op("pool", lambda e: e.affine_select(out=vmask[:], in_=vmask[:], pattern=[[-128, NTT]], compare_op=ALU.is_ge, fill=0.0,
                                           base=NT - 1, channel_multiplier=-1), reads=[vmask], writes=[vmask])

    for m in range(4):
        for n in range(3):
            sl = slice(n * TCP, (n + 1) * TCP)
            pv, pg = ps_rot.next(), ps_rot.next()
            for c in range(4):
                p.op("pe", lambda e: e.matmul(pv[:, 0:TCP], lhsT=wg[:, c, m * 128:(m + 1) * 128], rhs=ys[:, c, sl], start=(c == 0), stop=(c == 3)),
                     reads=[wg, ys], writes=[pv], accum=(c > 0))
            for c in range(4):
                p.op("pe", lambda e: e.matmul(pg[:, 0:TCP], lhsT=wg[:, c, 512 + m * 128:512 + (m + 1) * 128], rhs=ys[:, c, sl], start=(c == 0), stop=(c == 3)),
                     reads=[wg, ys], writes=[pg], accum=(c > 0))
            sg = sg_rot.next()
            p.op("act", lambda e: e.activation(out=sg[:], in_=pg[:, 0:TCP], func=AF.Sigmoid, bias=bglu[:, 4 + m:5 + m]), reads=[pg, bglu], writes=[sg])
            p.op("dve", lambda e: e.scalar_tensor_tensor(out=mx[:, 4 + m, sl], in0=pv[:, 0:TCP], scalar=bglu[:, m:m + 1], in1=sg[:],
                                                         op0=ALU.add, op1=ALU.mult), reads=[pv, bglu, sg], writes=[mx], accum=True)
    wo_v = wout_d.rearrange("(c p) n -> p c n", p=128)
    for g in range(4):
        wb = wb_rot.next()
        p.dma("pool", wb[:], wo_v[:, :, g * 512:(g + 1) * 512], writes=[wb])
        for j in range(4):
            ct = g * 4 + j
            for n in range(3):
                sl = slice(n * TCP, (n + 1) * TCP)
                ps = ps_rot.next()
                for c in range(KC):
                    p.op("pe", lambda e: e.matmul(ps[:, 0:TCP], lhsT=wb[:, c, j * 128:(j + 1) * 128], rhs=mx[:, c, sl], start=(c == 0), stop=(c == KC - 1)),
                         reads=[wb, mx], writes=[ps], accum=(c > 0))
                p.op("dve", lambda e: e.tensor_tensor(out=z[:, ct, sl], in0=z[:, ct, sl], in1=ps[:, 0:TCP], op=ALU.add), reads=[z, ps], writes=[z])
    z1v = z1T.rearrange("(c p) t -> p c t", p=128)
    for q in range(4):
        p.dma("sp", z1v[:, 4 * q:4 * q + 4, :], z[:, 4 * q:4 * q + 4, 0:NT], reads=[z])
    p.barrier()
    p.free([ys, wg] + sg_rot.tiles + wb_rot.tiles)
    hn2 = mx
    sq_rot = Rot([p.sb([128, NTP], BF16, f"sq{i}") for i in range(2)])
    emit_rmsnorm(p, z, ln2, hn2, onesD, epsc, sq_rot, ps_rot, rstd, nt=NTP, tch=TCP)
    xtm = p.sb([128, NTT, D], BF16, "xtm_s")
    for i in range(NTT):
        for cq in range(4):
            pb = psb_rot.next()
            for cc in range(4):
                c = cq * 4 + cc
                p.op("pe", lambda e: e.transpose(out=pb[:, cc * 128:(cc + 1) * 128], in_=hn2[:, c, i * 128:(i + 1) * 128], identity=identb[:]),
                     reads=[hn2, identb], writes=[pb], accum=(cc > 0))
            evac(xtm[:, i, cq * 512:(cq + 1) * 512], pb[:, 0:512], [pb], [xtm], first=False)
    p.dma("sp", xtm_d[:, :, :], xtm[:], reads=[xtm])
    p.op("dve", lambda e: e.tensor_tensor(out=wr[:], in0=wr[:], in1=ln2[:].unsqueeze(2).to_broadcast([128, KC, NR]), op=ALU.mult),
         reads=[wr, ln2], writes=[wr])
    rstd_tm = p.sb([128, NTT], F32, "rstd_tm")
    lg = p.sb([128, NTT, NR], F32, "lg")
    for i in range(NTT):
        pt = ps_rot.next()
        p.op("pe", lambda e: e.transpose(out=pt[:, 0:128], in_=rstd[:, i * 128:(i + 1) * 128], identity=identf[:]), reads=[rstd, identf], writes=[pt])
        p.op("act", lambda e: e.activation(out=rstd_tm[:, i:i + 1], in_=pt[:, 0:1], func=AF.Copy), reads=[pt], writes=[rstd_tm], accum=True)
        ps = ps_rot.next()
        for c in range(KC):
            p.op("pe", lambda e: e.matmul(ps[:, 0:NR], lhsT=z[:, c, i * 128:(i + 1) * 128], rhs=wr[:, c, :], start=(c == 0), stop=(c == KC - 1)),
                 reads=[z, wr], writes=[ps], accum=(c > 0))
        p.op("dve", lambda e: e.scalar_tensor_tensor(out=lg[:, i, :], in0=ps[:, 0:NR], scalar=rstd_tm[:, i:i + 1], in1=br[:],
                                                     op0=ALU.mult, op1=ALU.add), reads=[ps, rstd_tm, br], writes=[lg], accum=True)

    def sbf(name, shape):
        return p.sb(list(shape), F32, name)

    def tt(out_ap, in0_ap, in1_ap, op, reads, writes, eng="dve"):
        p.op(eng, lambda e: e.tensor_tensor(out=out_ap, in0=in0_ap, in1=in1_ap, op=op), reads=reads, writes=writes)

    def red(out, in_ap, op, reads):
        p.op("dve", lambda e: e.tensor_reduce(out=out[:], in_=in_ap, axis=AX.X, op=op), reads=reads, writes=[out])

    gl = lg[:, :, 0:8]
    gmax, gsum, pg_ = sbf("gmax", (128, NTT)), sbf("gsum", (128, NTT)), sbf("pgrp", (128, NTT))
    ge, oh = sbf("ge", (128, NTT, 8)), sbf("oh", (128, NTT, 8))
    red(gmax, gl, ALU.max, [lg])
    b8 = lambda t_: t_[:].unsqueeze(2).to_broadcast([128, NTT, 8])
    tt(ge[:], gl, b8(gmax), ALU.subtract, [lg, gmax], [ge])
    tt(oh[:], gl, b8(gmax), ALU.is_equal, [lg, gmax], [oh])
    p.op("act", lambda e: e.activation(out=ge[:], in_=ge[:], func=AF.Exp), reads=[ge], writes=[ge])
    red(gsum, ge[:], ALU.add, [ge])
    p.op("dve", lambda e: e.reciprocal(out=pg_[:], in_=gsum[:]), reads=[gsum], writes=[pg_])
    el4 = sbf("el4", (128, NTT, 8, 8))
    esel = sbf("esel", (128, NTT, 8))
    lg4 = lg[:, :, 8:NR].rearrange("p t (g j) -> p t g j", j=8)
    tt(el4[:], lg4, oh[:].unsqueeze(3).to_broadcast([128, NTT, 8, 8]), ALU.mult, [lg, oh], [el4])
    red(esel, el4[:].rearrange("p t g j -> p t j g"), ALU.add, [el4])
    m1, m2 = sbf("m1", (128, NTT)), sbf("m2", (128, NTT))
    k1, k2, e2 = sbf("k1", (128, NTT, 8)), sbf("k2", (128, NTT, 8)), sbf("e2", (128, NTT, 8))
    red(m1, esel[:], ALU.max, [esel])
    tt(k1[:], esel[:], b8(m1), ALU.is_equal, [esel, m1], [k1])
    p.op("dve", lambda e: e.scalar_tensor_tensor(out=e2[:], in0=k1[:], scalar=BIGNEG, in1=esel[:], op0=ALU.mult, op1=ALU.add),
         reads=[k1, esel], writes=[e2])
    red(m2, e2[:], ALU.max, [e2])
    tt(k2[:], e2[:], b8(m2), ALU.is_equal, [e2, m2], [k2])
    dd, g1, g2 = sbf("dd", (128, NTT)), sbf("g1", (128, NTT)), sbf("g2", (128, NTT))
    tt(dd[:], m2[:], m1[:], ALU.subtract, [m2, m1], [dd])
    p.op("act", lambda e: e.activation(out=dd[:], in_=dd[:], func=AF.Exp), reads=[dd], writes=[dd])
    p.op("dve", lambda e: e.tensor_scalar(out=dd[:], in0=dd[:], scalar1=1.0, scalar2=None, op0=ALU.add), reads=[dd], writes=[dd])
    p.op("dve", lambda e: e.reciprocal(out=dd[:], in_=dd[:]), reads=[dd], writes=[dd])
    tt(g1[:], pg_[:], dd[:], ALU.mult, [pg_, dd], [g1])
    tt(g2[:], pg_[:], g1[:], ALU.subtract, [pg_, g1], [g2])
    tt(oh[:], oh[:], b8(vmask), ALU.mult, [oh, vmask], [oh])
    gj, aj = sbf("gj", (128, NTT, 8)), sbf("aj", (128, NTT, 8))
    tt(gj[:], k1[:], b8(g1), ALU.mult, [k1, g1], [gj])
    tt(e2[:], k2[:], b8(g2), ALU.mult, [k2, g2], [e2])
    tt(gj[:], gj[:], e2[:], ALU.add, [gj, e2], [gj])
    tt(aj[:], k1[:], k2[:], ALU.add, [k1, k2], [aj])
    Gt, Af = sbf("Gt", (128, NTT, 8, 8)), sbf("Af", (128, NTT, 8, 8))
    Ab = p.sb([128, NTT, NE], BF16, "Ab")
    ohb = oh[:].unsqueeze(3).to_broadcast([128, NTT, 8, 8])
    tt(Gt[:], ohb, gj[:].unsqueeze(2).to_broadcast([128, NTT, 8, 8]), ALU.mult, [oh, gj], [Gt])
    tt(Af[:], ohb, aj[:].unsqueeze(2).to_broadcast([128, NTT, 8, 8]), ALU.mult, [oh, aj], [Af])
    Af2 = Af[:].rearrange("p t g j -> p t (g j)")
    Gt2 = Gt[:].rearrange("p t g j -> p t (g j)")
    p.op("dve", lambda e: e.tensor_copy(out=Ab[:], in_=Af2), reads=[Af], writes=[Ab])
    rankp = sbf("rankp_s", (128, NTT, NE))
    slotT, gtT, AT = sbf("slotT_s", (NE, NTP)), sbf("gtT_s", (NE, NTP)), sbf("AT_s", (NE, NTP))
    for i in range(NTT):
        ps = ps_rot.next()
        for i2 in range(i + 1):
            p.op("pe", lambda e: e.matmul(ps[:, 0:NE], lhsT=(ustr[:] if i2 == i else ones_bf[:]), rhs=Ab[:, i2, :], start=(i2 == 0), stop=(i2 == i)),
                 reads=[ustr, ones_bf, Ab], writes=[ps], accum=(i2 > 0))
        p.op("dve", lambda e: e.scalar_tensor_tensor(out=rankp[:, i, :], in0=ps[:, 0:NE], scalar=1.0, in1=Af2[:, i, :], op0=ALU.add, op1=ALU.mult),
             reads=[ps, Af], writes=[rankp], accum=True)
        ps2 = ps_rot.next()
        for i2 in range(i + 1):
            p.op("pe", lambda e: e.matmul(ps2[0:NE, 0:128], lhsT=Ab[:, i2, :], rhs=(ustr[:] if i2 == i else ones_bf[:]), start=(i2 == 0), stop=(i2 == i)),
                 reads=[ustr, ones_bf, Ab], writes=[ps2], accum=(i2 > 0))
        p.op("act", lambda e: e.activation(out=slotT[:, i * 128:(i + 1) * 128], in_=ps2[0:NE, 0:128], func=AF.Copy), reads=[ps2], writes=[slotT], accum=True)
        ps3 = ps_rot.next()
        p.op("pe", lambda e: e.transpose(out=ps3[0:NE, 0:128], in_=Af2[:, i, :], identity=identf[:]), reads=[Af, identf], writes=[ps3])
        p.op("act", lambda e: e.activation(out=AT[:, i * 128:(i + 1) * 128], in_=ps3[0:NE, 0:128], func=AF.Copy), reads=[ps3], writes=[AT], accum=True)
        ps4 = ps_rot.next()
        p.op("pe", lambda e: e.transpose(out=ps4[0:NE, 0:128], in_=Gt2[:, i, :], identity=identf[:]), reads=[Gt, identf], writes=[ps4])
        p.op("act", lambda e: e.activation(out=gtT[:, i * 128:(i + 1) * 128], in_=ps4[0:NE, 0:128], func=AF.Copy), reads=[ps4], writes=[gtT], accum=True)
    p.op("dve", lambda e: e.tensor_scalar(out=rankp[:], in0=rankp[:], scalar1=-1.0, scalar2=None, op0=ALU.add), reads=[rankp], writes=[rankp])
    p.op("dve", lambda e: e.scalar_tensor_tensor(out=slotT[:], in0=slotT[:], scalar=1.0, in1=AT[:], op0=ALU.add, op1=ALU.mult),
         reads=[slotT, AT], writes=[slotT])
    p.op("dve", lambda e: e.tensor_scalar(out=slotT[:], in0=slotT[:], scalar1=-1.0, scalar2=None, op0=ALU.add), reads=[slotT], writes=[slotT])
    p.dma("sp", rankp_d[:, :, :], rankp[:], reads=[rankp])
    p.dma("sp", slotT_d[:, :], slotT[:], reads=[slotT])
    p.dma("sp", gtT_d[:, :], gtT[:], reads=[gtT])
    p.finish([z, xtm, rankp, slotT, gtT])
    return nc


DF = 512
SC = NT // 3


NSL = 128
NSRC = 8
ESL = NSRC * NSL
EPC = NE // 8


def build_C2a():
    nc = bass.Bass("TRN2", target_bir_lowering=False)
    p = P(nc)
    xtm_d = nc.dram_tensor("xtm", [128, NTT, D], BF16, kind="ExternalInput").ap()
    rankp_d = nc.dram_tensor("rankp", [128, NTT, NE], F32, kind="ExternalInput").ap()
    xe_d = nc.dram_tensor("xeT", [NE, D, NSL], BF16, kind="ExternalOutput").ap()
    X = p.sb([128, NTT, D], BF16, "X")
    rankp = p.sb([128, NTT, NE], F32, "rankp_s")
    iota_s = p.sb([128, 128], F32, "iota_s")
    p.dma("sp", X[:], xtm_d[:, :, :], writes=[X])
    p.dma("sp", rankp[:], rankp_d[:, :, :], writes=[rankp])
    p.op("pool", lambda e: e.iota(iota_s[:], pattern=[[1, 128]], base=0, channel_multiplier=0, allow_small_or_imprecise_dtypes=True), writes=[iota_s])
    S_rot = Rot([p.sb([128, NTT, 4, 128], BF16, f"S{i}") for i in range(2)])
    XeT_rot = Rot([p.sb([128, KC, 512], BF16, f"XeT{i}") for i in range(2)])
    ps_rot = Rot([p.ps([128, 512], F32, f"ps{i}") for i in range(8)])
    evac = Evac(p)
    for qd in range(NE // 4):
        e0 = 4 * qd
        S = S_rot.next()
        p.op("dve", lambda e: e.tensor_tensor(out=S[:], in0=iota_s[:].unsqueeze(1).unsqueeze(1).to_broadcast([128, NTT, 4, 128]),
                                              in1=rankp[:, :, e0:e0 + 4].unsqueeze(3).to_broadcast([128, NTT, 4, 128]), op=ALU.is_equal),
             reads=[iota_s, rankp], writes=[S])
        S2 = S[:].rearrange("p t e s -> p t (e s)")
        XeT = XeT_rot.next()
        for c in range(KC):
            ps = ps_rot.next()
            for i in range(NTT):
                p.op("pe", lambda e: e.matmul(ps[:, 0:512], lhsT=X[:, i, c * 128:(c + 1) * 128], rhs=S2[:, i, :], start=(i == 0), stop=(i == NTT - 1)),
                     reads=[X, S], writes=[ps], accum=(i > 0))
            evac(XeT[:, c, :], ps[:, 0:512], [ps], [XeT], first=(c == 0))
        for ee in range(4):
            p.dma("sp", xe_d[e0 + ee].rearrange("(c p) s -> p c s", p=128), XeT[:, :, ee * 128:(ee + 1) * 128], reads=[XeT])
    p.finish(XeT_rot.tiles)
    return nc


def build_C2b():
    nc = bass.Bass("TRN2", target_bir_lowering=False)
    p = P(nc)
    xe_d = nc.dram_tensor("xe", [EPC, D, ESL], BF16, kind="ExternalInput").ap()
    w1_d = nc.dram_tensor("w1", [EPC, D, DF], F32, kind="ExternalInput").ap()
    w3_d = nc.dram_tensor("w3", [EPC, D, DF], F32, kind="ExternalInput").ap()
    w2_d = nc.dram_tensor("w2", [EPC, DF, D], F32, kind="ExternalInput").ap()
    ye_d = nc.dram_tensor("ye", [EPC, ESL, D], BF16, kind="ExternalOutput").ap()
    W1 = Rot([p.sb([128, KC, DF], BF16, f"W1_{i}") for i in range(2)])
    W3 = Rot([p.sb([128, KC, DF], BF16, f"W3_{i}") for i in range(2)])
    W2 = Rot([p.sb([128, 4, D], BF16, f"W2_{i}") for i in range(2)])
    Xe = Rot([p.sb([128, KC, ESL], BF16, f"Xe{i}") for i in range(2)])
    Hg = Rot([p.sb([128, 4, 512], BF16, f"Hg{i}") for i in range(2)])
    h1 = Rot([p.sb([128, 512], F32, f"h1_{i}") for i in range(2)])
    yo = Rot([p.sb([128, D], BF16, f"yo{i}") for i in range(3)])
    ps_rot = Rot([p.ps([128, 512], F32, f"ps{i}") for i in range(8)])
    evac = Evac(p)
    for ex in range(EPC):
        w1, w3, w2, xe = W1.next(), W3.next(), W2.next(), Xe.next()
        p.dma("sp", xe[:], xe_d[ex].rearrange("(c p) s -> p c s", p=128), writes=[xe])
        p.dma("pool", w1[:], w1_d[ex].rearrange("(c p) f -> p c f", p=128), writes=[w1])
        p.dma("pool", w3[:], w3_d[ex].rearrange("(c p) f -> p c f", p=128), writes=[w3])
        p.dma("pool", w2[:], w2_d[ex].rearrange("(m p) d -> p m d", p=128), writes=[w2])
        for half in range(ESL // 512):
            hs = slice(half * 512, (half + 1) * 512)
            hg = Hg.next()
            for m in range(4):
                p1, p3 = ps_rot.next(), ps_rot.next()
                for c in range(KC):
                    p.op("pe", lambda e: e.matmul(p1[:, 0:512], lhsT=w1[:, c, m * 128:(m + 1) * 128], rhs=xe[:, c, hs], start=(c == 0), stop=(c == KC - 1)),
                         reads=[w1, xe], writes=[p1], accum=(c > 0))
                for c in range(KC):
                    p.op("pe", lambda e: e.matmul(p3[:, 0:512], lhsT=w3[:, c, m * 128:(m + 1) * 128], rhs=xe[:, c, hs], start=(c == 0), stop=(c == KC - 1)),
                         reads=[w3, xe], writes=[p3], accum=(c > 0))
                h = h1.next()
                p.op("act", lambda e: e.activation(out=h[:], in_=p1[:, 0:512], func=AF.Silu), reads=[p1], writes=[h])
                p.op("dve", lambda e: e.tensor_tensor(out=hg[:, m, :], in0=h[:], in1=p3[:, 0:512], op=ALU.mult), reads=[h, p3], writes=[hg], accum=(m > 0))
            for st in range(4):
                y = yo.next()
                for n in range(4):
                    ps = ps_rot.next()
                    for m in range(4):
                        p.op("pe", lambda e: e.matmul(ps[:, 0:512], lhsT=hg[:, m, st * 128:(st + 1) * 128], rhs=w2[:, m, n * 512:(n + 1) * 512],
                                                      start=(m == 0), stop=(m == 3)), reads=[hg, w2], writes=[ps], accum=(m > 0))
                    evac(y[:, n * 512:(n + 1) * 512], ps[:, 0:512], [ps], [y], first=(n == 0))
                r0 = half * 512 + st * 128
                p.dma("sp", ye_d[ex, r0:r0 + 128, :], y[:], reads=[y])
    p.finish(yo.tiles)
    return nc


def build_C2c(last):
    nc = bass.Bass("TRN2", target_bir_lowering=False)
    p = P(nc)
    zT = nc.dram_tensor("zT", [D, NT], F32, kind="ExternalInput").ap()
    slotT_d = nc.dram_tensor("slotT", [NE, NTP], F32, kind="ExternalInput").ap()
    gtT_d = nc.dram_tensor("gtT", [NE, NTP], F32, kind="ExternalInput").ap()
    ye_d = nc.dram_tensor("ye", [NE, NSL, D], BF16, kind="ExternalInput").ap()
    fw_d = nc.dram_tensor("fnw", [128, KC], F32, kind="ExternalInput").ap()
    outT = nc.dram_tensor("outT", [D, NT], F32, kind="ExternalOutput").ap()
    z = p.sb([128, KC, NT], F32, "z")
    slotT = p.sb([NE, NTP], BF16, "slotT_s")
    gtT = p.sb([NE, NTP], BF16, "gtT_s")
    fnw = p.sb([128, KC], F32, "fnw_s")
    identb = p.sb([128, 128], BF16, "identb")
    identf = p.sb([128, 128], F32, "identf")
    iota_p = p.sb([128, 1], F32, "iota_p")
    zv = zT.rearrange("(c p) t -> p c t", p=128)
    for q in range(4):
        p.dma("sp", z[:, 4 * q:4 * q + 4, :], zv[:, 4 * q:4 * q + 4, :], writes=[z], concurrent=True)
    p.dma("sp", fnw[:], fw_d[:, :], writes=[fnw])
    p.dma("pool", slotT[:], slotT_d[:, :], writes=[slotT])
    p.dma("pool", gtT[:], gtT_d[:, :], writes=[gtT])
    p.op("pool", lambda e: e.memset(identf[:], 0.0), writes=[identf])
    p.op("pool", lambda e: e.affine_select(out=identf[:], in_=identf[:], pattern=[[-1, 128]], compare_op=ALU.not_equal,
                                           fill=1.0, base=0, channel_multiplier=1), reads=[identf], writes=[identf])
    p.op("pool", lambda e: e.tensor_copy(out=identb[:], in_=identf[:]), reads=[identf], writes=[identb])
    p.op("pool", lambda e: e.iota(iota_p[:], pattern=[[0, 1]], base=0, channel_multiplier=1, allow_small_or_imprecise_dtypes=True), writes=[iota_p])
    NG = 4
    Ye_rot = Rot([p.sb([128, NG, D], BF16, f"Ye{i}") for i in range(2)])
    SgT_rot = Rot([p.sb([128, NG, NT], BF16, f"SgT{i}") for i in range(2)])
    sel_rot = Rot([p.sb([NE, 128], BF16, f"sel{i}") for i in range(2)])
    gB_rot = Rot([p.sb([128, SC], F32, f"gB{i}") for i in range(2)])
    ps_rot = Rot([p.ps([128, 512], F32, f"ps{i}") for i in range(8)])
    for qd in range(NE // NG):
        e0 = NG * qd
        Ye, SgT = Ye_rot.next(), SgT_rot.next()
        p.dma("sp", Ye[:], ye_d[e0:e0 + NG].rearrange("e s d -> s e d"), writes=[Ye])
        for ee in range(NG):
            ex = e0 + ee
            sel = sel_rot.next()
            p.op("dve", lambda e: e.tensor_copy(out=sel[:], in_=identb[0:NE, ex:ex + 1].to_broadcast([NE, 128])), reads=[identb], writes=[sel])
            for n in range(3):
                sl = slice(n * SC, (n + 1) * SC)
                pa, pg = ps_rot.next(), ps_rot.next()
                p.op("pe", lambda e: e.matmul(pa[:, 0:SC], lhsT=sel[:], rhs=slotT[:, sl], start=True, stop=True), reads=[sel, slotT], writes=[pa])
                p.op("pe", lambda e: e.matmul(pg[:, 0:SC], lhsT=sel[:], rhs=gtT[:, sl], start=True, stop=True), reads=[sel, gtT], writes=[pg])
                gB = gB_rot.next()
                p.op("act", lambda e: e.activation(out=gB[:], in_=pg[:, 0:SC], func=AF.Copy), reads=[pg], writes=[gB])
                p.op("dve", lambda e: e.scalar_tensor_tensor(out=SgT[:, ee, sl], in0=pa[:, 0:SC], scalar=iota_p[:, 0:1], in1=gB[:],
                                                             op0=ALU.is_equal, op1=ALU.mult), reads=[pa, iota_p, gB], writes=[SgT], accum=(ee + n > 0))
        for c in range(KC):
            for n in range(3):
                sl = slice(n * SC, (n + 1) * SC)
                ps = ps_rot.next()
                for ee in range(NG):
                    p.op("pe", lambda e: e.matmul(ps[:, 0:SC], lhsT=Ye[:, ee, c * 128:(c + 1) * 128], rhs=SgT[:, ee, sl], start=(ee == 0), stop=(ee == NG - 1)),
                         reads=[Ye, SgT], writes=[ps], accum=(ee > 0))
                p.op("dve", lambda e: e.tensor_tensor(out=z[:, c, sl], in0=z[:, c, sl], in1=ps[:, 0:SC], op=ALU.add), reads=[z, ps], writes=[z])
    ov = outT.rearrange("(c p) t -> p c t", p=128)
    if last:
        rstd = p.sb([128, NT], F32, "rstd")
        onesD = p.sb([128, 128], BF16, "onesD")
        epsc = p.sb([128, 1], F32, "epsc")
        p.op("pool", lambda e: e.memset(onesD[:], 1.0 / D), writes=[onesD])
        p.op("pool", lambda e: e.memset(epsc[:], EPS), writes=[epsc])
        sq_rot = Rot([p.sb([128, NT], BF16, f"sq{i}") for i in range(2)])
        emit_rmsnorm(p, z, fnw, None, onesD, epsc, sq_rot, ps_rot, rstd, hn32=z)
    for q in range(4):
        p.dma("sp", ov[:, 4 * q:4 * q + 4, :], z[:, 4 * q:4 * q + 4, :], reads=[z])
    p.finish([z])
    return nc


_PROGS = {}


def _prog(name, fn, *args):
    key = (name,) + args
    if key not in _PROGS:
        _PROGS[key] = fn(*args)
    return _PROGS[key]


def _run(nc, in_maps):
    res = run_bass_kernel_spmd(nc, in_maps, core_ids=list(range(8)))
    return res.results


def _colT(v, n):
    return np.ascontiguousarray(np.asarray(v, np.float32).reshape(n, 128).T)


def _rep(v, n=128):
    v = np.asarray(v, np.float32)
    return np.ascontiguousarray(np.broadcast_to(v[None], (n,) + v.shape))


def _pairs(a):
    a = np.asarray(a, np.float32)
    sh = a.shape[2:]
    nd = len(sh)
    return np.ascontiguousarray(a.reshape(8, 2, 64, *sh).transpose(1, 2, 0, *range(3, 3 + nd)).reshape(128, 8, *sh))


def _tok_tiles(a):
    o = np.zeros((NQT, 128, a.shape[-1]), np.float32)
    o[0, :NMETA] = a[:NMETA]
    o[1:] = a[NMETA:].reshape(16, 128, -1)
    return o.transpose(1, 0, 2)


def _untok_tiles(o):
    o = o.transpose(1, 0, 2)
    return np.concatenate([o[0, :NMETA], o[1:].reshape(SEQ, -1)], axis=0)


def kernel(x, meta_tokens, ln1_w, w_in, hgrn_lower_bounds, hgrn_norm_w, s5_a_re, s5_a_im,
           s5_b_re, s5_b_im, s5_c_re, s5_c_im, s5_d, s5_log_dt, s5_w_glu, s5_b_glu,
           diff_lambda_q1, diff_lambda_k1, diff_lambda_q2, diff_lambda_k2, diff_subln_w,
           w_out, ln2_w, router_group_w, router_group_b, router_expert_w, router_expert_b,
           expert_w1, expert_w3, expert_w2, final_norm_w):
    import math
    f32 = lambda a: np.asarray(a, np.float32)
    x, meta_tokens = f32(x), f32(meta_tokens)
    z0 = np.concatenate([np.broadcast_to(meta_tokens[None], (NB, NMETA, D)), x], axis=1)
    zTs = [np.ascontiguousarray(z0[c // 2, (c % 2) * NT:(c % 2 + 1) * NT].T) for c in range(8)]
    depth = f32(ln1_w).shape[0]
    hlb = f32(hgrn_lower_bounds)
    fnw = _colT(final_norm_w, KC)
    for layer in range(depth):
        lnw = _colT(f32(ln1_w)[layer], KC)
        wi = f32(w_in)[layer]
        r = _run(_prog("A", build_A), [{"zT": zTs[c], "lnw": lnw, "w_in": wi} for c in range(8)])
        proj = np.stack([np.concatenate([r[2 * b]["projT"].T, r[2 * b + 1]["projT"].T], axis=0) for b in range(NB)])
        del r
        lsel = np.full((128, 1), 1.0 if layer > 0 else 0.0, np.float32)
        nw = _rep(f32(hgrn_norm_w)[layer])
        ins = []
        for c in range(8):
            b, hh = c // 2, c % 2
            hs = [2 * hh, 2 * hh + 1]

            def padT(col0):
                o = np.zeros((2, 128, LP), np.float32)
                for i, h in enumerate(hs):
                    o[i, :, :L] = proj[b, :, col0 + h * 128:col0 + (h + 1) * 128].T
                return o

            def padded(col0):
                o = np.zeros((2, LP, 128), np.float32)
                for i, h in enumerate(hs):
                    o[i, :L] = proj[b, :, col0 + h * 128:col0 + (h + 1) * 128]
                return o

            hv = np.ascontiguousarray(padded(1024).reshape(2, NCK, HC, 128).transpose(0, 2, 1, 3))
            hg = np.ascontiguousarray(padded(1536).reshape(2, LP // 128, 128, 128).transpose(0, 2, 1, 3))
            lbraw = np.ascontiguousarray(np.stack([hlb[0:2, h * 128:(h + 1) * 128].T for h in hs], axis=1))
            ins.append({"hqT": padT(0), "hfT": padT(512), "hv": hv, "hg": hg, "lbraw": lbraw, "lsel": lsel, "nw": nw})
        r = _run(_prog("B1", build_B1), ins)
        o_a = np.zeros((NB, L, 512), np.float32)
        for c in range(8):
            b, hh = c // 2, c % 2
            oa = r[c]["oa"]
            for i in range(2):
                h = 2 * hh + i
                o_a[b, :, h * 128:(h + 1) * 128] = oa[i].transpose(1, 0, 2).reshape(LP, 128)[:L]
        ins = []
        for c in range(8):
            b, hh = c // 2, c % 2
            gs = slice(16 * hh, 16 * hh + 16)
            u = proj[b, :, 2048 + 256 * hh:2048 + 256 * (hh + 1)]
            ins.append({"uT": np.ascontiguousarray(u.T.reshape(2, 128, L)),
                        "a_re": _pairs(f32(s5_a_re)[layer, gs]), "a_im": _pairs(f32(s5_a_im)[layer, gs]),
                        "ldt": _pairs(np.broadcast_to(f32(s5_log_dt)[layer, gs][:, None], (16, 64))),
                        "b_re": _pairs(f32(s5_b_re)[layer, gs]), "b_im": _pairs(f32(s5_b_im)[layer, gs]),
                        "cT_re": _pairs(f32(s5_c_re)[layer, gs].transpose(0, 2, 1)), "cT_im": _pairs(f32(s5_c_im)[layer, gs].transpose(0, 2, 1)),
                        "dsk": np.ascontiguousarray(f32(s5_d)[layer, 256 * hh:256 * (hh + 1)].reshape(2, 128).T)})
        r = _run(_prog("B2", build_B2), ins)
        ys = np.zeros((NB, L, 512), np.float32)
        for c in range(8):
            b, hh = c // 2, c % 2
            ys[b, :, 256 * hh:256 * (hh + 1)] = r[c]["yT"].reshape(256, L).T
        li = 0.8 - 0.6 * math.exp(-0.3 * layer)
        linit = _rep(np.array([li, 1.0 - li], np.float32))
        lamv = _rep(np.stack([f32(diff_lambda_q1)[layer], f32(diff_lambda_k1)[layer], f32(diff_lambda_q2)[layer], f32(diff_lambda_k2)[layer]]))
        subw = _rep(f32(diff_subln_w)[layer])
        ins = []
        for c in range(8):
            b, hh = c // 2, c % 2
            dq = proj[b, :, 2560:3584].reshape(L, 4, 2, 128)
            dk = proj[b, :, 3584:4608].reshape(L, 4, 2, 128)
            dv = proj[b, :, 4608:5632].reshape(L, 4, 256)
            qT = np.ascontiguousarray(dq[:, 2 * hh:2 * hh + 2].reshape(L, 4, 128).transpose(1, 2, 0))
            kT = np.ascontiguousarray(dk[:, 2 * hh:2 * hh + 2].reshape(L, 4, 128).transpose(1, 2, 0))
            v = np.ascontiguousarray(np.stack([_tok_tiles(dv[:, 2 * hh + i]) for i in range(2)], axis=1))
            ins.append({"qT": qT, "kT": kT, "v": v, "lamv": lamv, "subw": subw, "linit": linit})
        r = _run(_prog("B3", build_B3), ins)
        o_c = np.zeros((NB, L, 1024), np.float32)
        for c in range(8):
            b, hh = c // 2, c % 2
            oc = r[c]["oc"]
            for i in range(2):
                h = 2 * hh + i
                o_c[b, :, h * 256:(h + 1) * 256] = _untok_tiles(oc[:, i])
        del proj
        w_r = np.ascontiguousarray(np.concatenate([f32(router_group_w)[layer], f32(router_expert_w)[layer]], axis=1))
        b_r = _rep(np.concatenate([f32(router_group_b)[layer], f32(router_expert_b)[layer]]))
        ins = []
        for c in range(8):
            b, tk = c // 2, slice((c % 2) * NT, (c % 2 + 1) * NT)
            ins.append({"zT": zTs[c], "oaT": np.ascontiguousarray(o_a[b, tk].T), "ysT": np.ascontiguousarray(ys[b, tk].T),
                        "ocT": np.ascontiguousarray(o_c[b, tk].T), "w_glu": f32(s5_w_glu)[layer], "b_glu": _colT(f32(s5_b_glu)[layer], 8),
                        "w_out": f32(w_out)[layer], "ln2": _colT(f32(ln2_w)[layer], KC), "w_r": w_r, "b_r": b_r})
        r1 = _run(_prog("C1", build_C1), ins)
        r2 = _run(_prog("C2a", build_C2a), [{"xtm": r1[c]["xtm"], "rankp": r1[c]["rankp"]} for c in range(8)])
        ins = []
        for g in range(8):
            es = slice(EPC * g, EPC * (g + 1))
            xe = np.ascontiguousarray(np.concatenate([r2[c]["xeT"][es] for c in range(8)], axis=2))
            ins.append({"xe": xe, "w1": f32(expert_w1)[layer, es], "w3": f32(expert_w3)[layer, es], "w2": f32(expert_w2)[layer, es]})
        del r2
        r3 = _run(_prog("C2b", build_C2b), ins)
        last = layer == depth - 1
        ins = []
        for c in range(8):
            ye = np.ascontiguousarray(np.concatenate([r3[g]["ye"][:, c * NSL:(c + 1) * NSL] for g in range(8)], axis=0))
            ins.append({"zT": r1[c]["z1T"], "slotT": r1[c]["slotT"], "gtT": r1[c]["gtT"], "ye": ye, "fnw": fnw})
        del r3
        r4 = _run(_prog("C2c", build_C2c, last), ins)
        zTs = [r4[c]["outT"] for c in range(8)]
    out = np.stack([np.concatenate([zTs[2 * b].T, zTs[2 * b + 1].T], axis=0) for b in range(NB)])
    return np.ascontiguousarray(out[:, NMETA:]).astype(np.float32)
```

```python
import numpy as np
import concourse.bass as bass
import concourse.mybir as mybir
from concourse.bass_utils import run_bass_kernel_spmd

F32 = mybir.dt.float32
BF16 = mybir.dt.bfloat16
AF = mybir.ActivationFunctionType
ALU = mybir.AluOpType
AX = mybir.AxisListType


class Res:
    __slots__ = ("w", "r", "dsem", "dcnt", "t", "name", "cm")

    def __init__(self, name=""):
        self.w = {}
        self.r = {}
        self.dsem = None
        self.dcnt = 0
        self.t = None
        self.name = name


class T(Res):
    def __getitem__(self, idx):
        return self.t[idx]


class P:
    def __init__(self, nc, same_engine_sync=True):
        self.nc = nc
        self.eng = {"pe": nc.tensor, "dve": nc.vector, "act": nc.scalar, "pool": nc.gpsimd, "sp": nc.sync}
        self.sem = {}
        self.cnt = {}
        for e in ("pe", "dve", "act", "pool"):
            self.sem[e] = nc.semaphore("sem_" + e).__enter__()
            self.cnt[e] = 0
        self.semid = {id(s): e for e, s in self.sem.items()}
        self.waited = {e: {} for e in self.eng}
        self.same_engine_sync = same_engine_sync
        self.out_tokens = []
        self.ntile = 0
        self.tiles = []
        self.ptiles = []
        self.prefix = ""

    def sb(self, shape, dt=F32, name=None):
        self.ntile += 1
        name = self.prefix + (name or f"t{self.ntile}")
        t = T(name)
        t.cm = self.nc.sbuf_tensor(name, list(shape), dt)
        t.t = t.cm.__enter__()
        self.tiles.append(t)
        return t

    def ps(self, shape, dt=F32, name=None):
        self.ntile += 1
        name = self.prefix + (name or f"p{self.ntile}")
        t = T(name)
        t.cm = self.nc.psum_tensor(name, list(shape), dt)
        t.t = t.cm.__enter__()
        self.ptiles.append(t)
        return t

    def _wait(self, e, deps, skip_own=False):
        eng = self.eng[e]
        own = self.sem.get(e)
        for sem, val in deps.items():
            if sem is own and (skip_own or not self.same_engine_sync):
                continue
            k = id(sem)
            if self.waited[e].get(k, 0) >= val:
                continue
            eng.wait_ge(sem, val)
            self.waited[e][k] = val

    @staticmethod
    def _merge(d, s):
        for k, v in s.items():
            if d.get(k, 0) < v:
                d[k] = v

    def op(self, e, fn, reads=(), writes=(), accum=False):
        deps = {}
        for r in reads:
            self._merge(deps, r.w)
        for w in writes:
            self._merge(deps, w.w)
            self._merge(deps, w.r)
        self._wait(e, deps, skip_own=(e == "pe"))
        inst = fn(self.eng[e])
        self.cnt[e] += 1
        inst.then_inc(self.sem[e], 1)
        tok = {self.sem[e]: self.cnt[e]}
        for r in reads:
            self._merge(r.r, tok)
        for w in writes:
            if accum:
                self._merge(w.w, tok)
            else:
                w.w = dict(tok)
                w.r = {}
        return inst

    def dma(self, q, out, in_, reads=(), writes=(), semres=None, concurrent=False, **kw):
        if semres is None:
            semres = (list(writes) + list(reads))[0]
        if semres.dsem is None:
            semres.dsem = self.nc.semaphore("d_" + semres.name).__enter__()
        deps = {}
        for r in reads:
            self._merge(deps, r.w)
        for w in writes:
            if concurrent:
                ww = {k: v for k, v in w.w.items() if k is not w.dsem}
                self._merge(deps, ww)
            else:
                self._merge(deps, w.w)
            self._merge(deps, w.r)
        self._wait(q, deps)
        inst = self.eng[q].dma_start(out=out, in_=in_, **kw)
        semres.dcnt += 16
        inst.then_inc(semres.dsem, 16)
        tok = {semres.dsem: semres.dcnt}
        for r in reads:
            self._merge(r.r, tok)
        for w in writes:
            w.w = dict(tok)
            w.r = {}
        return tok

    def barrier(self):
        deps = {}
        for e in ("pe", "dve", "act", "pool"):
            if self.cnt[e] > 0:
                deps[self.sem[e]] = self.cnt[e]
        for t in self.tiles:
            if t.dsem is not None and t.dcnt > 0:
                deps[t.dsem] = t.dcnt
        for e in self.eng:
            self._wait(e, deps, skip_own=True)

    def mark(self):
        return (len(self.tiles), len(self.ptiles))

    def release(self, mark):
        self.barrier()
        for lst, n in ((self.tiles, mark[0]), (self.ptiles, mark[1])):
            while len(lst) > n:
                t = lst.pop()
                t.cm.__exit__(None, None, None)

    def free(self, tiles):
        for t in reversed(tiles):
            t.cm.__exit__(None, None, None)
            self.tiles.remove(t)

    def finish(self, out_res):
        deps = {}
        for r in out_res:
            self._merge(deps, r.r)
            self._merge(deps, r.w)
        self._wait("sp", deps)


D = 2048
NB = 4
SEQ = 2048
NMETA = 16
L = SEQ + NMETA
NT = L // 2
NCH = 3
TCH = NT // NCH
INW = 5632
KC = D // 128
EPS = 1e-6


class Rot:
    def __init__(self, tiles):
        self.tiles = tiles
        self.i = 0

    def next(self):
        t = self.tiles[self.i % len(self.tiles)]
        self.i += 1
        return t


def emit_rmsnorm(p, z, lnw, hn, ones_bf, epsc, sq_rot, ps_rot, rstd, hn32=None, nt=NT, tch=TCH):
    nch = nt // tch
    pss = [ps_rot.next() for _ in range(nch)]
    for c in range(KC):
        sq = sq_rot.next()
        p.op("act", lambda e: e.activation(out=sq[:, 0:nt], in_=z[:, c, 0:nt], func=AF.Square), reads=[z], writes=[sq])
        for n in range(nch):
            p.op("pe", lambda e: e.matmul(pss[n][:, 0:tch], lhsT=ones_bf[:], rhs=sq[:, n * tch:(n + 1) * tch],
                                          start=(c == 0), stop=(c == KC - 1)),
                 reads=[sq, ones_bf], writes=[pss[n]], accum=(c > 0))
    for n in range(nch):
        sl = slice(n * tch, (n + 1) * tch)
        p.op("act", lambda e: e.activation(out=rstd[:, sl], in_=pss[n][:, 0:tch], func=AF.Sqrt, bias=epsc[:, 0:1]),
             reads=[pss[n], epsc], writes=[rstd], accum=(n > 0))
    p.op("dve", lambda e: e.reciprocal(out=rstd[:, 0:nt], in_=rstd[:, 0:nt]), reads=[rstd], writes=[rstd])
    for c in range(KC):
        if hn is not None:
            p.op("dve", lambda e: e.scalar_tensor_tensor(out=hn[:, c, 0:nt], in0=z[:, c, 0:nt], scalar=lnw[:, c:c + 1], in1=rstd[:, 0:nt],
                                                         op0=ALU.mult, op1=ALU.mult), reads=[z, lnw, rstd], writes=[hn])
        if hn32 is not None:
            p.op("dve", lambda e: e.scalar_tensor_tensor(out=hn32[:, c, 0:nt], in0=z[:, c, 0:nt], scalar=lnw[:, c:c + 1], in1=rstd[:, 0:nt],
                                                         op0=ALU.mult, op1=ALU.mult), reads=[z, lnw, rstd], writes=[hn32])


def emit_inproj(p, hn, w_in, projT, wb_rot, ps_rot, st_rot, evac):
    GW = 512
    w_v = w_in.rearrange("(c p) n -> p c n", p=128)
    for g in range(INW // GW):
        wb = wb_rot.next()
        p.dma("pool", wb[:], w_v[:, :, g * GW:(g + 1) * GW], writes=[wb])
        for j in range(GW // 128):
            st = st_rot.next()
            for n in range(NCH):
                ps = ps_rot.next()
                for c in range(KC):
                    p.op("pe", lambda e: e.matmul(ps[:, 0:TCH], lhsT=wb[:, c, j * 128:(j + 1) * 128],
                                                  rhs=hn[:, c, n * TCH:(n + 1) * TCH], start=(c == 0), stop=(c == KC - 1)),
                         reads=[wb, hn], writes=[ps], accum=(c > 0))
                evac(st[:, n * TCH:(n + 1) * TCH], ps[:, 0:TCH], [ps], [st], first=(n == 0))
            r0 = g * GW + j * 128
            p.dma("sp", projT[r0:r0 + 128, :], st[:, 0:NT], reads=[st])


class Evac:
    def __init__(self, p):
        self.p = p
        self.i = 0

    def __call__(self, out, in_, reads, writes, first=True):
        self.i += 1
        p = self.p
        if self.i % 2 == 0:
            p.op("act", lambda e: e.activation(out=out, in_=in_, func=AF.Copy), reads=reads, writes=writes, accum=not first)
        else:
            p.op("dve", lambda e: e.tensor_copy(out=out, in_=in_), reads=reads, writes=writes, accum=not first)


def build_A():
    nc = bass.Bass("TRN2", target_bir_lowering=False)
    p = P(nc)
    zT = nc.dram_tensor("zT", [D, NT], F32, kind="ExternalInput").ap()
    lnw_d = nc.dram_tensor("lnw", [128, KC], F32, kind="ExternalInput").ap()
    w_in = nc.dram_tensor("w_in", [D, INW], F32, kind="ExternalInput").ap()
    projT = nc.dram_tensor("projT", [INW, NT], F32, kind="ExternalOutput").ap()
    z = p.sb([128, KC, NT], F32, "z")
    hn = p.sb([128, KC, NT], BF16, "hn")
    lnw = p.sb([128, KC], F32, "lnw_s")
    rstd = p.sb([128, NT], F32, "rstd")
    ones_bf = p.sb([128, 128], BF16, "ones")
    sq_rot = Rot([p.sb([128, NT], BF16, f"sq{i}") for i in range(2)])
    ps_rot = Rot([p.ps([128, 512], F32, f"ps{i}") for i in range(8)])
    wb_rot = Rot([p.sb([128, KC, 512], BF16, f"wb{i}") for i in range(2)])
    st_rot = Rot([p.sb([128, NT], F32, f"st{i}") for i in range(3)])
    evac = Evac(p)
    zv = zT.rearrange("(c p) t -> p c t", p=128)
    for q in range(4):
        p.dma("sp", z[:, 4 * q:4 * q + 4, :], zv[:, 4 * q:4 * q + 4, :], writes=[z], concurrent=True)
    p.dma("sp", lnw[:], lnw_d[:, :], writes=[lnw])
    p.op("pool", lambda e: e.memset(ones_bf[:], 1.0 / D), writes=[ones_bf])
    epsc = p.sb([128, 1], F32, "epsc")
    p.op("pool", lambda e: e.memset(epsc[:], EPS), writes=[epsc])
    emit_rmsnorm(p, z, lnw, hn, ones_bf, epsc, sq_rot, ps_rot, rstd)
    emit_inproj(p, hn, w_in, projT, wb_rot, ps_rot, st_rot, evac)
    p.finish(st_rot.tiles)
    return nc


HC = 32
LP = 2176
NCK = LP // HC


def build_B1():
    nc = bass.Bass("TRN2", target_bir_lowering=False)
    p = P(nc)
    outs = emit_B1(nc, p)
    p.finish(outs)
    return nc


def emit_B1(nc, p):
    hqT = nc.dram_tensor("hqT", [2, 128, LP], F32, kind="ExternalInput").ap()
    hfT = nc.dram_tensor("hfT", [2, 128, LP], F32, kind="ExternalInput").ap()
    hv = nc.dram_tensor("hv", [2, HC, NCK, 128], F32, kind="ExternalInput").ap()
    hg = nc.dram_tensor("hg", [2, 128, LP // 128, 128], F32, kind="ExternalInput").ap()
    lbraw_d = nc.dram_tensor("lbraw", [128, 2, 2], F32, kind="ExternalInput").ap()
    lsel_d = nc.dram_tensor("lsel", [128, 1], F32, kind="ExternalInput").ap()
    nw_d = nc.dram_tensor("nw", [128, 128], F32, kind="ExternalInput").ap()
    oa = nc.dram_tensor("oa", [2, 128, LP // 128, 128], F32, kind="ExternalOutput").ap()

    lb = p.sb([128, 2], F32, "lb_s")
    oml = p.sb([128, 2], F32, "oml")
    nw = p.sb([128, 128], F32, "nw_s")
    epsc = p.sb([128, 1], F32, "epsc")
    maskU = p.sb([HC, HC], F32, "maskU")
    mm = p.sb([128, LP], F32, "mm")
    ident = p.sb([128, 128], BF16, "ident")
    identf = p.sb([128, 128], F32, "identf")
    lbraw = p.sb([128, 2, 2], F32, "lbraw_s")
    lsel = p.sb([128, 1], F32, "lsel_s")
    p.dma("sp", lbraw[:], lbraw_d[:, :, :], writes=[lbraw])
    p.dma("sp", lsel[:], lsel_d[:, :], writes=[lsel])
    p.op("dve", lambda e: e.tensor_tensor(out=lb[:], in0=lbraw[:, :, 1], in1=lbraw[:, :, 0], op=ALU.subtract), reads=[lbraw], writes=[lb])
    p.op("act", lambda e: e.activation(out=lb[:], in_=lb[:], func=AF.Sigmoid), reads=[lb], writes=[lb])
    p.op("dve", lambda e: e.tensor_scalar(out=lb[:], in0=lb[:], scalar1=lsel[:, 0:1], scalar2=None, op0=ALU.mult), reads=[lb, lsel], writes=[lb])
    p.dma("sp", nw[:], nw_d[:, :], writes=[nw])
    p.op("pool", lambda e: e.memset(epsc[:], EPS), writes=[epsc])
    p.op("pool", lambda e: e.memset(maskU[:], 1.0), writes=[maskU])
    p.op("pool", lambda e: e.affine_select(out=maskU[:], in_=maskU[:], pattern=[[1, HC]], compare_op=ALU.is_ge, fill=0.0,
                                           base=0, channel_multiplier=-1), reads=[maskU], writes=[maskU])
    p.op("pool", lambda e: e.memset(identf[:], 0.0), writes=[identf])
    p.op("pool", lambda e: e.affine_select(out=identf[:], in_=identf[:], pattern=[[-1, 128]], compare_op=ALU.not_equal,
                                           fill=1.0, base=0, channel_multiplier=1), reads=[identf], writes=[identf])
    p.op("pool", lambda e: e.tensor_copy(out=ident[:], in_=identf[:]), reads=[identf], writes=[ident])
    p.op("pool", lambda e: e.memset(mm[:], 1.0), writes=[mm])
    p.op("pool", lambda e: e.memset(mm[:].rearrange("p (n c) -> p n c", c=HC)[:, :, 0:1], 0.0), reads=[mm], writes=[mm])
    p.op("dve", lambda e: e.tensor_scalar(out=oml[:], in0=lb[:], scalar1=-1.0, scalar2=1.0, op0=ALU.mult, op1=ALU.add),
         reads=[lb], writes=[oml])

    H = []
    shared = {nm: p.sb([128, LP], F32, "sh_" + nm) for nm in ("q", "f", "b", "e")}
    for h in range(2):
        d = {}
        for nm in ("q", "f", "b", "e"):
            d[nm] = shared[nm]
        d["qs"] = p.sb([128, LP], BF16, f"qs{h}")
        d["ks"] = p.sb([128, LP], BF16, f"ks{h}")
        d["kh"] = p.sb([128, LP], BF16, f"kh{h}")
        d["ebend"] = p.sb([128, NCK], F32, f"ebend{h}")
        d["v"] = p.sb([HC, NCK, 128], BF16, f"v{h}")
        d["g"] = p.sb([128, LP // 128, 128], F32, f"g{h}")
        d["o"] = p.sb([128, LP // 128, 128], F32, f"o{h}")
        d["S"] = p.sb([128, 128], F32, f"S{h}")
        d["Sb"] = p.sb([128, 128], BF16, f"Sb{h}")
        d["khT"] = Rot([p.sb([HC, 128], BF16, f"khT{h}_{i}") for i in range(3)])
        d["ATm"] = Rot([p.sb([HC, HC], BF16, f"ATm{h}_{i}") for i in range(2)])
        d["ms"] = p.sb([128, LP // 128], F32, f"ms{h}")
        H.append(d)
        p.dma("pool", d["v"][:], hv[h], writes=[d["v"]])
        p.dma("sp", d["g"][:], hg[h], writes=[d["g"]])
    psA = Rot([p.ps([128, 512], F32, f"psA{i}") for i in range(2)])
    psT = Rot([p.ps([128, 512], BF16, f"psT{i}") for i in range(2)])
    psO = Rot([p.ps([128, 512], F32, f"psO{i}") for i in range(2)])
    psS = Rot([p.ps([128, 512], F32, f"psS{i}") for i in range(2)])

    for h in range(2):
        d = H[h]
        q, f, b, e_ = d["q"], d["f"], d["b"], d["e"]
        p.dma("sp", q[:], hqT[h], writes=[q])
        p.dma("sp", f[:], hfT[h], writes=[f])
        p.op("act", lambda e: e.activation(out=f[:], in_=f[:], func=AF.Sigmoid), reads=[f], writes=[f])
        p.op("dve", lambda e: e.tensor_scalar(out=f[:], in0=f[:], scalar1=oml[:, h:h + 1], scalar2=lb[:, h:h + 1],
                                              op0=ALU.mult, op1=ALU.add), reads=[f, oml, lb], writes=[f])
        p.op("act", lambda e: e.activation(out=e_[:], in_=f[:], func=AF.Ln), reads=[f], writes=[e_])
        p.op("dve", lambda e: e.tensor_tensor_scan(out=b[:], data0=mm[:], data1=e_[:], initial=0.0, op0=ALU.mult, op1=ALU.add),
             reads=[mm, e_], writes=[b])
        p.op("dve", lambda e: e.tensor_scalar(out=f[:], in0=f[:], scalar1=-1.0, scalar2=1.0, op0=ALU.mult, op1=ALU.add),
             reads=[f], writes=[f])
        p.op("act", lambda e: e.activation(out=e_[:], in_=b[:], func=AF.Exp), reads=[b], writes=[e_])
        p.op("dve", lambda e: e.tensor_tensor(out=d["qs"][:], in0=q[:], in1=e_[:], op=ALU.mult), reads=[q, e_], writes=[d["qs"]])
        p.op("act", lambda e: e.activation(out=e_[:], in_=b[:], func=AF.Exp, scale=-1.0), reads=[b], writes=[e_])
        p.op("dve", lambda e: e.tensor_tensor(out=d["ks"][:], in0=f[:], in1=e_[:], op=ALU.mult), reads=[f, e_], writes=[d["ks"]])
        b3 = b[:].rearrange("p (n c) -> p n c", c=HC)
        p.op("act", lambda e: e.activation(out=d["ebend"][:], in_=b3[:, :, HC - 1], func=AF.Exp), reads=[b], writes=[d["ebend"]])
        e3 = e_[:].rearrange("p (n c) -> p n c", c=HC)
        p.op("dve", lambda e: e.tensor_tensor(out=e3, in0=b3[:, :, HC - 1:HC].to_broadcast([128, NCK, HC]), in1=b3, op=ALU.subtract),
             reads=[b], writes=[e_])
        p.op("act", lambda e: e.activation(out=e_[:], in_=e_[:], func=AF.Exp), reads=[e_], writes=[e_])
        p.op("dve", lambda e: e.tensor_tensor(out=d["kh"][:], in0=f[:], in1=e_[:], op=ALU.mult), reads=[f, e_], writes=[d["kh"]])
        p.op("pool", lambda e: e.memset(d["S"][:], 0.0), writes=[d["S"]])
        p.op("pool", lambda e: e.memset(d["Sb"][:], 0.0), writes=[d["Sb"]])

    for n in range(NCK):
        for h in range(2):
            d = H[h]
            sl = slice(n * HC, (n + 1) * HC)
            qs, ks, kh, v, S, Sb = d["qs"], d["ks"], d["kh"], d["v"], d["S"], d["Sb"]
            pa = psA.next()
            p.op("pe", lambda e: e.matmul(pa[0:HC, 0:HC], lhsT=ks[:, sl], rhs=qs[:, sl], start=True, stop=True),
                 reads=[ks, qs], writes=[pa])
            atm = d["ATm"].next()
            p.op("dve", lambda e: e.tensor_tensor(out=atm[:], in0=pa[0:HC, 0:HC], in1=maskU[:], op=ALU.mult),
                 reads=[pa, maskU], writes=[atm])
            pt = psT.next()
            p.op("pe", lambda e: e.transpose(out=pt[0:HC, 0:128], in_=kh[:, sl], identity=ident[:]), reads=[kh, ident], writes=[pt])
            kht = d["khT"].next()
            p.op("act", lambda e: e.activation(out=kht[:], in_=pt[0:HC, 0:128], func=AF.Copy), reads=[pt], writes=[kht])
            po = psO.next()
            p.op("pe", lambda e: e.matmul(po[0:HC, 0:128], lhsT=atm[:], rhs=v[:, n, :], start=True, stop=False),
                 reads=[atm, v], writes=[po])
            p.op("pe", lambda e: e.matmul(po[0:HC, 0:128], lhsT=qs[:, sl], rhs=Sb[:], start=False, stop=True),
                 reads=[qs, Sb], writes=[po], accum=True)
            pj = HC * (n % 4)
            p.op("act", lambda e: e.activation(out=d["o"][pj:pj + HC, n // 4, :], in_=po[0:HC, 0:128], func=AF.Copy), reads=[po],
                 writes=[d["o"]], accum=(n > 0))
            pS = psS.next()
            p.op("pe", lambda e: e.matmul(pS[:, 0:128], lhsT=kht[:], rhs=v[:, n, :], start=True, stop=True),
                 reads=[kht, v], writes=[pS])
            p.op("dve", lambda e: e.scalar_tensor_tensor(out=S[:], in0=S[:], scalar=d["ebend"][:, n:n + 1], in1=pS[:, 0:128],
                                                         op0=ALU.mult, op1=ALU.add), reads=[S, d["ebend"], pS], writes=[S])
            p.op("act", lambda e: e.activation(out=Sb[:], in_=S[:], func=AF.Copy), reads=[S], writes=[Sb])

    for h in range(2):
        d = H[h]
        o, g, ms = d["o"], d["g"], d["ms"]
        sq = shared["q"]
        NT_ = LP // 128
        sq3 = sq[:].rearrange("p (n c) -> p n c", c=128)
        p.op("pool", lambda e: e.tensor_tensor(out=sq3, in0=o[:], in1=o[:], op=ALU.mult), reads=[o], writes=[sq])
        p.op("dve", lambda e: e.tensor_reduce(out=ms[:], in_=sq3, axis=AX.X, op=ALU.add), reads=[sq], writes=[ms])
        p.op("act", lambda e: e.activation(out=ms[:], in_=ms[:], func=AF.Sqrt, bias=epsc[:, 0:1], scale=1.0 / 128),
             reads=[ms, epsc], writes=[ms])
        p.op("dve", lambda e: e.reciprocal(out=ms[:], in_=ms[:]), reads=[ms], writes=[ms])
        p.op("dve", lambda e: e.tensor_tensor(out=o[:], in0=o[:], in1=ms[:].unsqueeze(2).to_broadcast([128, NT_, 128]), op=ALU.mult),
             reads=[o, ms], writes=[o])
        p.op("pool", lambda e: e.tensor_tensor(out=o[:], in0=o[:], in1=nw[:].unsqueeze(1).to_broadcast([128, NT_, 128]), op=ALU.mult),
             reads=[o, nw], writes=[o])
        p.op("act", lambda e: e.activation(out=g[:], in_=g[:], func=AF.Silu), reads=[g], writes=[g])
        p.op("dve", lambda e: e.tensor_tensor(out=o[:], in0=o[:], in1=g[:], op=ALU.mult), reads=[o, g], writes=[o])
        p.dma("sp", oa[h], o[:], reads=[o])
    return [H[0]["o"], H[1]["o"]]


S5N = 6
S5C = L // S5N
PI = 3.14159265358979


def build_B2():
    nc = bass.Bass("TRN2", target_bir_lowering=False)
    p = P(nc)
    outs = emit_B2(nc, p)
    p.finish(outs)
    return nc


def emit_B2(nc, p):
    uT = nc.dram_tensor("uT", [2, 128, L], F32, kind="ExternalInput").ap()
    are_d = nc.dram_tensor("a_re", [128, 8], F32, kind="ExternalInput").ap()
    aim_d = nc.dram_tensor("a_im", [128, 8], F32, kind="ExternalInput").ap()
    ldt_d = nc.dram_tensor("ldt", [128, 8], F32, kind="ExternalInput").ap()
    bre_d = nc.dram_tensor("b_re", [128, 8, 16], F32, kind="ExternalInput").ap()
    bim_d = nc.dram_tensor("b_im", [128, 8, 16], F32, kind="ExternalInput").ap()
    cre_d = nc.dram_tensor("cT_re", [128, 8, 16], F32, kind="ExternalInput").ap()
    cim_d = nc.dram_tensor("cT_im", [128, 8, 16], F32, kind="ExternalInput").ap()
    dsk_d = nc.dram_tensor("dsk", [128, 2], F32, kind="ExternalInput").ap()
    yT = nc.dram_tensor("yT", [2, 128, L], F32, kind="ExternalOutput").ap()

    def small(name, shape=(128, 8)):
        return p.sb(list(shape), F32, name)

    a_re, a_im, ldt = small("are_s"), small("aim_s"), small("ldt_s")
    b_re, b_im = small("bre_s", (128, 8, 16)), small("bim_s", (128, 8, 16))
    c_re, c_im = small("cre_s", (128, 8, 16)), small("cim_s", (128, 8, 16))
    dsk = small("dsk_s", (128, 2))
    for t_, d_ in ((a_re, are_d), (a_im, aim_d), (ldt, ldt_d), (dsk, dsk_d)):
        p.dma("sp", t_[:], d_[:, :], writes=[t_])
    for t_, d_ in ((b_re, bre_d), (b_im, bim_d), (c_re, cre_d), (c_im, cim_d)):
        p.dma("sp", t_[:], d_[:, :, :], writes=[t_])
    u = p.sb([128, 2, L], F32, "u")
    ub = p.sb([128, 2, L], BF16, "ub")
    p.dma("sp", u[:], uT.rearrange("k p t -> p k t"), writes=[u])
    p.dma("pool", ub[:], uT.rearrange("k p t -> p k t"), writes=[ub])
    pic = small("pic", (128, 1))
    p.op("pool", lambda e: e.memset(pic[:], PI), writes=[pic])
    identf = p.sb([128, 128], F32, "identf")
    p.op("pool", lambda e: e.memset(identf[:], 0.0), writes=[identf])
    p.op("pool", lambda e: e.affine_select(out=identf[:], in_=identf[:], pattern=[[-1, 128]], compare_op=ALU.not_equal,
                                           fill=1.0, base=0, channel_multiplier=1), reads=[identf], writes=[identf])
    iot = p.sb([128, L], F32, "iot")
    p.op("pool", lambda e: e.iota(iot[:], pattern=[[1, L]], base=0, channel_multiplier=0, allow_small_or_imprecise_dtypes=True), writes=[iot])

    def ts(out, in0, s1, s2, op0, op1=None, eng="dve"):
        rd = [in0] + [x for x in (s1, s2) if isinstance(x, T)]
        a1 = s1[:] if isinstance(s1, T) else s1
        a2 = s2[:] if isinstance(s2, T) else s2
        if op1 is None:
            p.op(eng, lambda e: e.tensor_scalar(out=out[:], in0=in0[:], scalar1=a1, scalar2=None, op0=op0), reads=rd, writes=[out])
        else:
            p.op(eng, lambda e: e.tensor_scalar(out=out[:], in0=in0[:], scalar1=a1, scalar2=a2, op0=op0, op1=op1), reads=rd, writes=[out])

    def tt(out, in0, in1, op, eng="dve"):
        p.op(eng, lambda e: e.tensor_tensor(out=out[:], in0=in0[:], in1=in1[:], op=op), reads=[in0, in1], writes=[out])

    I32 = mybir.dt.int32

    def wrap_sin(out, x, tf, ti, shift=0.0):
        if shift != 0.0:
            ts(tf, x, shift, None, ALU.add, eng="pool")
            x = tf
        p.op("dve", lambda e: e.tensor_scalar(out=ti[:], in0=x[:], scalar1=1.0 / (2 * PI), scalar2=None, op0=ALU.mult), reads=[x], writes=[ti])
        kf = out
        p.op("pool", lambda e: e.tensor_copy(out=kf[:], in_=ti[:]), reads=[ti], writes=[kf])
        p.op("dve", lambda e: e.scalar_tensor_tensor(out=tf[:], in0=kf[:], scalar=-2 * PI, in1=x[:], op0=ALU.mult, op1=ALU.add),
             reads=[kf, x], writes=[tf])
        ts(tf, tf, PI, -PI, ALU.min, ALU.max, eng="pool")
        p.op("act", lambda e: e.activation(out=out[:], in_=tf[:], func=AF.Sin), reads=[tf], writes=[out])

    dt, th, mag, tmp, tmp2 = small("dt"), small("th"), small("mag"), small("tmp"), small("tmp2")
    s1, c1, abr, abi, den, zre, zim = (small(n) for n in ("s1", "c1", "abr", "abi", "den", "zre", "zim"))
    p.op("act", lambda e: e.activation(out=dt[:], in_=ldt[:], func=AF.Exp), reads=[ldt], writes=[dt])
    tt(tmp, a_re, dt, ALU.mult)
    p.op("act", lambda e: e.activation(out=mag[:], in_=tmp[:], func=AF.Exp), reads=[tmp], writes=[mag])
    tt(th, a_im, dt, ALU.mult)
    smi = p.sb([128, 8], I32, "smi")
    p.op("dve", lambda e: e.tensor_scalar(out=smi[:], in0=th[:], scalar1=1.0 / (2 * PI), scalar2=None, op0=ALU.mult), reads=[th], writes=[smi])
    p.op("dve", lambda e: e.tensor_copy(out=tmp[:], in_=smi[:]), reads=[smi], writes=[tmp])
    p.op("dve", lambda e: e.scalar_tensor_tensor(out=th[:], in0=tmp[:], scalar=-2 * PI, in1=th[:], op0=ALU.mult, op1=ALU.add),
         reads=[tmp, th], writes=[th])
    wrap_sin(s1, th, tmp, smi)
    wrap_sin(c1, th, tmp, smi, shift=PI / 2)
    tt(abr, mag, c1, ALU.mult)
    tt(abi, mag, s1, ALU.mult)
    tt(den, a_re, a_re, ALU.mult)
    tt(tmp, a_im, a_im, ALU.mult)
    tt(den, den, tmp, ALU.add)
    p.op("dve", lambda e: e.reciprocal(out=den[:], in_=den[:]), reads=[den], writes=[den])
    ts(tmp, abr, -1.0, None, ALU.add)
    tt(zre, tmp, a_re, ALU.mult)
    tt(tmp2, abi, a_im, ALU.mult)
    tt(zre, zre, tmp2, ALU.add)
    tt(zre, zre, den, ALU.mult)
    tt(zim, abi, a_re, ALU.mult)
    tt(tmp2, tmp, a_im, ALU.mult)
    tt(zim, zim, tmp2, ALU.subtract)
    tt(zim, zim, den, ALU.mult)
    bbr, bbi, t3a, t3b = (small(n, (128, 8, 16)) for n in ("bbr", "bbi", "t3a", "t3b"))

    def bc(x):
        return x[:].unsqueeze(2).to_broadcast([128, 8, 16])

    p.op("dve", lambda e: e.tensor_tensor(out=t3a[:], in0=b_re[:], in1=bc(zre), op=ALU.mult), reads=[b_re, zre], writes=[t3a])
    p.op("dve", lambda e: e.tensor_tensor(out=t3b[:], in0=b_im[:], in1=bc(zim), op=ALU.mult), reads=[b_im, zim], writes=[t3b])
    tt(bbr, t3a, t3b, ALU.subtract)
    p.op("dve", lambda e: e.tensor_tensor(out=t3a[:], in0=b_im[:], in1=bc(zre), op=ALU.mult), reads=[b_im, zre], writes=[t3a])
    p.op("dve", lambda e: e.tensor_tensor(out=t3b[:], in0=b_re[:], in1=bc(zim), op=ALU.mult), reads=[b_re, zim], writes=[t3b])
    tt(bbi, t3a, t3b, ALU.add)
    ts(c_im, c_im, -1.0, None, ALU.mult)

    ps_rot = Rot([p.ps([128, 512], F32, f"ps{i}") for i in range(6)])
    WB = [[p.sb([128, 128], BF16, f"WB{ri}_{j}") for j in range(8)] for ri in range(2)]
    WC = [[p.sb([128, 128], BF16, f"WC{ri}_{j}") for j in range(8)] for ri in range(2)]
    stg = Rot([p.sb([128, 128], F32, f"stg{i}") for i in range(2)])
    for j in range(8):
        j4 = j % 4
        for ri, src in enumerate((bbr, bbi)):
            st = stg.next()
            p.op("pool", lambda e: e.memset(st[:], 0.0), writes=[st])
            p.op("pool", lambda e: e.tensor_copy(out=st[0:64, 32 * j4:32 * j4 + 16], in_=src[0:64, j, :]), reads=[src], writes=[st], accum=True)
            p.op("pool", lambda e: e.tensor_copy(out=st[64:128, 32 * j4 + 16:32 * j4 + 32], in_=src[64:128, j, :]), reads=[src], writes=[st], accum=True)
            ps = ps_rot.next()
            p.op("pe", lambda e: e.transpose(out=ps[:, 0:128], in_=st[:], identity=identf[:]), reads=[st, identf], writes=[ps])
            p.op("act", lambda e: e.activation(out=WB[ri][j][:], in_=ps[:, 0:128], func=AF.Copy), reads=[ps], writes=[WB[ri][j]])
        for ri, src in enumerate((c_re, c_im)):
            w = WC[ri][j]
            p.op("pool", lambda e: e.memset(w[:], 0.0), writes=[w])
            p.op("pool", lambda e: e.tensor_copy(out=w[0:64, 32 * j4:32 * j4 + 16], in_=src[0:64, j, :]), reads=[src], writes=[w], accum=True)
            p.op("pool", lambda e: e.tensor_copy(out=w[64:128, 32 * j4 + 16:32 * j4 + 32], in_=src[64:128, j, :]), reads=[src], writes=[w], accum=True)

    big = lambda n, dt_=F32: p.sb([128, L], dt_, n)
    sinT, cosT, arg, rB = big("sinT"), big("cosT"), big("arg"), big("rB")
    argi = p.sb([128, L], mybir.dt.int32, "argi")
    bur, bui, wr, wi, ta, tb = big("bur"), big("bui"), big("wr"), big("wi"), big("ta"), big("tb")
    xs = [[big(f"x{ri}_{j4}", BF16) for j4 in range(4)] for ri in range(2)]
    yo = Rot([big(f"yo{i}") for i in range(2)])
    evac = Evac(p)
    for j in range(8):
        k, j4 = j // 4, j % 4
        p.op("dve", lambda e: e.tensor_scalar(out=arg[:], in0=iot[:], scalar1=th[:, j:j + 1], scalar2=None, op0=ALU.mult),
             reads=[iot, th], writes=[arg])
        wrap_sin(sinT, arg, ta, argi)
        wrap_sin(cosT, arg, ta, argi, shift=PI / 2)
        p.op("pool", lambda e: e.memset(rB[:], 1.0), writes=[rB])
        p.op("pool", lambda e: e.tensor_scalar(out=rB[:], in0=rB[:], scalar1=mag[:, j:j + 1], scalar2=None, op0=ALU.mult), reads=[rB, mag], writes=[rB])
        for n in range(S5N):
            sl = slice(n * S5C, (n + 1) * S5C)
            for ri, dst in enumerate((bur, bui)):
                ps = ps_rot.next()
                p.op("pe", lambda e: e.matmul(ps[:, 0:S5C], lhsT=WB[ri][j][:], rhs=ub[:, k, sl], start=True, stop=True),
                     reads=[WB[ri][j], ub], writes=[ps])
                evac(dst[:, sl], ps[:, 0:S5C], [ps], [dst], first=(n == 0))
        tt(ta, cosT, bur, ALU.mult)
        tt(tb, sinT, bui, ALU.mult, eng="pool")
        tt(wr, ta, tb, ALU.add)
        tt(ta, cosT, bui, ALU.mult)
        tt(tb, sinT, bur, ALU.mult, eng="pool")
        tt(wi, ta, tb, ALU.subtract)
        p.op("dve", lambda e: e.tensor_tensor_scan(out=bur[:], data0=rB[:], data1=wr[:], initial=0.0, op0=ALU.mult, op1=ALU.add),
             reads=[rB, wr], writes=[bur])
        p.op("dve", lambda e: e.tensor_tensor_scan(out=bui[:], data0=rB[:], data1=wi[:], initial=0.0, op0=ALU.mult, op1=ALU.add),
             reads=[rB, wi], writes=[bui])
        tt(ta, cosT, bur, ALU.mult)
        tt(tb, sinT, bui, ALU.mult, eng="pool")
        tt(xs[0][j4], ta, tb, ALU.subtract)
        tt(wr, cosT, bui, ALU.mult)
        tt(wi, sinT, bur, ALU.mult, eng="pool")
        tt(xs[1][j4], wr, wi, ALU.add)
        if j4 == 3:
            y = yo.next()
            for n in range(S5N):
                sl = slice(n * S5C, (n + 1) * S5C)
                ps = ps_rot.next()
                i = 0
                for jj in range(4):
                    for ri in range(2):
                        p.op("pe", lambda e: e.matmul(ps[:, 0:S5C], lhsT=WC[ri][4 * k + jj][:], rhs=xs[ri][jj][:, sl], start=(i == 0), stop=(i == 7)),
                             reads=[WC[ri][4 * k + jj], xs[ri][jj]], writes=[ps], accum=(i > 0))
                        i += 1
                p.op("dve", lambda e: e.scalar_tensor_tensor(out=y[:, sl], in0=u[:, k, sl], scalar=dsk[:, k:k + 1], in1=ps[:, 0:S5C],
                                                             op0=ALU.mult, op1=ALU.add), reads=[u, dsk, ps], writes=[y], accum=(n > 0))
            tt(ta, y, y, ALU.mult)
            tt(ta, ta, y, ALU.mult)
            p.op("dve", lambda e: e.scalar_tensor_tensor(out=ta[:], in0=ta[:], scalar=0.044715, in1=y[:], op0=ALU.mult, op1=ALU.add),
                 reads=[ta, y], writes=[ta])
            p.op("act", lambda e: e.activation(out=ta[:], in_=ta[:], func=AF.Sigmoid, scale=1.5957691216), reads=[ta], writes=[ta])
            tt(y, y, ta, ALU.mult)
            p.dma("sp", yT[k], y[:], reads=[y])
    return yo.tiles


def build_B():
    nc = bass.Bass("TRN2", target_bir_lowering=False)
    p = P(nc)
    outs = []
    for pre, fn in (("b1_", emit_B1), ("b2_", emit_B2), ("b3_", emit_B3)):
        p.prefix = pre
        m = p.mark()
        outs += fn(nc, p)
        p.release(m)
    p.finish(outs)
    return nc


NQT = 17
DH = 128
DV = 256


def build_B3():
    nc = bass.Bass("TRN2", target_bir_lowering=False)
    p = P(nc)
    outs = emit_B3(nc, p)
    p.finish(outs)
    return nc


def emit_B3(nc, p):
    qT_d = nc.dram_tensor("qT", [4, 128, L], F32, kind="ExternalInput").ap()
    kT_d = nc.dram_tensor("kT", [4, 128, L], F32, kind="ExternalInput").ap()
    v_d = nc.dram_tensor("v", [128, 2, NQT, DV], F32, kind="ExternalInput").ap()
    lam_d = nc.dram_tensor("lamv", [128, 4, DH], F32, kind="ExternalInput").ap()
    sw_d = nc.dram_tensor("subw", [128, DV], F32, kind="ExternalInput").ap()
    li_d = nc.dram_tensor("linit", [128, 2], F32, kind="ExternalInput").ap()
    oc = nc.dram_tensor("oc", [128, 2, NQT, DV], F32, kind="ExternalOutput").ap()

    qT = p.sb([128, 4, L], BF16, "qT_s")
    kT = p.sb([128, 4, L], BF16, "kT_s")
    V = p.sb([128, 2, NQT, DV + 1], BF16, "V")
    lamv = p.sb([128, 4, DH], F32, "lamv_s")
    subw = p.sb([128, DV], F32, "subw_s")
    linit = p.sb([128, 2], F32, "linit_s")
    epsc = p.sb([128, 1], F32, "epsc")
    p.op("pool", lambda e: e.memset(epsc[:], EPS), writes=[epsc])
    p.op("pool", lambda e: e.memset(V[:], 1.0), writes=[V])
    p.dma("pool", V[:, :, :, 0:DV], v_d[:, :, :, :], writes=[V])
    for i in range(4):
        p.dma("pool", qT[:, i, :], qT_d[i], writes=[qT], concurrent=True)
        p.dma("pool", kT[:, i, :], kT_d[i], writes=[kT], concurrent=True)
    p.dma("sp", lamv[:], lam_d[:, :, :], writes=[lamv])
    p.dma("sp", subw[:], sw_d[:, :], writes=[subw])
    p.dma("sp", linit[:], li_d[:, :], writes=[linit])
    lt = p.sb([128, 2, DH], F32, "lt")
    ls = p.sb([128, 2], F32, "ls")
    nlam = p.sb([128, 1], F32, "nlam")
    p.op("dve", lambda e: e.tensor_tensor(out=lt[:, 0, :], in0=lamv[:, 0, :], in1=lamv[:, 1, :], op=ALU.mult), reads=[lamv], writes=[lt])
    p.op("dve", lambda e: e.tensor_tensor(out=lt[:, 1, :], in0=lamv[:, 2, :], in1=lamv[:, 3, :], op=ALU.mult), reads=[lamv], writes=[lt], accum=True)
    p.op("dve", lambda e: e.tensor_reduce(out=ls[:], in_=lt[:], axis=AX.X, op=ALU.add), reads=[lt], writes=[ls])
    p.op("act", lambda e: e.activation(out=ls[:], in_=ls[:], func=AF.Exp), reads=[ls], writes=[ls])
    p.op("dve", lambda e: e.tensor_tensor(out=nlam[:], in0=ls[:, 1:2], in1=ls[:, 0:1], op=ALU.subtract), reads=[ls], writes=[nlam])
    p.op("dve", lambda e: e.tensor_tensor(out=nlam[:], in0=nlam[:], in1=linit[:, 0:1], op=ALU.subtract), reads=[nlam, linit], writes=[nlam])

    acc = [p.sb([128, NQT, DV], F32, f"acc{h}") for h in range(2)]
    psS = Rot([p.ps([128, 512], F32, f"psS{i}") for i in range(3)])
    psO = [p.ps([128, 512], F32, f"psO{i}") for i in range(4)]
    PT = Rot([p.sb([128, 512], BF16, f"PT{i}") for i in range(3)])
    rl = Rot([p.sb([128, 1], F32, f"rl{i}") for i in range(4)])
    on = Rot([p.sb([128, DV], F32, f"on{i}") for i in range(2)])
    scale = DH ** -0.5

    def tok0(i):
        return (0, 16) if i == 0 else (16 + 128 * (i - 1), 128)

    def finish_tile(h, s, i, po, nq):
        r = rl.next()
        p.op("dve", lambda e: e.reciprocal(out=r[0:nq, :], in_=po[0:nq, DV:DV + 1]), reads=[po], writes=[r])
        if s == 0:
            p.op("act", lambda e: e.activation(out=acc[h][0:nq, i, :], in_=po[0:nq, 0:DV], func=AF.Copy, scale=r[0:nq, 0:1]),
                 reads=[po, r], writes=[acc[h]], accum=True)
        else:
            o_ = on.next()
            p.op("act", lambda e: e.activation(out=o_[0:nq, :], in_=po[0:nq, 0:DV], func=AF.Copy, scale=r[0:nq, 0:1]),
                 reads=[po, r], writes=[o_])
            p.op("dve", lambda e: e.scalar_tensor_tensor(out=acc[h][0:nq, i, :], in0=o_[0:nq, :], scalar=nlam[0:nq, 0:1],
                                                         in1=acc[h][0:nq, i, :], op0=ALU.mult, op1=ALU.add),
                 reads=[o_, nlam, acc[h]], writes=[acc[h]])

    for h in range(2):
        p.op("pool", lambda e: e.memset(acc[h][:], 0.0), writes=[acc[h]])
    for h in range(2):
        for s in range(2):
            hs = 2 * h + s
            ps = psS.next()
            p.op("pe", lambda e: e.matmul(ps[0:16, 0:16], lhsT=kT[:, hs, 0:16], rhs=qT[:, hs, 0:16], start=True, stop=True),
                 reads=[kT, qT], writes=[ps])
            pt = PT.next()
            p.op("act", lambda e: e.activation(out=pt[0:16, 0:16], in_=ps[0:16, 0:16], func=AF.Exp, scale=scale), reads=[ps], writes=[pt])
            po = psO[0]
            p.op("pe", lambda e: e.matmul(po[0:16, 0:DV + 1], lhsT=pt[0:16, 0:16], rhs=V[0:16, h, 0, :], start=True, stop=True),
                 reads=[pt, V], writes=[po])
            finish_tile(h, s, 0, po, 16)
            for g in range(4):
                q0 = 16 + 512 * g
                tiles = [4 * g + 1 + a for a in range(4)]
                for j in range(0, 4 * g + 5):
                    k0, nk = tok0(j)
                    ps = psS.next()
                    p.op("pe", lambda e: e.matmul(ps[0:nk, 0:512], lhsT=kT[:, hs, k0:k0 + nk], rhs=qT[:, hs, q0:q0 + 512], start=True, stop=True),
                         reads=[kT, qT], writes=[ps])
                    pt = PT.next()
                    p.op("act", lambda e: e.activation(out=pt[0:nk, :], in_=ps[0:nk, 0:512], func=AF.Exp, scale=scale), reads=[ps], writes=[pt])
                    if j in tiles:
                        a = tiles.index(j)
                        p.op("pool", lambda e: e.memset(pt[64:128, 128 * a:128 * a + 64], 0.0), reads=[pt], writes=[pt])
                    for a, i in enumerate(tiles):
                        if i < j:
                            continue
                        p.op("pe", lambda e: e.matmul(psO[a][:, 0:DV + 1], lhsT=pt[0:nk, 128 * a:128 * (a + 1)], rhs=V[0:nk, h, j, :],
                                                      start=(j == 0), stop=(j == i)), reads=[pt, V], writes=[psO[a]], accum=(j > 0))
                        if j == i:
                            finish_tile(h, s, i, psO[a], 128)
    sq = p.sb([128, NQT, DV], F32, "sq")
    ms = p.sb([128, NQT], F32, "ms")
    for h in range(2):
        a = acc[h]
        p.op("pool", lambda e: e.tensor_tensor(out=sq[:], in0=a[:], in1=a[:], op=ALU.mult), reads=[a], writes=[sq])
        p.op("dve", lambda e: e.tensor_reduce(out=ms[:], in_=sq[:], axis=AX.X, op=ALU.add), reads=[sq], writes=[ms])
        p.op("act", lambda e: e.activation(out=ms[:], in_=ms[:], func=AF.Sqrt, bias=epsc[:, 0:1], scale=1.0 / DV), reads=[ms, epsc], writes=[ms])
        p.op("dve", lambda e: e.reciprocal(out=ms[:], in_=ms[:]), reads=[ms], writes=[ms])
        p.op("dve", lambda e: e.tensor_scalar(out=ms[:], in0=ms[:], scalar1=linit[:, 1:2], scalar2=None, op0=ALU.mult), reads=[ms, linit], writes=[ms])
        p.op("dve", lambda e: e.tensor_tensor(out=a[:], in0=a[:], in1=ms[:].unsqueeze(2).to_broadcast([128, NQT, DV]), op=ALU.mult),
             reads=[a, ms], writes=[a])
        p.op("pool", lambda e: e.tensor_tensor(out=a[:], in0=a[:], in1=subw[:].unsqueeze(1).to_broadcast([128, NQT, DV]), op=ALU.mult),
             reads=[a, subw], writes=[a])
        p.dma("sp", oc[:, h, :, :], a[:], reads=[a])
    return acc


NTP = 1152
NTT = NTP // 128
TCP = NTP // 3
NE = 64
NR = 72
BIGNEG = -1.0e30


def build_C1(gather=True):
    nc = bass.Bass("TRN2", target_bir_lowering=False)
    p = P(nc)
    zT = nc.dram_tensor("zT", [D, NT], F32, kind="ExternalInput").ap()
    oaT = nc.dram_tensor("oaT", [512, NT], F32, kind="ExternalInput").ap()
    ysT = nc.dram_tensor("ysT", [512, NT], F32, kind="ExternalInput").ap()
    ocT = nc.dram_tensor("ocT", [1024, NT], F32, kind="ExternalInput").ap()
    wglu_d = nc.dram_tensor("w_glu", [512, 1024], F32, kind="ExternalInput").ap()
    bglu_d = nc.dram_tensor("b_glu", [128, 8], F32, kind="ExternalInput").ap()
    wout_d = nc.dram_tensor("w_out", [D, D], F32, kind="ExternalInput").ap()
    ln2_d = nc.dram_tensor("ln2", [128, KC], F32, kind="ExternalInput").ap()
    wr_d = nc.dram_tensor("w_r", [D, NR], F32, kind="ExternalInput").ap()
    br_d = nc.dram_tensor("b_r", [128, NR], F32, kind="ExternalInput").ap()
    z1T = nc.dram_tensor("z1T", [D, NT], F32, kind="ExternalOutput").ap()
    if gather:
        xe_d = nc.dram_tensor("xeT", [NE, D, NSL], BF16, kind="ExternalOutput").ap()
    else:
        xtm_d = nc.dram_tensor("xtm", [128, NTT, D], BF16, kind="ExternalOutput").ap()
        rankp_d = nc.dram_tensor("rankp", [128, NTT, NE], F32, kind="ExternalOutput").ap()
    slotT_d = nc.dram_tensor("slotT", [NE, NTP], F32, kind="ExternalOutput").ap()
    gtT_d = nc.dram_tensor("gtT", [NE, NTP], F32, kind="ExternalOutput").ap()

    xtm = p.sb([128, NTT, D], BF16, "xtm_s")
    rankp = p.sb([128, NTT, NE], F32, "rankp_s")
    slotT = p.sb([NE, NTP], F32, "slotT_s")
    gtT = p.sb([NE, NTP], F32, "gtT_s")
    iota_s = p.sb([128, 128], F32, "iota_s")
    ps_rot = Rot([p.ps([128, 512], F32, f"ps{i}") for i in range(6)])
    psb_rot = Rot([p.ps([128, 512], BF16, f"psb{i}") for i in range(2)])
    mk = p.mark()
    z = p.sb([128, KC, NTP], F32, "z")
    mx = p.sb([128, KC, NTP], BF16, "mx")
    bglu = p.sb([128, 8], F32, "bglu_s")
    ln2 = p.sb([128, KC], F32, "ln2_s")
    wr = p.sb([128, KC, NR], F32, "wr_s")
    br = p.sb([128, NR], F32, "br_s")
    rstd = p.sb([128, NTP], F32, "rstd")
    ones_bf = p.sb([128, 128], BF16, "ones_bf")
    onesD = p.sb([128, 128], BF16, "onesD")
    epsc = p.sb([128, 1], F32, "epsc")
    identf = p.sb([128, 128], F32, "identf")
    identb = p.sb([128, 128], BF16, "identb")
    ustr = p.sb([128, 128], BF16, "ustr")
    vmask = p.sb([128, NTT], F32, "vmask")
    evac = Evac(p)
    ys = p.sb([128, 4, NTP], BF16, "ys")
    wg = p.sb([128, 4, 1024], BF16, "wg")
    sg_rot = Rot([p.sb([128, TCP], F32, f"sg{i}") for i in range(2)])
    wb_rot = Rot([p.sb([128, KC, 512], BF16, f"wb{i}") for i in range(1)])

    p.op("pool", lambda e: e.memset(z[:, :, NT:NTP], 0.0), writes=[z])
    p.op("pool", lambda e: e.memset(mx[:, :, NT:NTP], 0.0), writes=[mx])
    p.op("pool", lambda e: e.memset(ys[:, :, NT:NTP], 0.0), writes=[ys])
    zv = zT.rearrange("(c p) t -> p c t", p=128)
    for q in range(4):
        p.dma("sp", z[:, 4 * q:4 * q + 4, 0:NT], zv[:, 4 * q:4 * q + 4, :], writes=[z], concurrent=True)
    p.dma("pool", ys[:, :, 0:NT], ysT.rearrange("(c p) t -> p c t", p=128), writes=[ys])
    p.dma("pool", wg[:], wglu_d.rearrange("(c p) n -> p c n", p=128), writes=[wg])
    p.dma("pool", mx[:, 0:4, 0:NT], oaT.rearrange("(c p) t -> p c t", p=128), writes=[mx], concurrent=True)
    p.dma("pool", mx[:, 8:16, 0:NT], ocT.rearrange("(c p) t -> p c t", p=128), writes=[mx], concurrent=True)
    for t_, d_ in ((bglu, bglu_d), (ln2, ln2_d), (br, br_d)):
        p.dma("sp", t_[:], d_[:, :], writes=[t_])
    p.dma("sp", wr[:], wr_d.rearrange("(c p) n -> p c n", p=128), writes=[wr])
    p.op("pool", lambda e: e.memset(ones_bf[:], 1.0), writes=[ones_bf])
    p.op("pool", lambda e: e.memset(onesD[:], 1.0 / D), writes=[onesD])
    p.op("pool", lambda e: e.memset(epsc[:], EPS), writes=[epsc])
    p.op("pool", lambda e: e.memset(identf[:], 0.0), writes=[identf])
    p.op("pool", lambda e: e.affine_select(out=identf[:], in_=identf[:], pattern=[[-1, 128]], compare_op=ALU.not_equal,
                                           fill=1.0, base=0, channel_multiplier=1), reads=[identf], writes=[identf])
    p.op("pool", lambda e: e.tensor_copy(out=identb[:], in_=identf[:]), reads=[identf], writes=[identb])
    p.op("pool", lambda e: e.memset(ustr[:], 1.0), writes=[ustr])
    p.op("pool", lambda e: e.affine_select(out=ustr[:], in_=ustr[:], pattern=[[1, 128]], compare_op=ALU.is_ge, fill=0.0,
                                           base=-1, channel_multiplier=-1), reads=[ustr], writes=[ustr])
    p.op("pool", lambda e: e.memset(vmask[:], 1.0), writes=[vmask])
    p.op("pool", lambda e: e.affine_select(out=vmask[:], in_=vmask[:], pattern=[[-128, NTT]], compare_op=ALU.is_ge, fill=0.0,
                                           base=NT - 1, channel_multiplier=-1), reads=[vmask], writes=[vmask])

    for m in range(4):
        for n in range(3):
            sl = slice(n * TCP, (n + 1) * TCP)
            pv, pg = ps_rot.next(), ps_rot.next()
            for c in range(4):
                p.op("pe", lambda e: e.matmul(pv[:, 0:TCP], lhsT=wg[:, c, m * 128:(m + 1) * 128], rhs=ys[:, c, sl], start=(c == 0), stop=(c == 3)),
                     reads=[wg, ys], writes=[pv], accum=(c > 0))
            for c in range(4):
                p.op("pe", lambda e: e.matmul(pg[:, 0:TCP], lhsT=wg[:, c, 512 + m * 128:512 + (m + 1) * 128], rhs=ys[:, c, sl], start=(c == 0), stop=(c == 3)),
                     reads=[wg, ys], writes=[pg], accum=(c > 0))
            sg = sg_rot.next()
            p.op("act", lambda e: e.activation(out=sg[:], in_=pg[:, 0:TCP], func=AF.Sigmoid, bias=bglu[:, 4 + m:5 + m]), reads=[pg, bglu], writes=[sg])
            p.op("dve", lambda e: e.scalar_tensor_tensor(out=mx[:, 4 + m, sl], in0=pv[:, 0:TCP], scalar=bglu[:, m:m + 1], in1=sg[:],
                                                         op0=ALU.add, op1=ALU.mult), reads=[pv, bglu, sg], writes=[mx], accum=True)
    wo_v = wout_d.rearrange("(c p) n -> p c n", p=128)
    for g in range(4):
        wb = wb_rot.next()
        p.dma("pool", wb[:], wo_v[:, :, g * 512:(g + 1) * 512], writes=[wb])
        for j in range(4):
            ct = g * 4 + j
            for n in range(3):
                sl = slice(n * TCP, (n + 1) * TCP)
                ps = ps_rot.next()
                for c in range(KC):
                    p.op("pe", lambda e: e.matmul(ps[:, 0:TCP], lhsT=wb[:, c, j * 128:(j + 1) * 128], rhs=mx[:, c, sl], start=(c == 0), stop=(c == KC - 1)),
                         reads=[wb, mx], writes=[ps], accum=(c > 0))
                p.op("dve", lambda e: e.tensor_tensor(out=z[:, ct, sl], in0=z[:, ct, sl], in1=ps[:, 0:TCP], op=ALU.add), reads=[z, ps], writes=[z])
    z1v = z1T.rearrange("(c p) t -> p c t", p=128)
    for q in range(4):
        p.dma("sp", z1v[:, 4 * q:4 * q + 4, :], z[:, 4 * q:4 * q + 4, 0:NT], reads=[z])
    p.barrier()
    p.free([ys, wg] + sg_rot.tiles + wb_rot.tiles)
    hn2 = mx
    sq_rot = Rot([p.sb([128, NTP], BF16, f"sq{i}") for i in range(2)])
    emit_rmsnorm(p, z, ln2, hn2, onesD, epsc, sq_rot, ps_rot, rstd, nt=NTP, tch=TCP)
    for i in range(NTT):
        for cq in range(4):
            pb = psb_rot.next()
            for cc in range(4):
                c = cq * 4 + cc
                p.op("pe", lambda e: e.transpose(out=pb[:, cc * 128:(cc + 1) * 128], in_=hn2[:, c, i * 128:(i + 1) * 128], identity=identb[:]),
                     reads=[hn2, identb], writes=[pb], accum=(cc > 0))
            evac(xtm[:, i, cq * 512:(cq + 1) * 512], pb[:, 0:512], [pb], [xtm], first=False)
    if not gather:
        p.dma("sp", xtm_d[:, :, :], xtm[:], reads=[xtm])
    p.op("dve", lambda e: e.tensor_tensor(out=wr[:], in0=wr[:], in1=ln2[:].unsqueeze(2).to_broadcast([128, KC, NR]), op=ALU.mult),
         reads=[wr, ln2], writes=[wr])
    rstd_tm = p.sb([128, NTT], F32, "rstd_tm")
    lg = p.sb([128, NTT, NR], F32, "lg")
    for i in range(NTT):
        pt = ps_rot.next()
        p.op("pe", lambda e: e.transpose(out=pt[:, 0:128], in_=rstd[:, i * 128:(i + 1) * 128], identity=identf[:]), reads=[rstd, identf], writes=[pt])
        p.op("act", lambda e: e.activation(out=rstd_tm[:, i:i + 1], in_=pt[:, 0:1], func=AF.Copy), reads=[pt], writes=[rstd_tm], accum=True)
        ps = ps_rot.next()
        for c in range(KC):
            p.op("pe", lambda e: e.matmul(ps[:, 0:NR], lhsT=z[:, c, i * 128:(i + 1) * 128], rhs=wr[:, c, :], start=(c == 0), stop=(c == KC - 1)),
                 reads=[z, wr], writes=[ps], accum=(c > 0))
        p.op("dve", lambda e: e.scalar_tensor_tensor(out=lg[:, i, :], in0=ps[:, 0:NR], scalar=rstd_tm[:, i:i + 1], in1=br[:],
                                                     op0=ALU.mult, op1=ALU.add), reads=[ps, rstd_tm, br], writes=[lg], accum=True)

    def sbf(name, shape):
        return p.sb(list(shape), F32, name)

    def tt(out_ap, in0_ap, in1_ap, op, reads, writes, eng="dve"):
        p.op(eng, lambda e: e.tensor_tensor(out=out_ap, in0=in0_ap, in1=in1_ap, op=op), reads=reads, writes=writes)

    def red(out, in_ap, op, reads):
        p.op("dve", lambda e: e.tensor_reduce(out=out[:], in_=in_ap, axis=AX.X, op=op), reads=reads, writes=[out])

    gl = lg[:, :, 0:8]
    gmax, gsum, pg_ = sbf("gmax", (128, NTT)), sbf("gsum", (128, NTT)), sbf("pgrp", (128, NTT))
    ge, oh = sbf("ge", (128, NTT, 8)), sbf("oh", (128, NTT, 8))
    red(gmax, gl, ALU.max, [lg])
    b8 = lambda t_: t_[:].unsqueeze(2).to_broadcast([128, NTT, 8])
    tt(ge[:], gl, b8(gmax), ALU.subtract, [lg, gmax], [ge])
    tt(oh[:], gl, b8(gmax), ALU.is_equal, [lg, gmax], [oh])
    p.op("act", lambda e: e.activation(out=ge[:], in_=ge[:], func=AF.Exp), reads=[ge], writes=[ge])
    red(gsum, ge[:], ALU.add, [ge])
    p.op("dve", lambda e: e.reciprocal(out=pg_[:], in_=gsum[:]), reads=[gsum], writes=[pg_])
    el4 = sbf("el4", (128, NTT, 8, 8))
    esel = sbf("esel", (128, NTT, 8))
    lg4 = lg[:, :, 8:NR].rearrange("p t (g j) -> p t g j", j=8)
    tt(el4[:], lg4, oh[:].unsqueeze(3).to_broadcast([128, NTT, 8, 8]), ALU.mult, [lg, oh], [el4])
    red(esel, el4[:].rearrange("p t g j -> p t j g"), ALU.add, [el4])
    m1, m2 = sbf("m1", (128, NTT)), sbf("m2", (128, NTT))
    k1, k2, e2 = sbf("k1", (128, NTT, 8)), sbf("k2", (128, NTT, 8)), sbf("e2", (128, NTT, 8))
    red(m1, esel[:], ALU.max, [esel])
    tt(k1[:], esel[:], b8(m1), ALU.is_equal, [esel, m1], [k1])
    p.op("dve", lambda e: e.scalar_tensor_tensor(out=e2[:], in0=k1[:], scalar=BIGNEG, in1=esel[:], op0=ALU.mult, op1=ALU.add),
         reads=[k1, esel], writes=[e2])
    red(m2, e2[:], ALU.max, [e2])
    tt(k2[:], e2[:], b8(m2), ALU.is_equal, [e2, m2], [k2])
    dd, g1, g2 = sbf("dd", (128, NTT)), sbf("g1", (128, NTT)), sbf("g2", (128, NTT))
    tt(dd[:], m2[:], m1[:], ALU.subtract, [m2, m1], [dd])
    p.op("act", lambda e: e.activation(out=dd[:], in_=dd[:], func=AF.Exp), reads=[dd], writes=[dd])
    p.op("dve", lambda e: e.tensor_scalar(out=dd[:], in0=dd[:], scalar1=1.0, scalar2=None, op0=ALU.add), reads=[dd], writes=[dd])
    p.op("dve", lambda e: e.reciprocal(out=dd[:], in_=dd[:]), reads=[dd], writes=[dd])
    tt(g1[:], pg_[:], dd[:], ALU.mult, [pg_, dd], [g1])
    tt(g2[:], pg_[:], g1[:], ALU.subtract, [pg_, g1], [g2])
    tt(oh[:], oh[:], b8(vmask), ALU.mult, [oh, vmask], [oh])
    gj, aj = sbf("gj", (128, NTT, 8)), sbf("aj", (128, NTT, 8))
    tt(gj[:], k1[:], b8(g1), ALU.mult, [k1, g1], [gj])
    tt(e2[:], k2[:], b8(g2), ALU.mult, [k2, g2], [e2])
    tt(gj[:], gj[:], e2[:], ALU.add, [gj, e2], [gj])
    tt(aj[:], k1[:], k2[:], ALU.add, [k1, k2], [aj])
    Gt, Af = sbf("Gt", (128, NTT, 8, 8)), sbf("Af", (128, NTT, 8, 8))
    Ab = p.sb([128, NTT, NE], BF16, "Ab")
    ohb = oh[:].unsqueeze(3).to_broadcast([128, NTT, 8, 8])
    tt(Gt[:], ohb, gj[:].unsqueeze(2).to_broadcast([128, NTT, 8, 8]), ALU.mult, [oh, gj], [Gt])
    tt(Af[:], ohb, aj[:].unsqueeze(2).to_broadcast([128, NTT, 8, 8]), ALU.mult, [oh, aj], [Af])
    Af2 = Af[:].rearrange("p t g j -> p t (g j)")
    Gt2 = Gt[:].rearrange("p t g j -> p t (g j)")
    p.op("dve", lambda e: e.tensor_copy(out=Ab[:], in_=Af2), reads=[Af], writes=[Ab])
    AT = sbf("AT_s", (NE, NTP))
    for i in range(NTT):
        ps = ps_rot.next()
        for i2 in range(i + 1):
            p.op("pe", lambda e: e.matmul(ps[:, 0:NE], lhsT=(ustr[:] if i2 == i else ones_bf[:]), rhs=Ab[:, i2, :], start=(i2 == 0), stop=(i2 == i)),
                 reads=[ustr, ones_bf, Ab], writes=[ps], accum=(i2 > 0))
        p.op("dve", lambda e: e.scalar_tensor_tensor(out=rankp[:, i, :], in0=ps[:, 0:NE], scalar=1.0, in1=Af2[:, i, :], op0=ALU.add, op1=ALU.mult),
             reads=[ps, Af], writes=[rankp], accum=True)
        ps2 = ps_rot.next()
        for i2 in range(i + 1):
            p.op("pe", lambda e: e.matmul(ps2[0:NE, 0:128], lhsT=Ab[:, i2, :], rhs=(ustr[:] if i2 == i else ones_bf[:]), start=(i2 == 0), stop=(i2 == i)),
                 reads=[ustr, ones_bf, Ab], writes=[ps2], accum=(i2 > 0))
        p.op("act", lambda e: e.activation(out=slotT[:, i * 128:(i + 1) * 128], in_=ps2[0:NE, 0:128], func=AF.Copy), reads=[ps2], writes=[slotT], accum=True)
        ps3 = ps_rot.next()
        p.op("pe", lambda e: e.transpose(out=ps3[0:NE, 0:128], in_=Af2[:, i, :], identity=identf[:]), reads=[Af, identf], writes=[ps3])
        p.op("act", lambda e: e.activation(out=AT[:, i * 128:(i + 1) * 128], in_=ps3[0:NE, 0:128], func=AF.Copy), reads=[ps3], writes=[AT], accum=True)
        ps4 = ps_rot.next()
        p.op("pe", lambda e: e.transpose(out=ps4[0:NE, 0:128], in_=Gt2[:, i, :], identity=identf[:]), reads=[Gt, identf], writes=[ps4])
        p.op("act", lambda e: e.activation(out=gtT[:, i * 128:(i + 1) * 128], in_=ps4[0:NE, 0:128], func=AF.Copy), reads=[ps4], writes=[gtT], accum=True)
    p.op("dve", lambda e: e.tensor_scalar(out=rankp[:], in0=rankp[:], scalar1=-1.0, scalar2=None, op0=ALU.add), reads=[rankp], writes=[rankp])
    p.op("dve", lambda e: e.scalar_tensor_tensor(out=slotT[:], in0=slotT[:], scalar=1.0, in1=AT[:], op0=ALU.add, op1=ALU.mult),
         reads=[slotT, AT], writes=[slotT])
    p.op("dve", lambda e: e.tensor_scalar(out=slotT[:], in0=slotT[:], scalar1=-1.0, scalar2=None, op0=ALU.add), reads=[slotT], writes=[slotT])
    p.dma("sp", slotT_d[:, :], slotT[:], reads=[slotT])
    p.dma("sp", gtT_d[:, :], gtT[:], reads=[gtT])
    if not gather:
        p.dma("sp", rankp_d[:, :, :], rankp[:], reads=[rankp])
        p.finish([z, xtm, rankp, slotT, gtT])
        return nc
    p.release(mk)
    p.op("pool", lambda e: e.iota(iota_s[:], pattern=[[1, 128]], base=0, channel_multiplier=0, allow_small_or_imprecise_dtypes=True), writes=[iota_s])
    S_rot = Rot([p.sb([128, NTT, 4, 128], BF16, f"S{i}") for i in range(2)])
    XeT_rot = Rot([p.sb([128, KC, 512], BF16, f"XeT{i}") for i in range(2)])
    for qd in range(NE // 4):
        e0 = 4 * qd
        S = S_rot.next()
        p.op("dve", lambda e: e.tensor_tensor(out=S[:], in0=iota_s[:].unsqueeze(1).unsqueeze(1).to_broadcast([128, NTT, 4, 128]),
                                              in1=rankp[:, :, e0:e0 + 4].unsqueeze(3).to_broadcast([128, NTT, 4, 128]), op=ALU.is_equal),
             reads=[iota_s, rankp], writes=[S])
        S2 = S[:].rearrange("p t e s -> p t (e s)")
        XeT = XeT_rot.next()
        for c in range(KC):
            ps = ps_rot.next()
            for i in range(NTT):
                p.op("pe", lambda e: e.matmul(ps[:, 0:512], lhsT=xtm[:, i, c * 128:(c + 1) * 128], rhs=S2[:, i, :], start=(i == 0), stop=(i == NTT - 1)),
                     reads=[xtm, S], writes=[ps], accum=(i > 0))
            evac(XeT[:, c, :], ps[:, 0:512], [ps], [XeT], first=(c == 0))
        for ee in range(4):
            p.dma("sp", xe_d[e0 + ee].rearrange("(c p) s -> p c s", p=128), XeT[:, :, ee * 128:(ee + 1) * 128], reads=[XeT])
    p.finish([z, slotT, gtT] + XeT_rot.tiles)
    return nc


DF = 512
SC = NT // 3


NSL = 128
NSRC = 8
ESL = NSRC * NSL
EPC = NE // 8


def build_C2a():
    nc = bass.Bass("TRN2", target_bir_lowering=False)
    p = P(nc)
    xtm_d = nc.dram_tensor("xtm", [128, NTT, D], BF16, kind="ExternalInput").ap()
    rankp_d = nc.dram_tensor("rankp", [128, NTT, NE], F32, kind="ExternalInput").ap()
    xe_d = nc.dram_tensor("xeT", [NE, D, NSL], BF16, kind="ExternalOutput").ap()
    X = p.sb([128, NTT, D], BF16, "X")
    rankp = p.sb([128, NTT, NE], F32, "rankp_s")
    iota_s = p.sb([128, 128], F32, "iota_s")
    p.dma("sp", X[:], xtm_d[:, :, :], writes=[X])
    p.dma("sp", rankp[:], rankp_d[:, :, :], writes=[rankp])
    p.op("pool", lambda e: e.iota(iota_s[:], pattern=[[1, 128]], base=0, channel_multiplier=0, allow_small_or_imprecise_dtypes=True), writes=[iota_s])
    S_rot = Rot([p.sb([128, NTT, 4, 128], BF16, f"S{i}") for i in range(2)])
    XeT_rot = Rot([p.sb([128, KC, 512], BF16, f"XeT{i}") for i in range(2)])
    ps_rot = Rot([p.ps([128, 512], F32, f"ps{i}") for i in range(8)])
    evac = Evac(p)
    for qd in range(NE // 4):
        e0 = 4 * qd
        S = S_rot.next()
        p.op("dve", lambda e: e.tensor_tensor(out=S[:], in0=iota_s[:].unsqueeze(1).unsqueeze(1).to_broadcast([128, NTT, 4, 128]),
                                              in1=rankp[:, :, e0:e0 + 4].unsqueeze(3).to_broadcast([128, NTT, 4, 128]), op=ALU.is_equal),
             reads=[iota_s, rankp], writes=[S])
        S2 = S[:].rearrange("p t e s -> p t (e s)")
        XeT = XeT_rot.next()
        for c in range(KC):
            ps = ps_rot.next()
            for i in range(NTT):
                p.op("pe", lambda e: e.matmul(ps[:, 0:512], lhsT=X[:, i, c * 128:(c + 1) * 128], rhs=S2[:, i, :], start=(i == 0), stop=(i == NTT - 1)),
                     reads=[X, S], writes=[ps], accum=(i > 0))
            evac(XeT[:, c, :], ps[:, 0:512], [ps], [XeT], first=(c == 0))
        for ee in range(4):
            p.dma("sp", xe_d[e0 + ee].rearrange("(c p) s -> p c s", p=128), XeT[:, :, ee * 128:(ee + 1) * 128], reads=[XeT])
    p.finish(XeT_rot.tiles)
    return nc


def build_C2b():
    nc = bass.Bass("TRN2", target_bir_lowering=False)
    p = P(nc)
    xe_d = nc.dram_tensor("xe", [EPC, D, ESL], BF16, kind="ExternalInput").ap()
    w1_d = nc.dram_tensor("w1", [EPC, D, DF], F32, kind="ExternalInput").ap()
    w3_d = nc.dram_tensor("w3", [EPC, D, DF], F32, kind="ExternalInput").ap()
    w2_d = nc.dram_tensor("w2", [EPC, DF, D], F32, kind="ExternalInput").ap()
    ye_d = nc.dram_tensor("ye", [EPC, ESL, D], BF16, kind="ExternalOutput").ap()
    W1 = Rot([p.sb([128, KC, DF], BF16, f"W1_{i}") for i in range(2)])
    W3 = Rot([p.sb([128, KC, DF], BF16, f"W3_{i}") for i in range(2)])
    W2 = Rot([p.sb([128, 4, D], BF16, f"W2_{i}") for i in range(2)])
    Xe = Rot([p.sb([128, KC, ESL], BF16, f"Xe{i}") for i in range(2)])
    Hg = Rot([p.sb([128, 4, 512], BF16, f"Hg{i}") for i in range(2)])
    h1 = Rot([p.sb([128, 512], F32, f"h1_{i}") for i in range(2)])
    yo = Rot([p.sb([128, D], BF16, f"yo{i}") for i in range(3)])
    ps_rot = Rot([p.ps([128, 512], F32, f"ps{i}") for i in range(8)])
    evac = Evac(p)
    for ex in range(EPC):
        w1, w3, w2, xe = W1.next(), W3.next(), W2.next(), Xe.next()
        p.dma("sp", xe[:], xe_d[ex].rearrange("(c p) s -> p c s", p=128), writes=[xe])
        p.dma("pool", w1[:], w1_d[ex].rearrange("(c p) f -> p c f", p=128), writes=[w1])
        p.dma("pool", w3[:], w3_d[ex].rearrange("(c p) f -> p c f", p=128), writes=[w3])
        p.dma("pool", w2[:], w2_d[ex].rearrange("(m p) d -> p m d", p=128), writes=[w2])
        for half in range(ESL // 512):
            hs = slice(half * 512, (half + 1) * 512)
            hg = Hg.next()
            for m in range(4):
                p1, p3 = ps_rot.next(), ps_rot.next()
                for c in range(KC):
                    p.op("pe", lambda e: e.matmul(p1[:, 0:512], lhsT=w1[:, c, m * 128:(m + 1) * 128], rhs=xe[:, c, hs], start=(c == 0), stop=(c == KC - 1)),
                         reads=[w1, xe], writes=[p1], accum=(c > 0))
                for c in range(KC):
                    p.op("pe", lambda e: e.matmul(p3[:, 0:512], lhsT=w3[:, c, m * 128:(m + 1) * 128], rhs=xe[:, c, hs], start=(c == 0), stop=(c == KC - 1)),
                         reads=[w3, xe], writes=[p3], accum=(c > 0))
                h = h1.next()
                p.op("act", lambda e: e.activation(out=h[:], in_=p1[:, 0:512], func=AF.Silu), reads=[p1], writes=[h])
                p.op("dve", lambda e: e.tensor_tensor(out=hg[:, m, :], in0=h[:], in1=p3[:, 0:512], op=ALU.mult), reads=[h, p3], writes=[hg], accum=(m > 0))
            for st in range(4):
                y = yo.next()
                for n in range(4):
                    ps = ps_rot.next()
                    for m in range(4):
                        p.op("pe", lambda e: e.matmul(ps[:, 0:512], lhsT=hg[:, m, st * 128:(st + 1) * 128], rhs=w2[:, m, n * 512:(n + 1) * 512],
                                                      start=(m == 0), stop=(m == 3)), reads=[hg, w2], writes=[ps], accum=(m > 0))
                    evac(y[:, n * 512:(n + 1) * 512], ps[:, 0:512], [ps], [y], first=(n == 0))
                r0 = half * 512 + st * 128
                p.dma("sp", ye_d[ex, r0:r0 + 128, :], y[:], reads=[y])
    p.finish(yo.tiles)
    return nc


def build_C2c(last):
    nc = bass.Bass("TRN2", target_bir_lowering=False)
    p = P(nc)
    zT = nc.dram_tensor("zT", [D, NT], F32, kind="ExternalInput").ap()
    slotT_d = nc.dram_tensor("slotT", [NE, NTP], F32, kind="ExternalInput").ap()
    gtT_d = nc.dram_tensor("gtT", [NE, NTP], F32, kind="ExternalInput").ap()
    ye_d = nc.dram_tensor("ye", [NE, NSL, D], BF16, kind="ExternalInput").ap()
    fw_d = nc.dram_tensor("fnw", [128, KC], F32, kind="ExternalInput").ap()
    outT = nc.dram_tensor("outT", [D, NT], F32, kind="ExternalOutput").ap()
    if not last:
        lnw_d = nc.dram_tensor("lnw", [128, KC], F32, kind="ExternalInput").ap()
        w_in = nc.dram_tensor("w_in", [D, INW], F32, kind="ExternalInput").ap()
        projT = nc.dram_tensor("projT", [INW, NT], F32, kind="ExternalOutput").ap()
    z = p.sb([128, KC, NT], F32, "z")
    slotT = p.sb([NE, NTP], BF16, "slotT_s")
    gtT = p.sb([NE, NTP], BF16, "gtT_s")
    fnw = p.sb([128, KC], F32, "fnw_s")
    identb = p.sb([128, 128], BF16, "identb")
    identf = p.sb([128, 128], F32, "identf")
    iota_p = p.sb([128, 1], F32, "iota_p")
    zv = zT.rearrange("(c p) t -> p c t", p=128)
    for q in range(4):
        p.dma("sp", z[:, 4 * q:4 * q + 4, :], zv[:, 4 * q:4 * q + 4, :], writes=[z], concurrent=True)
    p.dma("sp", fnw[:], fw_d[:, :], writes=[fnw])
    p.dma("pool", slotT[:], slotT_d[:, :], writes=[slotT])
    p.dma("pool", gtT[:], gtT_d[:, :], writes=[gtT])
    p.op("pool", lambda e: e.memset(identf[:], 0.0), writes=[identf])
    p.op("pool", lambda e: e.affine_select(out=identf[:], in_=identf[:], pattern=[[-1, 128]], compare_op=ALU.not_equal,
                                           fill=1.0, base=0, channel_multiplier=1), reads=[identf], writes=[identf])
    p.op("pool", lambda e: e.tensor_copy(out=identb[:], in_=identf[:]), reads=[identf], writes=[identb])
    p.op("pool", lambda e: e.iota(iota_p[:], pattern=[[0, 1]], base=0, channel_multiplier=1, allow_small_or_imprecise_dtypes=True), writes=[iota_p])
    ps_rot = Rot([p.ps([128, 512], F32, f"ps{i}") for i in range(8)])
    mk = p.mark()
    NG = 4
    Ye_rot = Rot([p.sb([128, NG, D], BF16, f"Ye{i}") for i in range(2)])
    SgT_rot = Rot([p.sb([128, NG, NT], BF16, f"SgT{i}") for i in range(2)])
    sel_rot = Rot([p.sb([NE, 128], BF16, f"sel{i}") for i in range(2)])
    gB_rot = Rot([p.sb([128, SC], F32, f"gB{i}") for i in range(2)])
    for qd in range(NE // NG):
        e0 = NG * qd
        Ye, SgT = Ye_rot.next(), SgT_rot.next()
        p.dma("sp", Ye[:], ye_d[e0:e0 + NG].rearrange("e s d -> s e d"), writes=[Ye])
        for ee in range(NG):
            ex = e0 + ee
            sel = sel_rot.next()
            p.op("dve", lambda e: e.tensor_copy(out=sel[:], in_=identb[0:NE, ex:ex + 1].to_broadcast([NE, 128])), reads=[identb], writes=[sel])
            for n in range(3):
                sl = slice(n * SC, (n + 1) * SC)
                pa, pg = ps_rot.next(), ps_rot.next()
                p.op("pe", lambda e: e.matmul(pa[:, 0:SC], lhsT=sel[:], rhs=slotT[:, sl], start=True, stop=True), reads=[sel, slotT], writes=[pa])
                p.op("pe", lambda e: e.matmul(pg[:, 0:SC], lhsT=sel[:], rhs=gtT[:, sl], start=True, stop=True), reads=[sel, gtT], writes=[pg])
                gB = gB_rot.next()
                p.op("act", lambda e: e.activation(out=gB[:], in_=pg[:, 0:SC], func=AF.Copy), reads=[pg], writes=[gB])
                p.op("dve", lambda e: e.scalar_tensor_tensor(out=SgT[:, ee, sl], in0=pa[:, 0:SC], scalar=iota_p[:, 0:1], in1=gB[:],
                                                             op0=ALU.is_equal, op1=ALU.mult), reads=[pa, iota_p, gB], writes=[SgT], accum=(ee + n > 0))
        for c in range(KC):
            for n in range(3):
                sl = slice(n * SC, (n + 1) * SC)
                ps = ps_rot.next()
                for ee in range(NG):
                    p.op("pe", lambda e: e.matmul(ps[:, 0:SC], lhsT=Ye[:, ee, c * 128:(c + 1) * 128], rhs=SgT[:, ee, sl], start=(ee == 0), stop=(ee == NG - 1)),
                         reads=[Ye, SgT], writes=[ps], accum=(ee > 0))
                p.op("dve", lambda e: e.tensor_tensor(out=z[:, c, sl], in0=z[:, c, sl], in1=ps[:, 0:SC], op=ALU.add), reads=[z, ps], writes=[z])
    ov = outT.rearrange("(c p) t -> p c t", p=128)
    p.release(mk)
    rstd = p.sb([128, NT], F32, "rstd")
    onesD = p.sb([128, 128], BF16, "onesD")
    epsc = p.sb([128, 1], F32, "epsc")
    p.op("pool", lambda e: e.memset(onesD[:], 1.0 / D), writes=[onesD])
    p.op("pool", lambda e: e.memset(epsc[:], EPS), writes=[epsc])
    sq_rot = Rot([p.sb([128, NT], BF16, f"sq{i}") for i in range(2)])
    if last:
        emit_rmsnorm(p, z, fnw, None, onesD, epsc, sq_rot, ps_rot, rstd, hn32=z)
        for q in range(4):
            p.dma("sp", ov[:, 4 * q:4 * q + 4, :], z[:, 4 * q:4 * q + 4, :], reads=[z])
        p.finish([z])
        return nc
    for q in range(4):
        p.dma("sp", ov[:, 4 * q:4 * q + 4, :], z[:, 4 * q:4 * q + 4, :], reads=[z])
    lnw = p.sb([128, KC], F32, "lnw_s")
    p.dma("sp", lnw[:], lnw_d[:, :], writes=[lnw])
    hn = p.sb([128, KC, NT], BF16, "hn")
    wb_rot = Rot([p.sb([128, KC, 512], BF16, f"wb{i}") for i in range(2)])
    st_rot = Rot([p.sb([128, NT], F32, f"st{i}") for i in range(3)])
    emit_rmsnorm(p, z, lnw, hn, onesD, epsc, sq_rot, ps_rot, rstd)
    emit_inproj(p, hn, w_in, projT, wb_rot, ps_rot, st_rot, Evac(p))
    p.finish([z] + st_rot.tiles)
    return nc


_PROGS = {}


def _prog(name, fn, *args):
    key = (name,) + args
    if key not in _PROGS:
        _PROGS[key] = fn(*args)
    return _PROGS[key]


def _run(nc, in_maps):
    res = run_bass_kernel_spmd(nc, in_maps, core_ids=list(range(8)))
    return res.results


def _colT(v, n):
    return np.ascontiguousarray(np.asarray(v, np.float32).reshape(n, 128).T)


def _rep(v, n=128):
    v = np.asarray(v, np.float32)
    return np.ascontiguousarray(np.broadcast_to(v[None], (n,) + v.shape))


def _pairs(a):
    a = np.asarray(a, np.float32)
    sh = a.shape[2:]
    nd = len(sh)
    return np.ascontiguousarray(a.reshape(8, 2, 64, *sh).transpose(1, 2, 0, *range(3, 3 + nd)).reshape(128, 8, *sh))


def _tok_tiles(a):
    o = np.zeros((NQT, 128, a.shape[-1]), np.float32)
    o[0, :NMETA] = a[:NMETA]
    o[1:] = a[NMETA:].reshape(16, 128, -1)
    return o.transpose(1, 0, 2)


def _untok_tiles(o):
    o = o.transpose(1, 0, 2)
    return np.concatenate([o[0, :NMETA], o[1:].reshape(SEQ, -1)], axis=0)


def kernel(x, meta_tokens, ln1_w, w_in, hgrn_lower_bounds, hgrn_norm_w, s5_a_re, s5_a_im,
           s5_b_re, s5_b_im, s5_c_re, s5_c_im, s5_d, s5_log_dt, s5_w_glu, s5_b_glu,
           diff_lambda_q1, diff_lambda_k1, diff_lambda_q2, diff_lambda_k2, diff_subln_w,
           w_out, ln2_w, router_group_w, router_group_b, router_expert_w, router_expert_b,
           expert_w1, expert_w3, expert_w2, final_norm_w):
    import math
    f32 = lambda a: np.asarray(a, np.float32)
    x, meta_tokens = f32(x), f32(meta_tokens)
    z0 = np.concatenate([np.broadcast_to(meta_tokens[None], (NB, NMETA, D)), x], axis=1)
    zTs = [np.ascontiguousarray(z0[c // 2, (c % 2) * NT:(c % 2 + 1) * NT].T) for c in range(8)]
    depth = f32(ln1_w).shape[0]
    hlb = f32(hgrn_lower_bounds)
    fnw = _colT(final_norm_w, KC)
    for layer in range(depth):
        if layer == 0:
            r = _run(_prog("A", build_A), [{"zT": zTs[c], "lnw": _colT(f32(ln1_w)[0], KC), "w_in": f32(w_in)[0]} for c in range(8)])
            projTs = [r[c]["projT"] for c in range(8)]
            del r
        proj = np.stack([np.concatenate([projTs[2 * b].T, projTs[2 * b + 1].T], axis=0) for b in range(NB)])
        del projTs
        lsel = np.full((128, 1), 1.0 if layer > 0 else 0.0, np.float32)
        nw = _rep(f32(hgrn_norm_w)[layer])
        li = 0.8 - 0.6 * math.exp(-0.3 * layer)
        linit = _rep(np.array([li, 1.0 - li], np.float32))
        lamv = _rep(np.stack([f32(diff_lambda_q1)[layer], f32(diff_lambda_k1)[layer], f32(diff_lambda_q2)[layer], f32(diff_lambda_k2)[layer]]))
        subw = _rep(f32(diff_subln_w)[layer])
        ins = []
        for c in range(8):
            b, hh = c // 2, c % 2
            hs = [2 * hh, 2 * hh + 1]

            def padT(col0):
                o = np.zeros((2, 128, LP), np.float32)
                for i, h in enumerate(hs):
                    o[i, :, :L] = proj[b, :, col0 + h * 128:col0 + (h + 1) * 128].T
                return o

            def padded(col0):
                o = np.zeros((2, LP, 128), np.float32)
                for i, h in enumerate(hs):
                    o[i, :L] = proj[b, :, col0 + h * 128:col0 + (h + 1) * 128]
                return o

            hv = np.ascontiguousarray(padded(1024).reshape(2, NCK, HC, 128).transpose(0, 2, 1, 3))
            hg = np.ascontiguousarray(padded(1536).reshape(2, LP // 128, 128, 128).transpose(0, 2, 1, 3))
            lbraw = np.ascontiguousarray(np.stack([hlb[0:2, h * 128:(h + 1) * 128].T for h in hs], axis=1))
            d_ = {"hqT": padT(0), "hfT": padT(512), "hv": hv, "hg": hg, "lbraw": lbraw, "lsel": lsel, "nw": nw}
            gs = slice(16 * hh, 16 * hh + 16)
            u = proj[b, :, 2048 + 256 * hh:2048 + 256 * (hh + 1)]
            d_.update({"uT": np.ascontiguousarray(u.T.reshape(2, 128, L)),
                       "a_re": _pairs(f32(s5_a_re)[layer, gs]), "a_im": _pairs(f32(s5_a_im)[layer, gs]),
                       "ldt": _pairs(np.broadcast_to(f32(s5_log_dt)[layer, gs][:, None], (16, 64))),
                       "b_re": _pairs(f32(s5_b_re)[layer, gs]), "b_im": _pairs(f32(s5_b_im)[layer, gs]),
                       "cT_re": _pairs(f32(s5_c_re)[layer, gs].transpose(0, 2, 1)), "cT_im": _pairs(f32(s5_c_im)[layer, gs].transpose(0, 2, 1)),
                       "dsk": np.ascontiguousarray(f32(s5_d)[layer, 256 * hh:256 * (hh + 1)].reshape(2, 128).T)})
            dq = proj[b, :, 2560:3584].reshape(L, 4, 2, 128)
            dk = proj[b, :, 3584:4608].reshape(L, 4, 2, 128)
            dv = proj[b, :, 4608:5632].reshape(L, 4, 256)
            d_.update({"qT": np.ascontiguousarray(dq[:, 2 * hh:2 * hh + 2].reshape(L, 4, 128).transpose(1, 2, 0)),
                       "kT": np.ascontiguousarray(dk[:, 2 * hh:2 * hh + 2].reshape(L, 4, 128).transpose(1, 2, 0)),
                       "v": np.ascontiguousarray(np.stack([_tok_tiles(dv[:, 2 * hh + i]) for i in range(2)], axis=1)),
                       "lamv": lamv, "subw": subw, "linit": linit})
            ins.append(d_)
        r = _run(_prog("B", build_B), ins)
        o_a = np.zeros((NB, L, 512), np.float32)
        ys = np.zeros((NB, L, 512), np.float32)
        o_c = np.zeros((NB, L, 1024), np.float32)
        for c in range(8):
            b, hh = c // 2, c % 2
            for i in range(2):
                h = 2 * hh + i
                o_a[b, :, h * 128:(h + 1) * 128] = r[c]["oa"][i].transpose(1, 0, 2).reshape(LP, 128)[:L]
                o_c[b, :, h * 256:(h + 1) * 256] = _untok_tiles(r[c]["oc"][:, i])
            ys[b, :, 256 * hh:256 * (hh + 1)] = r[c]["yT"].reshape(256, L).T
        del r
        del proj
        w_r = np.ascontiguousarray(np.concatenate([f32(router_group_w)[layer], f32(router_expert_w)[layer]], axis=1))
        b_r = _rep(np.concatenate([f32(router_group_b)[layer], f32(router_expert_b)[layer]]))
        ins = []
        for c in range(8):
            b, tk = c // 2, slice((c % 2) * NT, (c % 2 + 1) * NT)
            ins.append({"zT": zTs[c], "oaT": np.ascontiguousarray(o_a[b, tk].T), "ysT": np.ascontiguousarray(ys[b, tk].T),
                        "ocT": np.ascontiguousarray(o_c[b, tk].T), "w_glu": f32(s5_w_glu)[layer], "b_glu": _colT(f32(s5_b_glu)[layer], 8),
                        "w_out": f32(w_out)[layer], "ln2": _colT(f32(ln2_w)[layer], KC), "w_r": w_r, "b_r": b_r})
        r2 = r1 = _run(_prog("C1", build_C1), ins)
        ins = []
        for g in range(8):
            es = slice(EPC * g, EPC * (g + 1))
            xe = np.ascontiguousarray(np.concatenate([r2[c]["xeT"][es] for c in range(8)], axis=2))
            ins.append({"xe": xe, "w1": f32(expert_w1)[layer, es], "w3": f32(expert_w3)[layer, es], "w2": f32(expert_w2)[layer, es]})
        del r2
        r3 = _run(_prog("C2b", build_C2b), ins)
        last = layer == depth - 1
        ins = []
        for c in range(8):
            ye = np.ascontiguousarray(np.concatenate([r3[g]["ye"][:, c * NSL:(c + 1) * NSL] for g in range(8)], axis=0))
            d_ = {"zT": r1[c]["z1T"], "slotT": r1[c]["slotT"], "gtT": r1[c]["gtT"], "ye": ye, "fnw": fnw}
            if not last:
                d_.update({"lnw": _colT(f32(ln1_w)[layer + 1], KC), "w_in": f32(w_in)[layer + 1]})
            ins.append(d_)
        del r3
        r4 = _run(_prog("C2c", build_C2c, last), ins)
        zTs = [r4[c]["outT"] for c in range(8)]
        if not last:
            projTs = [r4[c]["projT"] for c in range(8)]
        del r4
    out = np.stack([np.concatenate([zTs[2 * b].T, zTs[2 * b + 1].T], axis=0) for b in range(NB)])
    return np.ascontiguousarray(out[:, NMETA:]).astype(np.float32)
```

```python
import numpy as np
import concourse.bass as bass
import concourse.mybir as mybir
from concourse.bass_utils import run_bass_kernel_spmd

F32 = mybir.dt.float32
BF16 = mybir.dt.bfloat16
AF = mybir.ActivationFunctionType
ALU = mybir.AluOpType
AX = mybir.AxisListType


class Res:
    __slots__ = ("w", "r", "dsem", "dcnt", "t", "name", "cm")

    def __init__(self, name=""):
        self.w = {}
        self.r = {}
        self.dsem = None
        self.dcnt = 0
        self.t = None
        self.name = name


class T(Res):
    def __getitem__(self, idx):
        return self.t[idx]


class P:
    def __init__(self, nc, same_engine_sync=True):
        self.nc = nc
        self.eng = {"pe": nc.tensor, "dve": nc.vector, "act": nc.scalar, "pool": nc.gpsimd, "sp": nc.sync}
        self.sem = {}
        self.cnt = {}
        for e in ("pe", "dve", "act", "pool"):
            self.sem[e] = nc.semaphore("sem_" + e).__enter__()
            self.cnt[e] = 0
        self.semid = {id(s): e for e, s in self.sem.items()}
        self.waited = {e: {} for e in self.eng}
        self.same_engine_sync = same_engine_sync
        self.out_tokens = []
        self.ntile = 0
        self.tiles = []
        self.ptiles = []
        self.prefix = ""

    def sb(self, shape, dt=F32, name=None):
        self.ntile += 1
        name = self.prefix + (name or f"t{self.ntile}")
        t = T(name)
        t.cm = self.nc.sbuf_tensor(name, list(shape), dt)
        t.t = t.cm.__enter__()
        self.tiles.append(t)
        return t

    def ps(self, shape, dt=F32, name=None):
        self.ntile += 1
        name = self.prefix + (name or f"p{self.ntile}")
        t = T(name)
        t.cm = self.nc.psum_tensor(name, list(shape), dt)
        t.t = t.cm.__enter__()
        self.ptiles.append(t)
        return t

    def _wait(self, e, deps, skip_own=False):
        eng = self.eng[e]
        own = self.sem.get(e)
        for sem, val in deps.items():
            if sem is own and (skip_own or not self.same_engine_sync):
                continue
            k = id(sem)
            if self.waited[e].get(k, 0) >= val:
                continue
            eng.wait_ge(sem, val)
            self.waited[e][k] = val

    @staticmethod
    def _merge(d, s):
        for k, v in s.items():
            if d.get(k, 0) < v:
                d[k] = v

    def op(self, e, fn, reads=(), writes=(), accum=False):
        deps = {}
        for r in reads:
            self._merge(deps, r.w)
        for w in writes:
            self._merge(deps, w.w)
            self._merge(deps, w.r)
        self._wait(e, deps, skip_own=(e == "pe"))
        inst = fn(self.eng[e])
        self.cnt[e] += 1
        inst.then_inc(self.sem[e], 1)
        tok = {self.sem[e]: self.cnt[e]}
        for r in reads:
            self._merge(r.r, tok)
        for w in writes:
            if accum:
                self._merge(w.w, tok)
            else:
                w.w = dict(tok)
                w.r = {}
        return inst

    def dma(self, q, out, in_, reads=(), writes=(), semres=None, concurrent=False, **kw):
        if semres is None:
            semres = (list(writes) + list(reads))[0]
        if semres.dsem is None:
            semres.dsem = self.nc.semaphore("d_" + semres.name).__enter__()
        deps = {}
        for r in reads:
            self._merge(deps, r.w)
        for w in writes:
            if concurrent:
                ww = {k: v for k, v in w.w.items() if k is not w.dsem}
                self._merge(deps, ww)
            else:
                self._merge(deps, w.w)
            self._merge(deps, w.r)
        self._wait(q, deps)
        inst = self.eng[q].dma_start(out=out, in_=in_, **kw)
        semres.dcnt += 16
        inst.then_inc(semres.dsem, 16)
        tok = {semres.dsem: semres.dcnt}
        for r in reads:
            self._merge(r.r, tok)
        for w in writes:
            w.w = dict(tok)
            w.r = {}
        return tok

    def barrier(self):
        deps = {}
        for e in ("pe", "dve", "act", "pool"):
            if self.cnt[e] > 0:
                deps[self.sem[e]] = self.cnt[e]
        for t in self.tiles:
            if t.dsem is not None and t.dcnt > 0:
                deps[t.dsem] = t.dcnt
        for e in self.eng:
            self._wait(e, deps, skip_own=True)

    def mark(self):
        return (len(self.tiles), len(self.ptiles))

    def release(self, mark):
        self.barrier()
        for lst, n in ((self.tiles, mark[0]), (self.ptiles, mark[1])):
            while len(lst) > n:
                t = lst.pop()
                t.cm.__exit__(None, None, None)

    def free(self, tiles):
        for t in reversed(tiles):
            t.cm.__exit__(None, None, None)
            self.tiles.remove(t)

    def finish(self, out_res):
        deps = {}
        for r in out_res:
            self._merge(deps, r.r)
            self._merge(deps, r.w)
        self._wait("sp", deps)


D = 2048
NB = 4
SEQ = 2048
NMETA = 16
L = SEQ + NMETA
NT = L // 2
NCH = 3
TCH = NT // NCH
INW = 5632
KC = D // 128
EPS = 1e-6


class Rot:
    def __init__(self, tiles):
        self.tiles = tiles
        self.i = 0

    def next(self):
        t = self.tiles[self.i % len(self.tiles)]
        self.i += 1
        return t


def emit_rmsnorm(p, z, lnw, hn, ones_bf, epsc, sq_rot, ps_rot, rstd, hn32=None, nt=NT, tch=TCH):
    nch = nt // tch
    pss = [ps_rot.next() for _ in range(nch)]
    for c in range(KC):
        sq = sq_rot.next()
        p.op("act", lambda e: e.activation(out=sq[:, 0:nt], in_=z[:, c, 0:nt], func=AF.Square), reads=[z], writes=[sq])
        for n in range(nch):
            p.op("pe", lambda e: e.matmul(pss[n][:, 0:tch], lhsT=ones_bf[:], rhs=sq[:, n * tch:(n + 1) * tch],
                                          start=(c == 0), stop=(c == KC - 1)),
                 reads=[sq, ones_bf], writes=[pss[n]], accum=(c > 0))
    for n in range(nch):
        sl = slice(n * tch, (n + 1) * tch)
        p.op("act", lambda e: e.activation(out=rstd[:, sl], in_=pss[n][:, 0:tch], func=AF.Sqrt, bias=epsc[:, 0:1]),
             reads=[pss[n], epsc], writes=[rstd], accum=(n > 0))
    p.op("dve", lambda e: e.reciprocal(out=rstd[:, 0:nt], in_=rstd[:, 0:nt]), reads=[rstd], writes=[rstd])
    for c in range(KC):
        if hn is not None:
            p.op("dve", lambda e: e.scalar_tensor_tensor(out=hn[:, c, 0:nt], in0=z[:, c, 0:nt], scalar=lnw[:, c:c + 1], in1=rstd[:, 0:nt],
                                                         op0=ALU.mult, op1=ALU.mult), reads=[z, lnw, rstd], writes=[hn])
        if hn32 is not None:
            p.op("dve", lambda e: e.scalar_tensor_tensor(out=hn32[:, c, 0:nt], in0=z[:, c, 0:nt], scalar=lnw[:, c:c + 1], in1=rstd[:, 0:nt],
                                                         op0=ALU.mult, op1=ALU.mult), reads=[z, lnw, rstd], writes=[hn32])


def emit_inproj(p, hn, w_in, projT, wb_rot, ps_rot, st_rot, evac):
    GW = 512
    w_v = w_in.rearrange("(c p) n -> p c n", p=128)
    for g in range(INW // GW):
        wb = wb_rot.next()
        p.dma("pool", wb[:], w_v[:, :, g * GW:(g + 1) * GW], writes=[wb])
        for j in range(GW // 128):
            st = st_rot.next()
            for n in range(NCH):
                ps = ps_rot.next()
                for c in range(KC):
                    p.op("pe", lambda e: e.matmul(ps[:, 0:TCH], lhsT=wb[:, c, j * 128:(j + 1) * 128],
                                                  rhs=hn[:, c, n * TCH:(n + 1) * TCH], start=(c == 0), stop=(c == KC - 1)),
                         reads=[wb, hn], writes=[ps], accum=(c > 0))
                evac(st[:, n * TCH:(n + 1) * TCH], ps[:, 0:TCH], [ps], [st], first=(n == 0))
            r0 = g * GW + j * 128
            p.dma("sp", projT[r0:r0 + 128, :], st[:, 0:NT], reads=[st])


class Evac:
    def __init__(self, p):
        self.p = p
        self.i = 0

    def __call__(self, out, in_, reads, writes, first=True):
        self.i += 1
        p = self.p
        if self.i % 2 == 0:
            p.op("act", lambda e: e.activation(out=out, in_=in_, func=AF.Copy), reads=reads, writes=writes, accum=not first)
        else:
            p.op("dve", lambda e: e.tensor_copy(out=out, in_=in_), reads=reads, writes=writes, accum=not first)


def build_A():
    nc = bass.Bass("TRN2", target_bir_lowering=False)
    p = P(nc)
    zT = nc.dram_tensor("zT", [D, NT], F32, kind="ExternalInput").ap()
    lnw_d = nc.dram_tensor("lnw", [128, KC], F32, kind="ExternalInput").ap()
    w_in = nc.dram_tensor("w_in", [D, INW], F32, kind="ExternalInput").ap()
    projT = nc.dram_tensor("projT", [INW, NT], F32, kind="ExternalOutput").ap()
    z = p.sb([128, KC, NT], F32, "z")
    hn = p.sb([128, KC, NT], BF16, "hn")
    lnw = p.sb([128, KC], F32, "lnw_s")
    rstd = p.sb([128, NT], F32, "rstd")
    ones_bf = p.sb([128, 128], BF16, "ones")
    sq_rot = Rot([p.sb([128, NT], BF16, f"sq{i}") for i in range(2)])
    ps_rot = Rot([p.ps([128, 512], F32, f"ps{i}") for i in range(8)])
    wb_rot = Rot([p.sb([128, KC, 512], BF16, f"wb{i}") for i in range(2)])
    st_rot = Rot([p.sb([128, NT], F32, f"st{i}") for i in range(3)])
    evac = Evac(p)
    zv = zT.rearrange("(c p) t -> p c t", p=128)
    for q in range(4):
        p.dma("sp", z[:, 4 * q:4 * q + 4, :], zv[:, 4 * q:4 * q + 4, :], writes=[z], concurrent=True)
    p.dma("sp", lnw[:], lnw_d[:, :], writes=[lnw])
    p.op("pool", lambda e: e.memset(ones_bf[:], 1.0 / D), writes=[ones_bf])
    epsc = p.sb([128, 1], F32, "epsc")
    p.op("pool", lambda e: e.memset(epsc[:], EPS), writes=[epsc])
    emit_rmsnorm(p, z, lnw, hn, ones_bf, epsc, sq_rot, ps_rot, rstd)
    emit_inproj(p, hn, w_in, projT, wb_rot, ps_rot, st_rot, evac)
    p.finish(st_rot.tiles)
    return nc


HC = 32
LP = 2176
NCK = LP // HC


def build_B1():
    nc = bass.Bass("TRN2", target_bir_lowering=False)
    p = P(nc)
    outs = emit_B1(nc, p)
    p.finish(outs)
    return nc


def emit_B1(nc, p):
    hqT = nc.dram_tensor("hqT", [2, 128, LP], F32, kind="ExternalInput").ap()
    hfT = nc.dram_tensor("hfT", [2, 128, LP], F32, kind="ExternalInput").ap()
    hv = nc.dram_tensor("hv", [2, HC, NCK, 128], F32, kind="ExternalInput").ap()
    hg = nc.dram_tensor("hg", [2, 128, LP // 128, 128], F32, kind="ExternalInput").ap()
    lbraw_d = nc.dram_tensor("lbraw", [128, 2, 2], F32, kind="ExternalInput").ap()
    lsel_d = nc.dram_tensor("lsel", [128, 1], F32, kind="ExternalInput").ap()
    nw_d = nc.dram_tensor("nw", [128, 128], F32, kind="ExternalInput").ap()
    oa = nc.dram_tensor("oa", [2, 128, LP // 128, 128], F32, kind="ExternalOutput").ap()

    lb = p.sb([128, 2], F32, "lb_s")
    oml = p.sb([128, 2], F32, "oml")
    nw = p.sb([128, 128], F32, "nw_s")
    epsc = p.sb([128, 1], F32, "epsc")
    maskU = p.sb([HC, HC], F32, "maskU")
    mm = p.sb([128, LP], F32, "mm")
    ident = p.sb([128, 128], BF16, "ident")
    identf = p.sb([128, 128], F32, "identf")
    lbraw = p.sb([128, 2, 2], F32, "lbraw_s")
    lsel = p.sb([128, 1], F32, "lsel_s")
    p.dma("sp", lbraw[:], lbraw_d[:, :, :], writes=[lbraw])
    p.dma("sp", lsel[:], lsel_d[:, :], writes=[lsel])
    p.op("dve", lambda e: e.tensor_tensor(out=lb[:], in0=lbraw[:, :, 1], in1=lbraw[:, :, 0], op=ALU.subtract), reads=[lbraw], writes=[lb])
    p.op("act", lambda e: e.activation(out=lb[:], in_=lb[:], func=AF.Sigmoid), reads=[lb], writes=[lb])
    p.op("dve", lambda e: e.tensor_scalar(out=lb[:], in0=lb[:], scalar1=lsel[:, 0:1], scalar2=None, op0=ALU.mult), reads=[lb, lsel], writes=[lb])
    p.dma("sp", nw[:], nw_d[:, :], writes=[nw])
    p.op("pool", lambda e: e.memset(epsc[:], EPS), writes=[epsc])
    p.op("pool", lambda e: e.memset(maskU[:], 1.0), writes=[maskU])
    p.op("pool", lambda e: e.affine_select(out=maskU[:], in_=maskU[:], pattern=[[1, HC]], compare_op=ALU.is_ge, fill=0.0,
                                           base=0, channel_multiplier=-1), reads=[maskU], writes=[maskU])
    p.op("pool", lambda e: e.memset(identf[:], 0.0), writes=[identf])
    p.op("pool", lambda e: e.affine_select(out=identf[:], in_=identf[:], pattern=[[-1, 128]], compare_op=ALU.not_equal,
                                           fill=1.0, base=0, channel_multiplier=1), reads=[identf], writes=[identf])
    p.op("pool", lambda e: e.tensor_copy(out=ident[:], in_=identf[:]), reads=[identf], writes=[ident])
    p.op("pool", lambda e: e.memset(mm[:], 1.0), writes=[mm])
    p.op("pool", lambda e: e.memset(mm[:].rearrange("p (n c) -> p n c", c=HC)[:, :, 0:1], 0.0), reads=[mm], writes=[mm])
    p.op("dve", lambda e: e.tensor_scalar(out=oml[:], in0=lb[:], scalar1=-1.0, scalar2=1.0, op0=ALU.mult, op1=ALU.add),
         reads=[lb], writes=[oml])

    H = []
    shared = {nm: p.sb([128, LP], F32, "sh_" + nm) for nm in ("q", "f", "b", "e")}
    for h in range(2):
        d = {}
        for nm in ("q", "f", "b", "e"):
            d[nm] = shared[nm]
        d["qs"] = p.sb([128, LP], BF16, f"qs{h}")
        d["ks"] = p.sb([128, LP], BF16, f"ks{h}")
        d["kh"] = p.sb([128, LP], BF16, f"kh{h}")
        d["ebend"] = p.sb([128, NCK], F32, f"ebend{h}")
        d["v"] = p.sb([HC, NCK, 128], BF16, f"v{h}")
        d["g"] = p.sb([128, LP // 128, 128], F32, f"g{h}")
        d["o"] = p.sb([128, LP // 128, 128], F32, f"o{h}")
        d["S"] = p.sb([128, 128], F32, f"S{h}")
        d["Sb"] = p.sb([128, 128], BF16, f"Sb{h}")
        d["khT"] = Rot([p.sb([HC, 128], BF16, f"khT{h}_{i}") for i in range(3)])
        d["ATm"] = Rot([p.sb([HC, HC], BF16, f"ATm{h}_{i}") for i in range(2)])
        d["ms"] = p.sb([128, LP // 128], F32, f"ms{h}")
        H.append(d)
        p.dma("pool", d["v"][:], hv[h], writes=[d["v"]])
        p.dma("sp", d["g"][:], hg[h], writes=[d["g"]])
    psA = Rot([p.ps([128, 512], F32, f"psA{i}") for i in range(2)])
    psT = Rot([p.ps([128, 512], BF16, f"psT{i}") for i in range(2)])
    psO = Rot([p.ps([128, 512], F32, f"psO{i}") for i in range(2)])
    psS = Rot([p.ps([128, 512], F32, f"psS{i}") for i in range(2)])

    for h in range(2):
        d = H[h]
        q, f, b, e_ = d["q"], d["f"], d["b"], d["e"]
        p.dma("sp", q[:], hqT[h], writes=[q])
        p.dma("sp", f[:], hfT[h], writes=[f])
        p.op("act", lambda e: e.activation(out=f[:], in_=f[:], func=AF.Sigmoid), reads=[f], writes=[f])
        p.op("dve", lambda e: e.tensor_scalar(out=f[:], in0=f[:], scalar1=oml[:, h:h + 1], scalar2=lb[:, h:h + 1],
                                              op0=ALU.mult, op1=ALU.add), reads=[f, oml, lb], writes=[f])
        p.op("act", lambda e: e.activation(out=e_[:], in_=f[:], func=AF.Ln), reads=[f], writes=[e_])
        p.op("dve", lambda e: e.tensor_tensor_scan(out=b[:], data0=mm[:], data1=e_[:], initial=0.0, op0=ALU.mult, op1=ALU.add),
             reads=[mm, e_], writes=[b])
        p.op("dve", lambda e: e.tensor_scalar(out=f[:], in0=f[:], scalar1=-1.0, scalar2=1.0, op0=ALU.mult, op1=ALU.add),
             reads=[f], writes=[f])
        p.op("act", lambda e: e.activation(out=e_[:], in_=b[:], func=AF.Exp), reads=[b], writes=[e_])
        p.op("dve", lambda e: e.tensor_tensor(out=d["qs"][:], in0=q[:], in1=e_[:], op=ALU.mult), reads=[q, e_], writes=[d["qs"]])
        p.op("act", lambda e: e.activation(out=e_[:], in_=b[:], func=AF.Exp, scale=-1.0), reads=[b], writes=[e_])
        p.op("dve", lambda e: e.tensor_tensor(out=d["ks"][:], in0=f[:], in1=e_[:], op=ALU.mult), reads=[f, e_], writes=[d["ks"]])
        b3 = b[:].rearrange("p (n c) -> p n c", c=HC)
        p.op("act", lambda e: e.activation(out=d["ebend"][:], in_=b3[:, :, HC - 1], func=AF.Exp), reads=[b], writes=[d["ebend"]])
        e3 = e_[:].rearrange("p (n c) -> p n c", c=HC)
        p.op("dve", lambda e: e.tensor_tensor(out=e3, in0=b3[:, :, HC - 1:HC].to_broadcast([128, NCK, HC]), in1=b3, op=ALU.subtract),
             reads=[b], writes=[e_])
        p.op("act", lambda e: e.activation(out=e_[:], in_=e_[:], func=AF.Exp), reads=[e_], writes=[e_])
        p.op("dve", lambda e: e.tensor_tensor(out=d["kh"][:], in0=f[:], in1=e_[:], op=ALU.mult), reads=[f, e_], writes=[d["kh"]])
        p.op("pool", lambda e: e.memset(d["S"][:], 0.0), writes=[d["S"]])
        p.op("pool", lambda e: e.memset(d["Sb"][:], 0.0), writes=[d["Sb"]])

    for n in range(NCK):
        for h in range(2):
            d = H[h]
            sl = slice(n * HC, (n + 1) * HC)
            qs, ks, kh, v, S, Sb = d["qs"], d["ks"], d["kh"], d["v"], d["S"], d["Sb"]
            pa = psA.next()
            p.op("pe", lambda e: e.matmul(pa[0:HC, 0:HC], lhsT=ks[:, sl], rhs=qs[:, sl], start=True, stop=True),
                 reads=[ks, qs], writes=[pa])
            atm = d["ATm"].next()
            p.op("dve", lambda e: e.tensor_tensor(out=atm[:], in0=pa[0:HC, 0:HC], in1=maskU[:], op=ALU.mult),
                 reads=[pa, maskU], writes=[atm])
            pt = psT.next()
            p.op("pe", lambda e: e.transpose(out=pt[0:HC, 0:128], in_=kh[:, sl], identity=ident[:]), reads=[kh, ident], writes=[pt])
            kht = d["khT"].next()
            p.op("act", lambda e: e.activation(out=kht[:], in_=pt[0:HC, 0:128], func=AF.Copy), reads=[pt], writes=[kht])
            po = psO.next()
            p.op("pe", lambda e: e.matmul(po[0:HC, 0:128], lhsT=atm[:], rhs=v[:, n, :], start=True, stop=False),
                 reads=[atm, v], writes=[po])
            p.op("pe", lambda e: e.matmul(po[0:HC, 0:128], lhsT=qs[:, sl], rhs=Sb[:], start=False, stop=True),
                 reads=[qs, Sb], writes=[po], accum=True)
            pj = HC * (n % 4)
            p.op("act", lambda e: e.activation(out=d["o"][pj:pj + HC, n // 4, :], in_=po[0:HC, 0:128], func=AF.Copy), reads=[po],
                 writes=[d["o"]], accum=(n > 0))
            pS = psS.next()
            p.op("pe", lambda e: e.matmul(pS[:, 0:128], lhsT=kht[:], rhs=v[:, n, :], start=True, stop=True),
                 reads=[kht, v], writes=[pS])
            p.op("dve", lambda e: e.scalar_tensor_tensor(out=S[:], in0=S[:], scalar=d["ebend"][:, n:n + 1], in1=pS[:, 0:128],
                                                         op0=ALU.mult, op1=ALU.add), reads=[S, d["ebend"], pS], writes=[S])
            p.op("act", lambda e: e.activation(out=Sb[:], in_=S[:], func=AF.Copy), reads=[S], writes=[Sb])

    for h in range(2):
        d = H[h]
        o, g, ms = d["o"], d["g"], d["ms"]
        sq = shared["q"]
        NT_ = LP // 128
        sq3 = sq[:].rearrange("p (n c) -> p n c", c=128)
        p.op("pool", lambda e: e.tensor_tensor(out=sq3, in0=o[:], in1=o[:], op=ALU.mult), reads=[o], writes=[sq])
        p.op("dve", lambda e: e.tensor_reduce(out=ms[:], in_=sq3, axis=AX.X, op=ALU.add), reads=[sq], writes=[ms])
        p.op("act", lambda e: e.activation(out=ms[:], in_=ms[:], func=AF.Sqrt, bias=epsc[:, 0:1], scale=1.0 / 128),
             reads=[ms, epsc], writes=[ms])
        p.op("dve", lambda e: e.reciprocal(out=ms[:], in_=ms[:]), reads=[ms], writes=[ms])
        p.op("dve", lambda e: e.tensor_tensor(out=o[:], in0=o[:], in1=ms[:].unsqueeze(2).to_broadcast([128, NT_, 128]), op=ALU.mult),
             reads=[o, ms], writes=[o])
        p.op("pool", lambda e: e.tensor_tensor(out=o[:], in0=o[:], in1=nw[:].unsqueeze(1).to_broadcast([128, NT_, 128]), op=ALU.mult),
             reads=[o, nw], writes=[o])
        p.op("act", lambda e: e.activation(out=g[:], in_=g[:], func=AF.Silu), reads=[g], writes=[g])
        p.op("dve", lambda e: e.tensor_tensor(out=o[:], in0=o[:], in1=g[:], op=ALU.mult), reads=[o, g], writes=[o])
        p.dma("sp", oa[h], o[:], reads=[o])
    return [H[0]["o"], H[1]["o"]]


S5N = 6
S5_ENG = ["dve", "dve", "dve", "dve"]
S5C = L // S5N
PI = 3.14159265358979


def build_B2():
    nc = bass.Bass("TRN2", target_bir_lowering=False)
    p = P(nc)
    outs = emit_B2(nc, p)
    p.finish(outs)
    return nc


def emit_B2(nc, p):
    uT = nc.dram_tensor("uT", [2, 128, L], F32, kind="ExternalInput").ap()
    are_d = nc.dram_tensor("a_re", [128, 8], F32, kind="ExternalInput").ap()
    aim_d = nc.dram_tensor("a_im", [128, 8], F32, kind="ExternalInput").ap()
    ldt_d = nc.dram_tensor("ldt", [128, 8], F32, kind="ExternalInput").ap()
    bre_d = nc.dram_tensor("b_re", [128, 8, 16], F32, kind="ExternalInput").ap()
    bim_d = nc.dram_tensor("b_im", [128, 8, 16], F32, kind="ExternalInput").ap()
    cre_d = nc.dram_tensor("cT_re", [128, 8, 16], F32, kind="ExternalInput").ap()
    cim_d = nc.dram_tensor("cT_im", [128, 8, 16], F32, kind="ExternalInput").ap()
    dsk_d = nc.dram_tensor("dsk", [128, 2], F32, kind="ExternalInput").ap()
    yT = nc.dram_tensor("yT", [2, 128, L], F32, kind="ExternalOutput").ap()

    def small(name, shape=(128, 8)):
        return p.sb(list(shape), F32, name)

    a_re, a_im, ldt = small("are_s"), small("aim_s"), small("ldt_s")
    b_re, b_im = small("bre_s", (128, 8, 16)), small("bim_s", (128, 8, 16))
    c_re, c_im = small("cre_s", (128, 8, 16)), small("cim_s", (128, 8, 16))
    dsk = small("dsk_s", (128, 2))
    for t_, d_ in ((a_re, are_d), (a_im, aim_d), (ldt, ldt_d), (dsk, dsk_d)):
        p.dma("sp", t_[:], d_[:, :], writes=[t_])
    for t_, d_ in ((b_re, bre_d), (b_im, bim_d), (c_re, cre_d), (c_im, cim_d)):
        p.dma("sp", t_[:], d_[:, :, :], writes=[t_])
    u = p.sb([128, 2, L], F32, "u")
    ub = p.sb([128, 2, L], BF16, "ub")
    p.dma("sp", u[:], uT.rearrange("k p t -> p k t"), writes=[u])
    p.dma("pool", ub[:], uT.rearrange("k p t -> p k t"), writes=[ub])
    pic = small("pic", (128, 1))
    p.op("pool", lambda e: e.memset(pic[:], PI), writes=[pic])
    identf = p.sb([128, 128], F32, "identf")
    p.op("pool", lambda e: e.memset(identf[:], 0.0), writes=[identf])
    p.op("pool", lambda e: e.affine_select(out=identf[:], in_=identf[:], pattern=[[-1, 128]], compare_op=ALU.not_equal,
                                           fill=1.0, base=0, channel_multiplier=1), reads=[identf], writes=[identf])
    iot = p.sb([128, L], F32, "iot")
    p.op("pool", lambda e: e.iota(iot[:], pattern=[[1, L]], base=0, channel_multiplier=0, allow_small_or_imprecise_dtypes=True), writes=[iot])

    def ts(out, in0, s1, s2, op0, op1=None, eng="dve"):
        rd = [in0] + [x for x in (s1, s2) if isinstance(x, T)]
        a1 = s1[:] if isinstance(s1, T) else s1
        a2 = s2[:] if isinstance(s2, T) else s2
        if op1 is None:
            p.op(eng, lambda e: e.tensor_scalar(out=out[:], in0=in0[:], scalar1=a1, scalar2=None, op0=op0), reads=rd, writes=[out])
        else:
            p.op(eng, lambda e: e.tensor_scalar(out=out[:], in0=in0[:], scalar1=a1, scalar2=a2, op0=op0, op1=op1), reads=rd, writes=[out])

    def tt(out, in0, in1, op, eng="dve"):
        p.op(eng, lambda e: e.tensor_tensor(out=out[:], in0=in0[:], in1=in1[:], op=op), reads=[in0, in1], writes=[out])

    I32 = mybir.dt.int32

    def wrap_sin(out, x, tf, ti, shift=0.0):
        if shift != 0.0:
            ts(tf, x, shift, None, ALU.add, eng="pool")
            x = tf
        p.op("dve", lambda e: e.tensor_scalar(out=ti[:], in0=x[:], scalar1=1.0 / (2 * PI), scalar2=None, op0=ALU.mult), reads=[x], writes=[ti])
        kf = out
        p.op("pool", lambda e: e.tensor_copy(out=kf[:], in_=ti[:]), reads=[ti], writes=[kf])
        p.op("dve", lambda e: e.scalar_tensor_tensor(out=tf[:], in0=kf[:], scalar=-2 * PI, in1=x[:], op0=ALU.mult, op1=ALU.add),
             reads=[kf, x], writes=[tf])
        ts(tf, tf, PI, -PI, ALU.min, ALU.max, eng="pool")
        p.op("act", lambda e: e.activation(out=out[:], in_=tf[:], func=AF.Sin), reads=[tf], writes=[out])

    dt, th, mag, tmp, tmp2 = small("dt"), small("th"), small("mag"), small("tmp"), small("tmp2")
    s1, c1, abr, abi, den, zre, zim = (small(n) for n in ("s1", "c1", "abr", "abi", "den", "zre", "zim"))
    p.op("act", lambda e: e.activation(out=dt[:], in_=ldt[:], func=AF.Exp), reads=[ldt], writes=[dt])
    tt(tmp, a_re, dt, ALU.mult)
    p.op("act", lambda e: e.activation(out=mag[:], in_=tmp[:], func=AF.Exp), reads=[tmp], writes=[mag])
    tt(th, a_im, dt, ALU.mult)
    smi = p.sb([128, 8], I32, "smi")
    p.op("dve", lambda e: e.tensor_scalar(out=smi[:], in0=th[:], scalar1=1.0 / (2 * PI), scalar2=None, op0=ALU.mult), reads=[th], writes=[smi])
    p.op("dve", lambda e: e.tensor_copy(out=tmp[:], in_=smi[:]), reads=[smi], writes=[tmp])
    p.op("dve", lambda e: e.scalar_tensor_tensor(out=th[:], in0=tmp[:], scalar=-2 * PI, in1=th[:], op0=ALU.mult, op1=ALU.add),
         reads=[tmp, th], writes=[th])
    wrap_sin(s1, th, tmp, smi)
    wrap_sin(c1, th, tmp, smi, shift=PI / 2)
    tt(abr, mag, c1, ALU.mult)
    tt(abi, mag, s1, ALU.mult)
    tt(den, a_re, a_re, ALU.mult)
    tt(tmp, a_im, a_im, ALU.mult)
    tt(den, den, tmp, ALU.add)
    p.op("dve", lambda e: e.reciprocal(out=den[:], in_=den[:]), reads=[den], writes=[den])
    ts(tmp, abr, -1.0, None, ALU.add)
    tt(zre, tmp, a_re, ALU.mult)
    tt(tmp2, abi, a_im, ALU.mult)
    tt(zre, zre, tmp2, ALU.add)
    tt(zre, zre, den, ALU.mult)
    tt(zim, abi, a_re, ALU.mult)
    tt(tmp2, tmp, a_im, ALU.mult)
    tt(zim, zim, tmp2, ALU.subtract)
    tt(zim, zim, den, ALU.mult)
    bbr, bbi, t3a, t3b = (small(n, (128, 8, 16)) for n in ("bbr", "bbi", "t3a", "t3b"))

    def bc(x):
        return x[:].unsqueeze(2).to_broadcast([128, 8, 16])

    p.op("dve", lambda e: e.tensor_tensor(out=t3a[:], in0=b_re[:], in1=bc(zre), op=ALU.mult), reads=[b_re, zre], writes=[t3a])
    p.op("dve", lambda e: e.tensor_tensor(out=t3b[:], in0=b_im[:], in1=bc(zim), op=ALU.mult), reads=[b_im, zim], writes=[t3b])
    tt(bbr, t3a, t3b, ALU.subtract)
    p.op("dve", lambda e: e.tensor_tensor(out=t3a[:], in0=b_im[:], in1=bc(zre), op=ALU.mult), reads=[b_im, zre], writes=[t3a])
    p.op("dve", lambda e: e.tensor_tensor(out=t3b[:], in0=b_re[:], in1=bc(zim), op=ALU.mult), reads=[b_re, zim], writes=[t3b])
    tt(bbi, t3a, t3b, ALU.add)
    ts(c_im, c_im, -1.0, None, ALU.mult)

    ps_rot = Rot([p.ps([128, 512], F32, f"ps{i}") for i in range(6)])
    WB = [[p.sb([128, 128], BF16, f"WB{ri}_{j}") for j in range(8)] for ri in range(2)]
    WC = [[p.sb([128, 128], BF16, f"WC{ri}_{j}") for j in range(8)] for ri in range(2)]
    stg = Rot([p.sb([128, 128], F32, f"stg{i}") for i in range(2)])
    for j in range(8):
        j4 = j % 4
        for ri, src in enumerate((bbr, bbi)):
            st = stg.next()
            p.op("pool", lambda e: e.memset(st[:], 0.0), writes=[st])
            p.op("pool", lambda e: e.tensor_copy(out=st[0:64, 32 * j4:32 * j4 + 16], in_=src[0:64, j, :]), reads=[src], writes=[st], accum=True)
            p.op("pool", lambda e: e.tensor_copy(out=st[64:128, 32 * j4 + 16:32 * j4 + 32], in_=src[64:128, j, :]), reads=[src], writes=[st], accum=True)
            ps = ps_rot.next()
            p.op("pe", lambda e: e.transpose(out=ps[:, 0:128], in_=st[:], identity=identf[:]), reads=[st, identf], writes=[ps])
            p.op("act", lambda e: e.activation(out=WB[ri][j][:], in_=ps[:, 0:128], func=AF.Copy), reads=[ps], writes=[WB[ri][j]])
        for ri, src in enumerate((c_re, c_im)):
            w = WC[ri][j]
            p.op("pool", lambda e: e.memset(w[:], 0.0), writes=[w])
            p.op("pool", lambda e: e.tensor_copy(out=w[0:64, 32 * j4:32 * j4 + 16], in_=src[0:64, j, :]), reads=[src], writes=[w], accum=True)
            p.op("pool", lambda e: e.tensor_copy(out=w[64:128, 32 * j4 + 16:32 * j4 + 32], in_=src[64:128, j, :]), reads=[src], writes=[w], accum=True)

    NH = L // 2
    HN = NH // S5C
    big = lambda n, dt_=F32: p.sb([128, L], dt_, n)
    hpic = small("hpic", (128, 1))
    p.op("pool", lambda e: e.memset(hpic[:], PI / 2), writes=[hpic])

    def mkset(i):
        d_ = {nm: p.sb([128, NH], F32, f"{nm}{i}") for nm in ("sinT", "cosT", "arg", "bur", "bui", "wr", "wi", "ta", "tb")}
        d_["argi"] = p.sb([128, NH], I32, f"argi{i}")
        return d_

    sets = [mkset(0), mkset(1)]
    rB_rot = Rot([p.sb([128, NH], F32, f"rB{i}") for i in range(2)])
    xs = [[big(f"x{ri}_{j4}", BF16) for j4 in range(4)] for ri in range(2)]
    yo = Rot([big(f"yo{i}") for i in range(2)])
    evac = Evac(p)

    def wsin(out, x, tf, ti, shift=0.0):
        if shift != 0.0:
            p.op("act", lambda e: e.activation(out=tf[:], in_=x[:], func=AF.Identity, bias=hpic[:, 0:1]), reads=[x, hpic], writes=[tf])
            x = tf
        p.op("dve", lambda e: e.tensor_scalar(out=ti[:], in0=x[:], scalar1=1.0 / (2 * PI), scalar2=None, op0=ALU.mult), reads=[x], writes=[ti])
        p.op("act", lambda e: e.activation(out=out[:], in_=ti[:], func=AF.Copy), reads=[ti], writes=[out])
        p.op("dve", lambda e: e.scalar_tensor_tensor(out=tf[:], in0=out[:], scalar=-2 * PI, in1=x[:], op0=ALU.mult, op1=ALU.add),
             reads=[out, x], writes=[tf])
        ts(tf, tf, PI, -PI, ALU.min, ALU.max, eng="dve")
        p.op("act", lambda e: e.activation(out=out[:], in_=tf[:], func=AF.Sin), reads=[tf], writes=[out])

    def tables(st):
        j, hf = st // 2, st % 2
        S_ = sets[st % 2]
        t0 = hf * NH
        p.op("dve", lambda e: e.tensor_scalar(out=S_["arg"][:], in0=iot[:, t0:t0 + NH], scalar1=th[:, j:j + 1], scalar2=None, op0=ALU.mult),
             reads=[iot, th], writes=[S_["arg"]])
        wsin(S_["sinT"], S_["arg"], S_["tb"], S_["argi"])
        wsin(S_["cosT"], S_["arg"], S_["tb"], S_["argi"], shift=PI / 2)

    tables(0)
    step = 0
    prev = None
    for j in range(8):
        k, j4 = j // 4, j % 4
        rB = rB_rot.next()
        p.op("pool", lambda e: e.memset(rB[:], 1.0), writes=[rB])
        p.op("pool", lambda e: e.tensor_scalar(out=rB[:], in0=rB[:], scalar1=mag[:, j:j + 1], scalar2=None, op0=ALU.mult), reads=[rB, mag], writes=[rB])
        for hf in range(2):
            S_ = sets[step % 2]
            step += 1
            t0 = hf * NH
            sinT, cosT, arg, argi = S_["sinT"], S_["cosT"], S_["arg"], S_["argi"]
            bur, bui, wr, wi, ta, tb = S_["bur"], S_["bui"], S_["wr"], S_["wi"], S_["ta"], S_["tb"]
            for n in range(HN):
                sl = slice(n * S5C, (n + 1) * S5C)
                gl_ = slice(t0 + n * S5C, t0 + (n + 1) * S5C)
                for ri, dst in enumerate((bur, bui)):
                    ps = ps_rot.next()
                    p.op("pe", lambda e: e.matmul(ps[:, 0:S5C], lhsT=WB[ri][j][:], rhs=ub[:, k, gl_], start=True, stop=True),
                         reads=[WB[ri][j], ub], writes=[ps])
                    evac(dst[:, sl], ps[:, 0:S5C], [ps], [dst], first=(n == 0))
            tt(ta, cosT, bur, ALU.mult)
            tt(tb, sinT, bui, ALU.mult, eng=S5_ENG[0])
            tt(wr, ta, tb, ALU.add)
            tt(ta, cosT, bui, ALU.mult)
            tt(tb, sinT, bur, ALU.mult, eng=S5_ENG[1])
            tt(wi, ta, tb, ALU.subtract)
            if step < 16:
                tables(step)
            if hf == 0:
                ir, ii, rd = 0.0, 0.0, []
            else:
                ir, ii, rd = prev["bur"][:, NH - 1:NH], prev["bui"][:, NH - 1:NH], [prev["bur"], prev["bui"]]
            p.op("dve", lambda e: e.tensor_tensor_scan(out=bur[:], data0=rB[:], data1=wr[:], initial=ir, op0=ALU.mult, op1=ALU.add),
                 reads=[rB, wr] + rd, writes=[bur])
            p.op("dve", lambda e: e.tensor_tensor_scan(out=bui[:], data0=rB[:], data1=wi[:], initial=ii, op0=ALU.mult, op1=ALU.add),
                 reads=[rB, wi] + rd, writes=[bui])
            xr, xi = xs[0][j4], xs[1][j4]
            tt(ta, cosT, bur, ALU.mult)
            tt(tb, sinT, bui, ALU.mult, eng=S5_ENG[2])
            p.op("dve", lambda e: e.tensor_tensor(out=xr[:, t0:t0 + NH], in0=ta[:], in1=tb[:], op=ALU.subtract), reads=[ta, tb], writes=[xr], accum=(hf > 0))
            tt(wr, cosT, bui, ALU.mult)
            tt(wi, sinT, bur, ALU.mult, eng=S5_ENG[3])
            p.op("dve", lambda e: e.tensor_tensor(out=xi[:, t0:t0 + NH], in0=wr[:], in1=wi[:], op=ALU.add), reads=[wr, wi], writes=[xi], accum=(hf > 0))
            prev = S_
        if j4 == 3:
            y = yo.next()
            for n in range(S5N):
                sl = slice(n * S5C, (n + 1) * S5C)
                ps = ps_rot.next()
                i = 0
                for jj in range(4):
                    for ri in range(2):
                        p.op("pe", lambda e: e.matmul(ps[:, 0:S5C], lhsT=WC[ri][4 * k + jj][:], rhs=xs[ri][jj][:, sl], start=(i == 0), stop=(i == 7)),
                             reads=[WC[ri][4 * k + jj], xs[ri][jj]], writes=[ps], accum=(i > 0))
                        i += 1
                p.op("dve", lambda e: e.scalar_tensor_tensor(out=y[:, sl], in0=u[:, k, sl], scalar=dsk[:, k:k + 1], in1=ps[:, 0:S5C],
                                                             op0=ALU.mult, op1=ALU.add), reads=[u, dsk, ps], writes=[y], accum=(n > 0))
            for hf in range(2):
                ta = sets[hf]["wr"]
                ysl = y[:, hf * NH:(hf + 1) * NH]
                p.op("pool", lambda e: e.tensor_tensor(out=ta[:], in0=ysl, in1=ysl, op=ALU.mult), reads=[y], writes=[ta])
                p.op("pool", lambda e: e.tensor_tensor(out=ta[:], in0=ta[:], in1=ysl, op=ALU.mult), reads=[y, ta], writes=[ta])
                p.op("dve", lambda e: e.scalar_tensor_tensor(out=ta[:], in0=ta[:], scalar=0.044715, in1=ysl, op0=ALU.mult, op1=ALU.add),
                     reads=[ta, y], writes=[ta])
                p.op("act", lambda e: e.activation(out=ta[:], in_=ta[:], func=AF.Sigmoid, scale=1.5957691216), reads=[ta], writes=[ta])
                p.op("dve", lambda e: e.tensor_tensor(out=ysl, in0=ysl, in1=ta[:], op=ALU.mult), reads=[y, ta], writes=[y], accum=(hf > 0))
            prev = None
            p.dma("sp", yT[k], y[:], reads=[y])
    return yo.tiles


def build_B():
    nc = bass.Bass("TRN2", target_bir_lowering=False)
    p = P(nc)
    outs = []
    for pre, fn in (("b1_", emit_B1), ("b2_", emit_B2), ("b3_", emit_B3)):
        p.prefix = pre
        m = p.mark()
        outs += fn(nc, p)
        p.release(m)
    p.finish(outs)
    return nc


NQT = 17
DH = 128
DV = 256


def build_B3():
    nc = bass.Bass("TRN2", target_bir_lowering=False)
    p = P(nc)
    outs = emit_B3(nc, p)
    p.finish(outs)
    return nc


def emit_B3(nc, p):
    qT_d = nc.dram_tensor("qT", [4, 128, L], F32, kind="ExternalInput").ap()
    kT_d = nc.dram_tensor("kT", [4, 128, L], F32, kind="ExternalInput").ap()
    v_d = nc.dram_tensor("v", [128, 2, NQT, DV], F32, kind="ExternalInput").ap()
    lam_d = nc.dram_tensor("lamv", [128, 4, DH], F32, kind="ExternalInput").ap()
    sw_d = nc.dram_tensor("subw", [128, DV], F32, kind="ExternalInput").ap()
    li_d = nc.dram_tensor("linit", [128, 2], F32, kind="ExternalInput").ap()
    oc = nc.dram_tensor("oc", [128, 2, NQT, DV], F32, kind="ExternalOutput").ap()

    qT = p.sb([128, 4, L], BF16, "qT_s")
    kT = p.sb([128, 4, L], BF16, "kT_s")
    V = p.sb([128, 2, NQT, DV + 1], BF16, "V")
    lamv = p.sb([128, 4, DH], F32, "lamv_s")
    subw = p.sb([128, DV], F32, "subw_s")
    linit = p.sb([128, 2], F32, "linit_s")
    epsc = p.sb([128, 1], F32, "epsc")
    p.op("pool", lambda e: e.memset(epsc[:], EPS), writes=[epsc])
    p.op("pool", lambda e: e.memset(V[:], 1.0), writes=[V])
    p.dma("pool", V[:, :, :, 0:DV], v_d[:, :, :, :], writes=[V])
    for i in range(4):
        p.dma("pool", qT[:, i, :], qT_d[i], writes=[qT], concurrent=True)
        p.dma("pool", kT[:, i, :], kT_d[i], writes=[kT], concurrent=True)
    p.dma("sp", lamv[:], lam_d[:, :, :], writes=[lamv])
    p.dma("sp", subw[:], sw_d[:, :], writes=[subw])
    p.dma("sp", linit[:], li_d[:, :], writes=[linit])
    lt = p.sb([128, 2, DH], F32, "lt")
    ls = p.sb([128, 2], F32, "ls")
    nlam = p.sb([128, 1], F32, "nlam")
    p.op("dve", lambda e: e.tensor_tensor(out=lt[:, 0, :], in0=lamv[:, 0, :], in1=lamv[:, 1, :], op=ALU.mult), reads=[lamv], writes=[lt])
    p.op("dve", lambda e: e.tensor_tensor(out=lt[:, 1, :], in0=lamv[:, 2, :], in1=lamv[:, 3, :], op=ALU.mult), reads=[lamv], writes=[lt], accum=True)
    p.op("dve", lambda e: e.tensor_reduce(out=ls[:], in_=lt[:], axis=AX.X, op=ALU.add), reads=[lt], writes=[ls])
    p.op("act", lambda e: e.activation(out=ls[:], in_=ls[:], func=AF.Exp), reads=[ls], writes=[ls])
    p.op("dve", lambda e: e.tensor_tensor(out=nlam[:], in0=ls[:, 1:2], in1=ls[:, 0:1], op=ALU.subtract), reads=[ls], writes=[nlam])
    p.op("dve", lambda e: e.tensor_tensor(out=nlam[:], in0=nlam[:], in1=linit[:, 0:1], op=ALU.subtract), reads=[nlam, linit], writes=[nlam])

    acc = [p.sb([128, NQT, DV], F32, f"acc{h}") for h in range(2)]
    psS = Rot([p.ps([128, 512], F32, f"psS{i}") for i in range(3)])
    psO = [p.ps([128, 512], F32, f"psO{i}") for i in range(4)]
    PT = Rot([p.sb([128, 512], BF16, f"PT{i}") for i in range(3)])
    rl = Rot([p.sb([128, 1], F32, f"rl{i}") for i in range(4)])
    on = Rot([p.sb([128, DV], F32, f"on{i}") for i in range(2)])
    scale = DH ** -0.5

    def tok0(i):
        return (0, 16) if i == 0 else (16 + 128 * (i - 1), 128)

    def finish_tile(h, s, i, po, nq):
        r = rl.next()
        p.op("dve", lambda e: e.reciprocal(out=r[0:nq, :], in_=po[0:nq, DV:DV + 1]), reads=[po], writes=[r])
        if s == 0:
            p.op("act", lambda e: e.activation(out=acc[h][0:nq, i, :], in_=po[0:nq, 0:DV], func=AF.Copy, scale=r[0:nq, 0:1]),
                 reads=[po, r], writes=[acc[h]], accum=True)
        else:
            o_ = on.next()
            p.op("act", lambda e: e.activation(out=o_[0:nq, :], in_=po[0:nq, 0:DV], func=AF.Copy, scale=r[0:nq, 0:1]),
                 reads=[po, r], writes=[o_])
            p.op("dve", lambda e: e.scalar_tensor_tensor(out=acc[h][0:nq, i, :], in0=o_[0:nq, :], scalar=nlam[0:nq, 0:1],
                                                         in1=acc[h][0:nq, i, :], op0=ALU.mult, op1=ALU.add),
                 reads=[o_, nlam, acc[h]], writes=[acc[h]])

    for h in range(2):
        p.op("pool", lambda e: e.memset(acc[h][:], 0.0), writes=[acc[h]])
    for h in range(2):
        for s in range(2):
            hs = 2 * h + s
            ps = psS.next()
            p.op("pe", lambda e: e.matmul(ps[0:16, 0:16], lhsT=kT[:, hs, 0:16], rhs=qT[:, hs, 0:16], start=True, stop=True),
                 reads=[kT, qT], writes=[ps])
            pt = PT.next()
            p.op("act", lambda e: e.activation(out=pt[0:16, 0:16], in_=ps[0:16, 0:16], func=AF.Exp, scale=scale), reads=[ps], writes=[pt])
            po = psO[0]
            p.op("pe", lambda e: e.matmul(po[0:16, 0:DV + 1], lhsT=pt[0:16, 0:16], rhs=V[0:16, h, 0, :], start=True, stop=True),
                 reads=[pt, V], writes=[po])
            finish_tile(h, s, 0, po, 16)
            for g in range(4):
                q0 = 16 + 512 * g
                tiles = [4 * g + 1 + a for a in range(4)]
                for j in range(0, 4 * g + 5):
                    k0, nk = tok0(j)
                    ps = psS.next()
                    p.op("pe", lambda e: e.matmul(ps[0:nk, 0:512], lhsT=kT[:, hs, k0:k0 + nk], rhs=qT[:, hs, q0:q0 + 512], start=True, stop=True),
                         reads=[kT, qT], writes=[ps])
                    pt = PT.next()
                    p.op("act", lambda e: e.activation(out=pt[0:nk, :], in_=ps[0:nk, 0:512], func=AF.Exp, scale=scale), reads=[ps], writes=[pt])
                    if j in tiles:
                        a = tiles.index(j)
                        p.op("pool", lambda e: e.memset(pt[64:128, 128 * a:128 * a + 64], 0.0), reads=[pt], writes=[pt])
                    for a, i in enumerate(tiles):
                        if i < j:
                            continue
                        p.op("pe", lambda e: e.matmul(psO[a][:, 0:DV + 1], lhsT=pt[0:nk, 128 * a:128 * (a + 1)], rhs=V[0:nk, h, j, :],
                                                      start=(j == 0), stop=(j == i)), reads=[pt, V], writes=[psO[a]], accum=(j > 0))
                        if j == i:
                            finish_tile(h, s, i, psO[a], 128)
    sq = p.sb([128, NQT, DV], F32, "sq")
    ms = p.sb([128, NQT], F32, "ms")
    for h in range(2):
        a = acc[h]
        p.op("pool", lambda e: e.tensor_tensor(out=sq[:], in0=a[:], in1=a[:], op=ALU.mult), reads=[a], writes=[sq])
        p.op("dve", lambda e: e.tensor_reduce(out=ms[:], in_=sq[:], axis=AX.X, op=ALU.add), reads=[sq], writes=[ms])
        p.op("act", lambda e: e.activation(out=ms[:], in_=ms[:], func=AF.Sqrt, bias=epsc[:, 0:1], scale=1.0 / DV), reads=[ms, epsc], writes=[ms])
        p.op("dve", lambda e: e.reciprocal(out=ms[:], in_=ms[:]), reads=[ms], writes=[ms])
        p.op("dve", lambda e: e.tensor_scalar(out=ms[:], in0=ms[:], scalar1=linit[:, 1:2], scalar2=None, op0=ALU.mult), reads=[ms, linit], writes=[ms])
        p.op("dve", lambda e: e.tensor_tensor(out=a[:], in0=a[:], in1=ms[:].unsqueeze(2).to_broadcast([128, NQT, DV]), op=ALU.mult),
             reads=[a, ms], writes=[a])
        p.op("pool", lambda e: e.tensor_tensor(out=a[:], in0=a[:], in1=subw[:].unsqueeze(1).to_broadcast([128, NQT, DV]), op=ALU.mult),
             reads=[a, subw], writes=[a])
        p.dma("sp", oc[:, h, :, :], a[:], reads=[a])
    return acc


NTP = 1152
NTT = NTP // 128
TCP = NTP // 3
NE = 64
NR = 72
BIGNEG = -1.0e30


def build_C1(gather=True):
    nc = bass.Bass("TRN2", target_bir_lowering=False)
    p = P(nc)
    zT = nc.dram_tensor("zT", [D, NT], F32, kind="ExternalInput").ap()
    oaT = nc.dram_tensor("oaT", [512, NT], F32, kind="ExternalInput").ap()
    ysT = nc.dram_tensor("ysT", [512, NT], F32, kind="ExternalInput").ap()
    ocT = nc.dram_tensor("ocT", [1024, NT], F32, kind="ExternalInput").ap()
    wglu_d = nc.dram_tensor("w_glu", [512, 1024], F32, kind="ExternalInput").ap()
    bglu_d = nc.dram_tensor("b_glu", [128, 8], F32, kind="ExternalInput").ap()
    wout_d = nc.dram_tensor("w_out", [D, D], F32, kind="ExternalInput").ap()
    ln2_d = nc.dram_tensor("ln2", [128, KC], F32, kind="ExternalInput").ap()
    wr_d = nc.dram_tensor("w_r", [D, NR], F32, kind="ExternalInput").ap()
    br_d = nc.dram_tensor("b_r", [128, NR], F32, kind="ExternalInput").ap()
    z1T = nc.dram_tensor("z1T", [D, NT], F32, kind="ExternalOutput").ap()
    if gather:
        xe_d = nc.dram_tensor("xeT", [NE, D, NSL], BF16, kind="ExternalOutput").ap()
    else:
        xtm_d = nc.dram_tensor("xtm", [128, NTT, D], BF16, kind="ExternalOutput").ap()
        rankp_d = nc.dram_tensor("rankp", [128, NTT, NE], F32, kind="ExternalOutput").ap()
    slotT_d = nc.dram_tensor("slotT", [NE, NTP], F32, kind="ExternalOutput").ap()
    gtT_d = nc.dram_tensor("gtT", [NE, NTP], F32, kind="ExternalOutput").ap()

    xtm = p.sb([128, NTT, D], BF16, "xtm_s")
    rankp = p.sb([128, NTT, NE], F32, "rankp_s")
    slotT = p.sb([NE, NTP], F32, "slotT_s")
    gtT = p.sb([NE, NTP], F32, "gtT_s")
    iota_s = p.sb([128, 128], F32, "iota_s")
    ps_rot = Rot([p.ps([128, 512], F32, f"ps{i}") for i in range(6)])
    psb_rot = Rot([p.ps([128, 512], BF16, f"psb{i}") for i in range(2)])
    mk = p.mark()
    z = p.sb([128, KC, NTP], F32, "z")
    mx = p.sb([128, KC, NTP], BF16, "mx")
    bglu = p.sb([128, 8], F32, "bglu_s")
    ln2 = p.sb([128, KC], F32, "ln2_s")
    wr = p.sb([128, KC, NR], F32, "wr_s")
    br = p.sb([128, NR], F32, "br_s")
    rstd = p.sb([128, NTP], F32, "rstd")
    ones_bf = p.sb([128, 128], BF16, "ones_bf")
    onesD = p.sb([128, 128], BF16, "onesD")
    epsc = p.sb([128, 1], F32, "epsc")
    identf = p.sb([128, 128], F32, "identf")
    identb = p.sb([128, 128], BF16, "identb")
    ustr = p.sb([128, 128], BF16, "ustr")
    vmask = p.sb([128, NTT], F32, "vmask")
    evac = Evac(p)
    ys = p.sb([128, 4, NTP], BF16, "ys")
    wg = p.sb([128, 4, 1024], BF16, "wg")
    sg_rot = Rot([p.sb([128, TCP], F32, f"sg{i}") for i in range(2)])
    wb_rot = Rot([p.sb([128, KC, 512], BF16, f"wb{i}") for i in range(1)])

    p.op("pool", lambda e: e.memset(z[:, :, NT:NTP], 0.0), writes=[z])
    p.op("pool", lambda e: e.memset(mx[:, :, NT:NTP], 0.0), writes=[mx])
    p.op("pool", lambda e: e.memset(ys[:, :, NT:NTP], 0.0), writes=[ys])
    zv = zT.rearrange("(c p) t -> p c t", p=128)
    for q in range(4):
        p.dma("sp", z[:, 4 * q:4 * q + 4, 0:NT], zv[:, 4 * q:4 * q + 4, :], writes=[z], concurrent=True)
    p.dma("pool", ys[:, :, 0:NT], ysT.rearrange("(c p) t -> p c t", p=128), writes=[ys])
    p.dma("pool", wg[:], wglu_d.rearrange("(c p) n -> p c n", p=128), writes=[wg])
    p.dma("pool", mx[:, 0:4, 0:NT], oaT.rearrange("(c p) t -> p c t", p=128), writes=[mx], concurrent=True)
    p.dma("pool", mx[:, 8:16, 0:NT], ocT.rearrange("(c p) t -> p c t", p=128), writes=[mx], concurrent=True)
    for t_, d_ in ((bglu, bglu_d), (ln2, ln2_d), (br, br_d)):
        p.dma("sp", t_[:], d_[:, :], writes=[t_])
    p.dma("sp", wr[:], wr_d.rearrange("(c p) n -> p c n", p=128), writes=[wr])
    p.op("pool", lambda e: e.memset(ones_bf[:], 1.0), writes=[ones_bf])
    p.op("pool", lambda e: e.memset(onesD[:], 1.0 / D), writes=[onesD])
    p.op("pool", lambda e: e.memset(epsc[:], EPS), writes=[epsc])
    p.op("pool", lambda e: e.memset(identf[:], 0.0), writes=[identf])
    p.op("pool", lambda e: e.affine_select(out=identf[:], in_=identf[:], pattern=[[-1, 128]], compare_op=ALU.not_equal,
                                           fill=1.0, base=0, channel_multiplier=1), reads=[identf], writes=[identf])
    p.op("pool", lambda e: e.tensor_copy(out=identb[:], in_=identf[:]), reads=[identf], writes=[identb])
    p.op("pool", lambda e: e.memset(ustr[:], 1.0), writes=[ustr])
    p.op("pool", lambda e: e.affine_select(out=ustr[:], in_=ustr[:], pattern=[[1, 128]], compare_op=ALU.is_ge, fill=0.0,
                                           base=-1, channel_multiplier=-1), reads=[ustr], writes=[ustr])
    p.op("pool", lambda e: e.memset(vmask[:], 1.0), writes=[vmask])
    p.op("pool", lambda e: e.affine_select(out=vmask[:], in_=vmask[:], pattern=[[-128, NTT]], compare_op=ALU.is_ge, fill=0.0,
                                           base=NT - 1, channel_multiplier=-1), reads=[vmask], writes=[vmask])

    for m in range(4):
        for n in range(3):
            sl = slice(n * TCP, (n + 1) * TCP)
            pv, pg = ps_rot.next(), ps_rot.next()
            for c in range(4):
                p.op("pe", lambda e: e.matmul(pv[:, 0:TCP], lhsT=wg[:, c, m * 128:(m + 1) * 128], rhs=ys[:, c, sl], start=(c == 0), stop=(c == 3)),
                     reads=[wg, ys], writes=[pv], accum=(c > 0))
            for c in range(4):
                p.op("pe", lambda e: e.matmul(pg[:, 0:TCP], lhsT=wg[:, c, 512 + m * 128:512 + (m + 1) * 128], rhs=ys[:, c, sl], start=(c == 0), stop=(c == 3)),
                     reads=[wg, ys], writes=[pg], accum=(c > 0))
            sg = sg_rot.next()
            p.op("act", lambda e: e.activation(out=sg[:], in_=pg[:, 0:TCP], func=AF.Sigmoid, bias=bglu[:, 4 + m:5 + m]), reads=[pg, bglu], writes=[sg])
            p.op("dve", lambda e: e.scalar_tensor_tensor(out=mx[:, 4 + m, sl], in0=pv[:, 0:TCP], scalar=bglu[:, m:m + 1], in1=sg[:],
                                                         op0=ALU.add, op1=ALU.mult), reads=[pv, bglu, sg], writes=[mx], accum=True)
    wo_v = wout_d.rearrange("(c p) n -> p c n", p=128)
    for g in range(4):
        wb = wb_rot.next()
        p.dma("pool", wb[:], wo_v[:, :, g * 512:(g + 1) * 512], writes=[wb])
        for j in range(4):
            ct = g * 4 + j
            for n in range(3):
                sl = slice(n * TCP, (n + 1) * TCP)
                ps = ps_rot.next()
                for c in range(KC):
                    p.op("pe", lambda e: e.matmul(ps[:, 0:TCP], lhsT=wb[:, c, j * 128:(j + 1) * 128], rhs=mx[:, c, sl], start=(c == 0), stop=(c == KC - 1)),
                         reads=[wb, mx], writes=[ps], accum=(c > 0))
                p.op("dve", lambda e: e.tensor_tensor(out=z[:, ct, sl], in0=z[:, ct, sl], in1=ps[:, 0:TCP], op=ALU.add), reads=[z, ps], writes=[z])
    z1v = z1T.rearrange("(c p) t -> p c t", p=128)
    for q in range(4):
        p.dma("sp", z1v[:, 4 * q:4 * q + 4, :], z[:, 4 * q:4 * q + 4, 0:NT], reads=[z])
    p.barrier()
    p.free([ys, wg] + sg_rot.tiles + wb_rot.tiles)
    hn2 = mx
    sq_rot = Rot([p.sb([128, NTP], BF16, f"sq{i}") for i in range(2)])
    emit_rmsnorm(p, z, ln2, hn2, onesD, epsc, sq_rot, ps_rot, rstd, nt=NTP, tch=TCP)
    for i in range(NTT):
        for cq in range(4):
            pb = psb_rot.next()
            for cc in range(4):
                c = cq * 4 + cc
                p.op("pe", lambda e: e.transpose(out=pb[:, cc * 128:(cc + 1) * 128], in_=hn2[:, c, i * 128:(i + 1) * 128], identity=identb[:]),
                     reads=[hn2, identb], writes=[pb], accum=(cc > 0))
            evac(xtm[:, i, cq * 512:(cq + 1) * 512], pb[:, 0:512], [pb], [xtm], first=False)
    if not gather:
        p.dma("sp", xtm_d[:, :, :], xtm[:], reads=[xtm])
    p.op("dve", lambda e: e.tensor_tensor(out=wr[:], in0=wr[:], in1=ln2[:].unsqueeze(2).to_broadcast([128, KC, NR]), op=ALU.mult),
         reads=[wr, ln2], writes=[wr])
    rstd_tm = p.sb([128, NTT], F32, "rstd_tm")
    lg = p.sb([128, NTT, NR], F32, "lg")
    for i in range(NTT):
        pt = ps_rot.next()
        p.op("pe", lambda e: e.transpose(out=pt[:, 0:128], in_=rstd[:, i * 128:(i + 1) * 128], identity=identf[:]), reads=[rstd, identf], writes=[pt])
        p.op("act", lambda e: e.activation(out=rstd_tm[:, i:i + 1], in_=pt[:, 0:1], func=AF.Copy), reads=[pt], writes=[rstd_tm], accum=True)
        ps = ps_rot.next()
        for c in range(KC):
            p.op("pe", lambda e: e.matmul(ps[:, 0:NR], lhsT=z[:, c, i * 128:(i + 1) * 128], rhs=wr[:, c, :], start=(c == 0), stop=(c == KC - 1)),
                 reads=[z, wr], writes=[ps], accum=(c > 0))
        p.op("dve", lambda e: e.scalar_tensor_tensor(out=lg[:, i, :], in0=ps[:, 0:NR], scalar=rstd_tm[:, i:i + 1], in1=br[:],
                                                     op0=ALU.mult, op1=ALU.add), reads=[ps, rstd_tm, br], writes=[lg], accum=True)

    def sbf(name, shape):
        return p.sb(list(shape), F32, name)

    def tt(out_ap, in0_ap, in1_ap, op, reads, writes, eng="dve"):
        p.op(eng, lambda e: e.tensor_tensor(out=out_ap, in0=in0_ap, in1=in1_ap, op=op), reads=reads, writes=writes)

    def red(out, in_ap, op, reads):
        p.op("dve", lambda e: e.tensor_reduce(out=out[:], in_=in_ap, axis=AX.X, op=op), reads=reads, writes=[out])

    gl = lg[:, :, 0:8]
    gmax, gsum, pg_ = sbf("gmax", (128, NTT)), sbf("gsum", (128, NTT)), sbf("pgrp", (128, NTT))
    ge, oh = sbf("ge", (128, NTT, 8)), sbf("oh", (128, NTT, 8))
    red(gmax, gl, ALU.max, [lg])
    b8 = lambda t_: t_[:].unsqueeze(2).to_broadcast([128, NTT, 8])
    tt(ge[:], gl, b8(gmax), ALU.subtract, [lg, gmax], [ge])
    tt(oh[:], gl, b8(gmax), ALU.is_equal, [lg, gmax], [oh])
    p.op("act", lambda e: e.activation(out=ge[:], in_=ge[:], func=AF.Exp), reads=[ge], writes=[ge])
    red(gsum, ge[:], ALU.add, [ge])
    p.op("dve", lambda e: e.reciprocal(out=pg_[:], in_=gsum[:]), reads=[gsum], writes=[pg_])
    el4 = sbf("el4", (128, NTT, 8, 8))
    esel = sbf("esel", (128, NTT, 8))
    lg4 = lg[:, :, 8:NR].rearrange("p t (g j) -> p t g j", j=8)
    tt(el4[:], lg4, oh[:].unsqueeze(3).to_broadcast([128, NTT, 8, 8]), ALU.mult, [lg, oh], [el4])
    red(esel, el4[:].rearrange("p t g j -> p t j g"), ALU.add, [el4])
    m1, m2 = sbf("m1", (128, NTT)), sbf("m2", (128, NTT))
    k1, k2, e2 = sbf("k1", (128, NTT, 8)), sbf("k2", (128, NTT, 8)), sbf("e2", (128, NTT, 8))
    red(m1, esel[:], ALU.max, [esel])
    tt(k1[:], esel[:], b8(m1), ALU.is_equal, [esel, m1], [k1])
    p.op("dve", lambda e: e.scalar_tensor_tensor(out=e2[:], in0=k1[:], scalar=BIGNEG, in1=esel[:], op0=ALU.mult, op1=ALU.add),
         reads=[k1, esel], writes=[e2])
    red(m2, e2[:], ALU.max, [e2])
    tt(k2[:], e2[:], b8(m2), ALU.is_equal, [e2, m2], [k2])
    dd, g1, g2 = sbf("dd", (128, NTT)), sbf("g1", (128, NTT)), sbf("g2", (128, NTT))
    tt(dd[:], m2[:], m1[:], ALU.subtract, [m2, m1], [dd])
    p.op("act", lambda e: e.activation(out=dd[:], in_=dd[:], func=AF.Exp), reads=[dd], writes=[dd])
    p.op("dve", lambda e: e.tensor_scalar(out=dd[:], in0=dd[:], scalar1=1.0, scalar2=None, op0=ALU.add), reads=[dd], writes=[dd])
    p.op("dve", lambda e: e.reciprocal(out=dd[:], in_=dd[:]), reads=[dd], writes=[dd])
    tt(g1[:], pg_[:], dd[:], ALU.mult, [pg_, dd], [g1])
    tt(g2[:], pg_[:], g1[:], ALU.subtract, [pg_, g1], [g2])
    tt(oh[:], oh[:], b8(vmask), ALU.mult, [oh, vmask], [oh])
    gj, aj = sbf("gj", (128, NTT, 8)), sbf("aj", (128, NTT, 8))
    tt(gj[:], k1[:], b8(g1), ALU.mult, [k1, g1], [gj])
    tt(e2[:], k2[:], b8(g2), ALU.mult, [k2, g2], [e2])
    tt(gj[:], gj[:], e2[:], ALU.add, [gj, e2], [gj])
    tt(aj[:], k1[:], k2[:], ALU.add, [k1, k2], [aj])
    Gt, Af = sbf("Gt", (128, NTT, 8, 8)), sbf("Af", (128, NTT, 8, 8))
    Ab = p.sb([128, NTT, NE], BF16, "Ab")
    ohb = oh[:].unsqueeze(3).to_broadcast([128, NTT, 8, 8])
    tt(Gt[:], ohb, gj[:].unsqueeze(2).to_broadcast([128, NTT, 8, 8]), ALU.mult, [oh, gj], [Gt])
    tt(Af[:], ohb, aj[:].unsqueeze(2).to_broadcast([128, NTT, 8, 8]), ALU.mult, [oh, aj], [Af])
    Af2 = Af[:].rearrange("p t g j -> p t (g j)")
    Gt2 = Gt[:].rearrange("p t g j -> p t (g j)")
    p.op("dve", lambda e: e.tensor_copy(out=Ab[:], in_=Af2), reads=[Af], writes=[Ab])
    AT = sbf("AT_s", (NE, NTP))
    for i in range(NTT):
        ps = ps_rot.next()
        for i2 in range(i + 1):
            p.op("pe", lambda e: e.matmul(ps[:, 0:NE], lhsT=(ustr[:] if i2 == i else ones_bf[:]), rhs=Ab[:, i2, :], start=(i2 == 0), stop=(i2 == i)),
                 reads=[ustr, ones_bf, Ab], writes=[ps], accum=(i2 > 0))
        p.op("dve", lambda e: e.scalar_tensor_tensor(out=rankp[:, i, :], in0=ps[:, 0:NE], scalar=1.0, in1=Af2[:, i, :], op0=ALU.add, op1=ALU.mult),
             reads=[ps, Af], writes=[rankp], accum=True)
        ps2 = ps_rot.next()
        for i2 in range(i + 1):
            p.op("pe", lambda e: e.matmul(ps2[0:NE, 0:128], lhsT=Ab[:, i2, :], rhs=(ustr[:] if i2 == i else ones_bf[:]), start=(i2 == 0), stop=(i2 == i)),
                 reads=[ustr, ones_bf, Ab], writes=[ps2], accum=(i2 > 0))
        p.op("act", lambda e: e.activation(out=slotT[:, i * 128:(i + 1) * 128], in_=ps2[0:NE, 0:128], func=AF.Copy), reads=[ps2], writes=[slotT], accum=True)
        ps3 = ps_rot.next()
        p.op("pe", lambda e: e.transpose(out=ps3[0:NE, 0:128], in_=Af2[:, i, :], identity=identf[:]), reads=[Af, identf], writes=[ps3])
        p.op("act", lambda e: e.activation(out=AT[:, i * 128:(i + 1) * 128], in_=ps3[0:NE, 0:128], func=AF.Copy), reads=[ps3], writes=[AT], accum=True)
        ps4 = ps_rot.next()
        p.op("pe", lambda e: e.transpose(out=ps4[0:NE, 0:128], in_=Gt2[:, i, :], identity=identf[:]), reads=[Gt, identf], writes=[ps4])
        p.op("act", lambda e: e.activation(out=gtT[:, i * 128:(i + 1) * 128], in_=ps4[0:NE, 0:128], func=AF.Copy), reads=[ps4], writes=[gtT], accum=True)
    p.op("dve", lambda e: e.tensor_scalar(out=rankp[:], in0=rankp[:], scalar1=-1.0, scalar2=None, op0=ALU.add), reads=[rankp], writes=[rankp])
    p.op("dve", lambda e: e.scalar_tensor_tensor(out=slotT[:], in0=slotT[:], scalar=1.0, in1=AT[:], op0=ALU.add, op1=ALU.mult),
         reads=[slotT, AT], writes=[slotT])
    p.op("dve", lambda e: e.tensor_scalar(out=slotT[:], in0=slotT[:], scalar1=-1.0, scalar2=None, op0=ALU.add), reads=[slotT], writes=[slotT])
    p.dma("sp", slotT_d[:, :], slotT[:], reads=[slotT])
    p.dma("sp", gtT_d[:, :], gtT[:], reads=[gtT])
    if not gather:
        p.dma("sp", rankp_d[:, :, :], rankp[:], reads=[rankp])
        p.finish([z, xtm, rankp, slotT, gtT])
        return nc
    p.release(mk)
    p.op("pool", lambda e: e.iota(iota_s[:], pattern=[[1, 128]], base=0, channel_multiplier=0, allow_small_or_imprecise_dtypes=True), writes=[iota_s])
    S_rot = Rot([p.sb([128, NTT, 4, 128], BF16, f"S{i}") for i in range(2)])
    XeT_rot = Rot([p.sb([128, KC, 512], BF16, f"XeT{i}") for i in range(2)])
    for qd in range(NE // 4):
        e0 = 4 * qd
        S = S_rot.next()
        p.op("dve", lambda e: e.tensor_tensor(out=S[:], in0=iota_s[:].unsqueeze(1).unsqueeze(1).to_broadcast([128, NTT, 4, 128]),
                                              in1=rankp[:, :, e0:e0 + 4].unsqueeze(3).to_broadcast([128, NTT, 4, 128]), op=ALU.is_equal),
             reads=[iota_s, rankp], writes=[S])
        S2 = S[:].rearrange("p t e s -> p t (e s)")
        XeT = XeT_rot.next()
        for c in range(KC):
            ps = ps_rot.next()
            for i in range(NTT):
                p.op("pe", lambda e: e.matmul(ps[:, 0:512], lhsT=xtm[:, i, c * 128:(c + 1) * 128], rhs=S2[:, i, :], start=(i == 0), stop=(i == NTT - 1)),
                     reads=[xtm, S], writes=[ps], accum=(i > 0))
            evac(XeT[:, c, :], ps[:, 0:512], [ps], [XeT], first=(c == 0))
        for ee in range(4):
            p.dma("sp", xe_d[e0 + ee].rearrange("(c p) s -> p c s", p=128), XeT[:, :, ee * 128:(ee + 1) * 128], reads=[XeT])
    p.finish([z, slotT, gtT] + XeT_rot.tiles)
    return nc


DF = 512
SC = NT // 3


NSL = 128
NSRC = 8
ESL = NSRC * NSL
EPC = NE // 8


def build_C2a():
    nc = bass.Bass("TRN2", target_bir_lowering=False)
    p = P(nc)
    xtm_d = nc.dram_tensor("xtm", [128, NTT, D], BF16, kind="ExternalInput").ap()
    rankp_d = nc.dram_tensor("rankp", [128, NTT, NE], F32, kind="ExternalInput").ap()
    xe_d = nc.dram_tensor("xeT", [NE, D, NSL], BF16, kind="ExternalOutput").ap()
    X = p.sb([128, NTT, D], BF16, "X")
    rankp = p.sb([128, NTT, NE], F32, "rankp_s")
    iota_s = p.sb([128, 128], F32, "iota_s")
    p.dma("sp", X[:], xtm_d[:, :, :], writes=[X])
    p.dma("sp", rankp[:], rankp_d[:, :, :], writes=[rankp])
    p.op("pool", lambda e: e.iota(iota_s[:], pattern=[[1, 128]], base=0, channel_multiplier=0, allow_small_or_imprecise_dtypes=True), writes=[iota_s])
    S_rot = Rot([p.sb([128, NTT, 4, 128], BF16, f"S{i}") for i in range(2)])
    XeT_rot = Rot([p.sb([128, KC, 512], BF16, f"XeT{i}") for i in range(2)])
    ps_rot = Rot([p.ps([128, 512], F32, f"ps{i}") for i in range(8)])
    evac = Evac(p)
    for qd in range(NE // 4):
        e0 = 4 * qd
        S = S_rot.next()
        p.op("dve", lambda e: e.tensor_tensor(out=S[:], in0=iota_s[:].unsqueeze(1).unsqueeze(1).to_broadcast([128, NTT, 4, 128]),
                                              in1=rankp[:, :, e0:e0 + 4].unsqueeze(3).to_broadcast([128, NTT, 4, 128]), op=ALU.is_equal),
             reads=[iota_s, rankp], writes=[S])
        S2 = S[:].rearrange("p t e s -> p t (e s)")
        XeT = XeT_rot.next()
        for c in range(KC):
            ps = ps_rot.next()
            for i in range(NTT):
                p.op("pe", lambda e: e.matmul(ps[:, 0:512], lhsT=X[:, i, c * 128:(c + 1) * 128], rhs=S2[:, i, :], start=(i == 0), stop=(i == NTT - 1)),
                     reads=[X, S], writes=[ps], accum=(i > 0))
            evac(XeT[:, c, :], ps[:, 0:512], [ps], [XeT], first=(c == 0))
        for ee in range(4):
            p.dma("sp", xe_d[e0 + ee].rearrange("(c p) s -> p c s", p=128), XeT[:, :, ee * 128:(ee + 1) * 128], reads=[XeT])
    p.finish(XeT_rot.tiles)
    return nc


def build_C2b():
    nc = bass.Bass("TRN2", target_bir_lowering=False)
    p = P(nc)
    xe_d = nc.dram_tensor("xe", [EPC, D, ESL], BF16, kind="ExternalInput").ap()
    w1_d = nc.dram_tensor("w1", [EPC, D, DF], F32, kind="ExternalInput").ap()
    w3_d = nc.dram_tensor("w3", [EPC, D, DF], F32, kind="ExternalInput").ap()
    w2_d = nc.dram_tensor("w2", [EPC, DF, D], F32, kind="ExternalInput").ap()
    ye_d = nc.dram_tensor("ye", [EPC, ESL, D], BF16, kind="ExternalOutput").ap()
    W1 = Rot([p.sb([128, KC, DF], BF16, f"W1_{i}") for i in range(2)])
    W3 = Rot([p.sb([128, KC, DF], BF16, f"W3_{i}") for i in range(2)])
    W2 = Rot([p.sb([128, 4, D], BF16, f"W2_{i}") for i in range(2)])
    Xe = Rot([p.sb([128, KC, ESL], BF16, f"Xe{i}") for i in range(2)])
    Hg = Rot([p.sb([128, 4, 512], BF16, f"Hg{i}") for i in range(2)])
    h1 = Rot([p.sb([128, 512], F32, f"h1_{i}") for i in range(2)])
    yo = Rot([p.sb([128, D], BF16, f"yo{i}") for i in range(3)])
    ps_rot = Rot([p.ps([128, 512], F32, f"ps{i}") for i in range(8)])
    evac = Evac(p)
    for ex in range(EPC):
        w1, w3, w2, xe = W1.next(), W3.next(), W2.next(), Xe.next()
        p.dma("sp", xe[:], xe_d[ex].rearrange("(c p) s -> p c s", p=128), writes=[xe])
        p.dma("pool", w1[:], w1_d[ex].rearrange("(c p) f -> p c f", p=128), writes=[w1])
        p.dma("pool", w3[:], w3_d[ex].rearrange("(c p) f -> p c f", p=128), writes=[w3])
        p.dma("pool", w2[:], w2_d[ex].rearrange("(m p) d -> p m d", p=128), writes=[w2])
        for half in range(ESL // 512):
            hs = slice(half * 512, (half + 1) * 512)
            hg = Hg.next()
            for m in range(4):
                p1, p3 = ps_rot.next(), ps_rot.next()
                for c in range(KC):
                    p.op("pe", lambda e: e.matmul(p1[:, 0:512], lhsT=w1[:, c, m * 128:(m + 1) * 128], rhs=xe[:, c, hs], start=(c == 0), stop=(c == KC - 1)),
                         reads=[w1, xe], writes=[p1], accum=(c > 0))
                for c in range(KC):
                    p.op("pe", lambda e: e.matmul(p3[:, 0:512], lhsT=w3[:, c, m * 128:(m + 1) * 128], rhs=xe[:, c, hs], start=(c == 0), stop=(c == KC - 1)),
                         reads=[w3, xe], writes=[p3], accum=(c > 0))
                h = h1.next()
                p.op("act", lambda e: e.activation(out=h[:], in_=p1[:, 0:512], func=AF.Silu), reads=[p1], writes=[h])
                p.op("dve", lambda e: e.tensor_tensor(out=hg[:, m, :], in0=h[:], in1=p3[:, 0:512], op=ALU.mult), reads=[h, p3], writes=[hg], accum=(m > 0))
            for st in range(4):
                y = yo.next()
                for n in range(4):
                    ps = ps_rot.next()
                    for m in range(4):
                        p.op("pe", lambda e: e.matmul(ps[:, 0:512], lhsT=hg[:, m, st * 128:(st + 1) * 128], rhs=w2[:, m, n * 512:(n + 1) * 512],
                                                      start=(m == 0), stop=(m == 3)), reads=[hg, w2], writes=[ps], accum=(m > 0))
                    evac(y[:, n * 512:(n + 1) * 512], ps[:, 0:512], [ps], [y], first=(n == 0))
                r0 = half * 512 + st * 128
                p.dma("sp", ye_d[ex, r0:r0 + 128, :], y[:], reads=[y])
    p.finish(yo.tiles)
    return nc


def build_C2c(last):
    nc = bass.Bass("TRN2", target_bir_lowering=False)
    p = P(nc)
    zT = nc.dram_tensor("zT", [D, NT], F32, kind="ExternalInput").ap()
    slotT_d = nc.dram_tensor("slotT", [NE, NTP], F32, kind="ExternalInput").ap()
    gtT_d = nc.dram_tensor("gtT", [NE, NTP], F32, kind="ExternalInput").ap()
    ye_d = nc.dram_tensor("ye", [NE, NSL, D], BF16, kind="ExternalInput").ap()
    fw_d = nc.dram_tensor("fnw", [128, KC], F32, kind="ExternalInput").ap()
    outT = nc.dram_tensor("outT", [D, NT], F32, kind="ExternalOutput").ap()
    if not last:
        lnw_d = nc.dram_tensor("lnw", [128, KC], F32, kind="ExternalInput").ap()
        w_in = nc.dram_tensor("w_in", [D, INW], F32, kind="ExternalInput").ap()
        projT = nc.dram_tensor("projT", [INW, NT], F32, kind="ExternalOutput").ap()
    z = p.sb([128, KC, NT], F32, "z")
    slotT = p.sb([NE, NTP], BF16, "slotT_s")
    gtT = p.sb([NE, NTP], BF16, "gtT_s")
    fnw = p.sb([128, KC], F32, "fnw_s")
    identb = p.sb([128, 128], BF16, "identb")
    identf = p.sb([128, 128], F32, "identf")
    iota_p = p.sb([128, 1], F32, "iota_p")
    zv = zT.rearrange("(c p) t -> p c t", p=128)
    for q in range(4):
        p.dma("sp", z[:, 4 * q:4 * q + 4, :], zv[:, 4 * q:4 * q + 4, :], writes=[z], concurrent=True)
    p.dma("sp", fnw[:], fw_d[:, :], writes=[fnw])
    p.dma("pool", slotT[:], slotT_d[:, :], writes=[slotT])
    p.dma("pool", gtT[:], gtT_d[:, :], writes=[gtT])
    p.op("pool", lambda e: e.memset(identf[:], 0.0), writes=[identf])
    p.op("pool", lambda e: e.affine_select(out=identf[:], in_=identf[:], pattern=[[-1, 128]], compare_op=ALU.not_equal,
                                           fill=1.0, base=0, channel_multiplier=1), reads=[identf], writes=[identf])
    p.op("pool", lambda e: e.tensor_copy(out=identb[:], in_=identf[:]), reads=[identf], writes=[identb])
    p.op("pool", lambda e: e.iota(iota_p[:], pattern=[[0, 1]], base=0, channel_multiplier=1, allow_small_or_imprecise_dtypes=True), writes=[iota_p])
    ps_rot = Rot([p.ps([128, 512], F32, f"ps{i}") for i in range(8)])
    mk = p.mark()
    NG = 4
    Ye_rot = Rot([p.sb([128, NG, D], BF16, f"Ye{i}") for i in range(2)])
    SgT_rot = Rot([p.sb([128, NG, NT], BF16, f"SgT{i}") for i in range(2)])
    sel_rot = Rot([p.sb([NE, 128], BF16, f"sel{i}") for i in range(2)])
    gB_rot = Rot([p.sb([128, SC], F32, f"gB{i}") for i in range(2)])
    for qd in range(NE // NG):
        e0 = NG * qd
        Ye, SgT = Ye_rot.next(), SgT_rot.next()
        p.dma("sp", Ye[:], ye_d[e0:e0 + NG].rearrange("e s d -> s e d"), writes=[Ye])
        for ee in range(NG):
            ex = e0 + ee
            sel = sel_rot.next()
            p.op("dve", lambda e: e.tensor_copy(out=sel[:], in_=identb[0:NE, ex:ex + 1].to_broadcast([NE, 128])), reads=[identb], writes=[sel])
            for n in range(3):
                sl = slice(n * SC, (n + 1) * SC)
                pa, pg = ps_rot.next(), ps_rot.next()
                p.op("pe", lambda e: e.matmul(pa[:, 0:SC], lhsT=sel[:], rhs=slotT[:, sl], start=True, stop=True), reads=[sel, slotT], writes=[pa])
                p.op("pe", lambda e: e.matmul(pg[:, 0:SC], lhsT=sel[:], rhs=gtT[:, sl], start=True, stop=True), reads=[sel, gtT], writes=[pg])
                gB = gB_rot.next()
                p.op("act", lambda e: e.activation(out=gB[:], in_=pg[:, 0:SC], func=AF.Copy), reads=[pg], writes=[gB])
                p.op("dve", lambda e: e.scalar_tensor_tensor(out=SgT[:, ee, sl], in0=pa[:, 0:SC], scalar=iota_p[:, 0:1], in1=gB[:],
                                                             op0=ALU.is_equal, op1=ALU.mult), reads=[pa, iota_p, gB], writes=[SgT], accum=(ee + n > 0))
        for c in range(KC):
            for n in range(3):
                sl = slice(n * SC, (n + 1) * SC)
                ps = ps_rot.next()
                for ee in range(NG):
                    p.op("pe", lambda e: e.matmul(ps[:, 0:SC], lhsT=Ye[:, ee, c * 128:(c + 1) * 128], rhs=SgT[:, ee, sl], start=(ee == 0), stop=(ee == NG - 1)),
                         reads=[Ye, SgT], writes=[ps], accum=(ee > 0))
                p.op("dve", lambda e: e.tensor_tensor(out=z[:, c, sl], in0=z[:, c, sl], in1=ps[:, 0:SC], op=ALU.add), reads=[z, ps], writes=[z])
    ov = outT.rearrange("(c p) t -> p c t", p=128)
    p.release(mk)
    rstd = p.sb([128, NT], F32, "rstd")
    onesD = p.sb([128, 128], BF16, "onesD")
    epsc = p.sb([128, 1], F32, "epsc")
    p.op("pool", lambda e: e.memset(onesD[:], 1.0 / D), writes=[onesD])
    p.op("pool", lambda e: e.memset(epsc[:], EPS), writes=[epsc])
    sq_rot = Rot([p.sb([128, NT], BF16, f"sq{i}") for i in range(2)])
    if last:
        emit_rmsnorm(p, z, fnw, None, onesD, epsc, sq_rot, ps_rot, rstd, hn32=z)
        for q in range(4):
            p.dma("sp", ov[:, 4 * q:4 * q + 4, :], z[:, 4 * q:4 * q + 4, :], reads=[z])
        p.finish([z])
        return nc
    for q in range(4):
        p.dma("sp", ov[:, 4 * q:4 * q + 4, :], z[:, 4 * q:4 * q + 4, :], reads=[z])
    lnw = p.sb([128, KC], F32, "lnw_s")
    p.dma("sp", lnw[:], lnw_d[:, :], writes=[lnw])
    hn = p.sb([128, KC, NT], BF16, "hn")
    wb_rot = Rot([p.sb([128, KC, 512], BF16, f"wb{i}") for i in range(2)])
    st_rot = Rot([p.sb([128, NT], F32, f"st{i}") for i in range(3)])
    emit_rmsnorm(p, z, lnw, hn, onesD, epsc, sq_rot, ps_rot, rstd)
    emit_inproj(p, hn, w_in, projT, wb_rot, ps_rot, st_rot, Evac(p))
    p.finish([z] + st_rot.tiles)
    return nc


_PROGS = {}


def _prog(name, fn, *args):
    key = (name,) + args
    if key not in _PROGS:
        _PROGS[key] = fn(*args)
    return _PROGS[key]


def _run(nc, in_maps):
    res = run_bass_kernel_spmd(nc, in_maps, core_ids=list(range(8)))
    return res.results


def _colT(v, n):
    return np.ascontiguousarray(np.asarray(v, np.float32).reshape(n, 128).T)


def _rep(v, n=128):
    v = np.asarray(v, np.float32)
    return np.ascontiguousarray(np.broadcast_to(v[None], (n,) + v.shape))


def _pairs(a):
    a = np.asarray(a, np.float32)
    sh = a.shape[2:]
    nd = len(sh)
    return np.ascontiguousarray(a.reshape(8, 2, 64, *sh).transpose(1, 2, 0, *range(3, 3 + nd)).reshape(128, 8, *sh))


def _tok_tiles(a):
    o = np.zeros((NQT, 128, a.shape[-1]), np.float32)
    o[0, :NMETA] = a[:NMETA]
    o[1:] = a[NMETA:].reshape(16, 128, -1)
    return o.transpose(1, 0, 2)


def _untok_tiles(o):
    o = o.transpose(1, 0, 2)
    return np.concatenate([o[0, :NMETA], o[1:].reshape(SEQ, -1)], axis=0)


def kernel(x, meta_tokens, ln1_w, w_in, hgrn_lower_bounds, hgrn_norm_w, s5_a_re, s5_a_im,
           s5_b_re, s5_b_im, s5_c_re, s5_c_im, s5_d, s5_log_dt, s5_w_glu, s5_b_glu,
           diff_lambda_q1, diff_lambda_k1, diff_lambda_q2, diff_lambda_k2, diff_subln_w,
           w_out, ln2_w, router_group_w, router_group_b, router_expert_w, router_expert_b,
           expert_w1, expert_w3, expert_w2, final_norm_w):
    import math
    f32 = lambda a: np.asarray(a, np.float32)
    x, meta_tokens = f32(x), f32(meta_tokens)
    z0 = np.concatenate([np.broadcast_to(meta_tokens[None], (NB, NMETA, D)), x], axis=1)
    zTs = [np.ascontiguousarray(z0[c // 2, (c % 2) * NT:(c % 2 + 1) * NT].T) for c in range(8)]
    depth = f32(ln1_w).shape[0]
    hlb = f32(hgrn_lower_bounds)
    fnw = _colT(final_norm_w, KC)
    for layer in range(depth):
        if layer == 0:
            r = _run(_prog("A", build_A), [{"zT": zTs[c], "lnw": _colT(f32(ln1_w)[0], KC), "w_in": f32(w_in)[0]} for c in range(8)])
            projTs = [r[c]["projT"] for c in range(8)]
            del r
        proj = np.stack([np.concatenate([projTs[2 * b].T, projTs[2 * b + 1].T], axis=0) for b in range(NB)])
        del projTs
        lsel = np.full((128, 1), 1.0 if layer > 0 else 0.0, np.float32)
        nw = _rep(f32(hgrn_norm_w)[layer])
        li = 0.8 - 0.6 * math.exp(-0.3 * layer)
        linit = _rep(np.array([li, 1.0 - li], np.float32))
        lamv = _rep(np.stack([f32(diff_lambda_q1)[layer], f32(diff_lambda_k1)[layer], f32(diff_lambda_q2)[layer], f32(diff_lambda_k2)[layer]]))
        subw = _rep(f32(diff_subln_w)[layer])
        ins = []
        for c in range(8):
            b, hh = c // 2, c % 2
            hs = [2 * hh, 2 * hh + 1]

            def padT(col0):
                o = np.zeros((2, 128, LP), np.float32)
                for i, h in enumerate(hs):
                    o[i, :, :L] = proj[b, :, col0 + h * 128:col0 + (h + 1) * 128].T
                return o

            def padded(col0):
                o = np.zeros((2, LP, 128), np.float32)
                for i, h in enumerate(hs):
                    o[i, :L] = proj[b, :, col0 + h * 128:col0 + (h + 1) * 128]
                return o

            hv = np.ascontiguousarray(padded(1024).reshape(2, NCK, HC, 128).transpose(0, 2, 1, 3))
            hg = np.ascontiguousarray(padded(1536).reshape(2, LP // 128, 128, 128).transpose(0, 2, 1, 3))
            lbraw = np.ascontiguousarray(np.stack([hlb[0:2, h * 128:(h + 1) * 128].T for h in hs], axis=1))
            d_ = {"hqT": padT(0), "hfT": padT(512), "hv": hv, "hg": hg, "lbraw": lbraw, "lsel": lsel, "nw": nw}
            gs = slice(16 * hh, 16 * hh + 16)
            u = proj[b, :, 2048 + 256 * hh:2048 + 256 * (hh + 1)]
            d_.update({"uT": np.ascontiguousarray(u.T.reshape(2, 128, L)),
                       "a_re": _pairs(f32(s5_a_re)[layer, gs]), "a_im": _pairs(f32(s5_a_im)[layer, gs]),
                       "ldt": _pairs(np.broadcast_to(f32(s5_log_dt)[layer, gs][:, None], (16, 64))),
                       "b_re": _pairs(f32(s5_b_re)[layer, gs]), "b_im": _pairs(f32(s5_b_im)[layer, gs]),
                       "cT_re": _pairs(f32(s5_c_re)[layer, gs].transpose(0, 2, 1)), "cT_im": _pairs(f32(s5_c_im)[layer, gs].transpose(0, 2, 1)),
                       "dsk": np.ascontiguousarray(f32(s5_d)[layer, 256 * hh:256 * (hh + 1)].reshape(2, 128).T)})
            dq = proj[b, :, 2560:3584].reshape(L, 4, 2, 128)
            dk = proj[b, :, 3584:4608].reshape(L, 4, 2, 128)
            dv = proj[b, :, 4608:5632].reshape(L, 4, 256)
            d_.update({"qT": np.ascontiguousarray(dq[:, 2 * hh:2 * hh + 2].reshape(L, 4, 128).transpose(1, 2, 0)),
                       "kT": np.ascontiguousarray(dk[:, 2 * hh:2 * hh + 2].reshape(L, 4, 128).transpose(1, 2, 0)),
                       "v": np.ascontiguousarray(np.stack([_tok_tiles(dv[:, 2 * hh + i]) for i in range(2)], axis=1)),
                       "lamv": lamv, "subw": subw, "linit": linit})
            ins.append(d_)
        r = _run(_prog("B", build_B), ins)
        o_a = np.zeros((NB, L, 512), np.float32)
        ys = np.zeros((NB, L, 512), np.float32)
        o_c = np.zeros((NB, L, 1024), np.float32)
        for c in range(8):
            b, hh = c // 2, c % 2
            for i in range(2):
                h = 2 * hh + i
                o_a[b, :, h * 128:(h + 1) * 128] = r[c]["oa"][i].transpose(1, 0, 2).reshape(LP, 128)[:L]
                o_c[b, :, h * 256:(h + 1) * 256] = _untok_tiles(r[c]["oc"][:, i])
            ys[b, :, 256 * hh:256 * (hh + 1)] = r[c]["yT"].reshape(256, L).T
        del r
        del proj
        w_r = np.ascontiguousarray(np.concatenate([f32(router_group_w)[layer], f32(router_expert_w)[layer]], axis=1))
        b_r = _rep(np.concatenate([f32(router_group_b)[layer], f32(router_expert_b)[layer]]))
        ins = []
        for c in range(8):
            b, tk = c // 2, slice((c % 2) * NT, (c % 2 + 1) * NT)
            ins.append({"zT": zTs[c], "oaT": np.ascontiguousarray(o_a[b, tk].T), "ysT": np.ascontiguousarray(ys[b, tk].T),
                        "ocT": np.ascontiguousarray(o_c[b, tk].T), "w_glu": f32(s5_w_glu)[layer], "b_glu": _colT(f32(s5_b_glu)[layer], 8),
                        "w_out": f32(w_out)[layer], "ln2": _colT(f32(ln2_w)[layer], KC), "w_r": w_r, "b_r": b_r})
        r2 = r1 = _run(_prog("C1", build_C1), ins)
        ins = []
        for g in range(8):
            es = slice(EPC * g, EPC * (g + 1))
            xe = np.ascontiguousarray(np.concatenate([r2[c]["xeT"][es] for c in range(8)], axis=2))
            ins.append({"xe": xe, "w1": f32(expert_w1)[layer, es], "w3": f32(expert_w3)[layer, es], "w2": f32(expert_w2)[layer, es]})
        del r2
        r3 = _run(_prog("C2b", build_C2b), ins)
        last = layer == depth - 1
        ins = []
        for c in range(8):
            ye = np.ascontiguousarray(np.concatenate([r3[g]["ye"][:, c * NSL:(c + 1) * NSL] for g in range(8)], axis=0))
            d_ = {"zT": r1[c]["z1T"], "slotT": r1[c]["slotT"], "gtT": r1[c]["gtT"], "ye": ye, "fnw": fnw}
            if not last:
                d_.update({"lnw": _colT(f32(ln1_w)[layer + 1], KC), "w_in": f32(w_in)[layer + 1]})
            ins.append(d_)
        del r3
        r4 = _run(_prog("C2c", build_C2c, last), ins)
        zTs = [r4[c]["outT"] for c in range(8)]
        if not last:
            projTs = [r4[c]["projT"] for c in range(8)]
        del r4
    out = np.stack([np.concatenate([zTs[2 * b].T, zTs[2 * b + 1].T], axis=0) for b in range(NB)])
    return np.ascontiguousarray(out[:, NMETA:]).astype(np.float32)
```

```python
import numpy as np
import concourse.bass as bass
import concourse.mybir as mybir
from concourse.bass_utils import run_bass_kernel_spmd

F32 = mybir.dt.float32
BF16 = mybir.dt.bfloat16
AF = mybir.ActivationFunctionType
ALU = mybir.AluOpType
AX = mybir.AxisListType


class Res:
    __slots__ = ("w", "r", "dsem", "dcnt", "t", "name", "cm")

    def __init__(self, name=""):
        self.w = {}
        self.r = {}
        self.dsem = None
        self.dcnt = 0
        self.t = None
        self.name = name


class T(Res):
    def __getitem__(self, idx):
        return self.t[idx]


class P:
    def __init__(self, nc, same_engine_sync=True):
        self.nc = nc
        self.eng = {"pe": nc.tensor, "dve": nc.vector, "act": nc.scalar, "pool": nc.gpsimd, "sp": nc.sync}
        self.sem = {}
        self.cnt = {}
        for e in ("pe", "dve", "act", "pool"):
            self.sem[e] = nc.semaphore("sem_" + e).__enter__()
            self.cnt[e] = 0
        self.semid = {id(s): e for e, s in self.sem.items()}
        self.waited = {e: {} for e in self.eng}
        self.same_engine_sync = same_engine_sync
        self.out_tokens = []
        self.ntile = 0
        self.tiles = []
        self.ptiles = []
        self.prefix = ""

    def sb(self, shape, dt=F32, name=None):
        self.ntile += 1
        name = self.prefix + (name or f"t{self.ntile}")
        t = T(name)
        t.cm = self.nc.sbuf_tensor(name, list(shape), dt)
        t.t = t.cm.__enter__()
        self.tiles.append(t)
        return t

    def ps(self, shape, dt=F32, name=None):
        self.ntile += 1
        name = self.prefix + (name or f"p{self.ntile}")
        t = T(name)
        t.cm = self.nc.psum_tensor(name, list(shape), dt)
        t.t = t.cm.__enter__()
        self.ptiles.append(t)
        return t

    def _wait(self, e, deps, skip_own=False):
        eng = self.eng[e]
        own = self.sem.get(e)
        for sem, val in deps.items():
            if sem is own and (skip_own or not self.same_engine_sync):
                continue
            k = id(sem)
            if self.waited[e].get(k, 0) >= val:
                continue
            eng.wait_ge(sem, val)
            self.waited[e][k] = val

    @staticmethod
    def _merge(d, s):
        for k, v in s.items():
            if d.get(k, 0) < v:
                d[k] = v

    def op(self, e, fn, reads=(), writes=(), accum=False):
        deps = {}
        for r in reads:
            self._merge(deps, r.w)
        for w in writes:
            self._merge(deps, w.w)
            self._merge(deps, w.r)
        self._wait(e, deps, skip_own=(e == "pe"))
        inst = fn(self.eng[e])
        self.cnt[e] += 1
        inst.then_inc(self.sem[e], 1)
        tok = {self.sem[e]: self.cnt[e]}
        for r in reads:
            self._merge(r.r, tok)
        for w in writes:
            if accum:
                self._merge(w.w, tok)
            else:
                w.w = dict(tok)
                w.r = {}
        return inst

    def dma(self, q, out, in_, reads=(), writes=(), semres=None, concurrent=False, **kw):
        if semres is None:
            semres = (list(writes) + list(reads))[0]
        if semres.dsem is None:
            semres.dsem = self.nc.semaphore("d_" + semres.name).__enter__()
        deps = {}
        for r in reads:
            self._merge(deps, r.w)
        for w in writes:
            if concurrent:
                ww = {k: v for k, v in w.w.items() if k is not w.dsem}
                self._merge(deps, ww)
            else:
                self._merge(deps, w.w)
            self._merge(deps, w.r)
        self._wait(q, deps)
        inst = self.eng[q].dma_start(out=out, in_=in_, **kw)
        semres.dcnt += 16
        inst.then_inc(semres.dsem, 16)
        tok = {semres.dsem: semres.dcnt}
        for r in reads:
            self._merge(r.r, tok)
        for w in writes:
            w.w = dict(tok)
            w.r = {}
        return tok

    def barrier(self):
        deps = {}
        for e in ("pe", "dve", "act", "pool"):
            if self.cnt[e] > 0:
                deps[self.sem[e]] = self.cnt[e]
        for t in self.tiles:
            if t.dsem is not None and t.dcnt > 0:
                deps[t.dsem] = t.dcnt
        for e in self.eng:
            self._wait(e, deps, skip_own=True)

    def mark(self):
        return (len(self.tiles), len(self.ptiles))

    def release(self, mark):
        self.barrier()
        for lst, n in ((self.tiles, mark[0]), (self.ptiles, mark[1])):
            while len(lst) > n:
                t = lst.pop()
                t.cm.__exit__(None, None, None)

    def free(self, tiles):
        for t in reversed(tiles):
            t.cm.__exit__(None, None, None)
            self.tiles.remove(t)

    def finish(self, out_res):
        deps = {}
        for r in out_res:
            self._merge(deps, r.r)
            self._merge(deps, r.w)
        self._wait("sp", deps)


D = 2048
NB = 4
SEQ = 2048
NMETA = 16
L = SEQ + NMETA
NT = L // 2
NCH = 3
TCH = NT // NCH
INW = 5632
KC = D // 128
EPS = 1e-6


class Rot:
    def __init__(self, tiles):
        self.tiles = tiles
        self.i = 0

    def next(self):
        t = self.tiles[self.i % len(self.tiles)]
        self.i += 1
        return t


def emit_rmsnorm(p, z, lnw, hn, ones_bf, epsc, sq_rot, ps_rot, rstd, hn32=None, nt=NT, tch=TCH):
    nch = nt // tch
    pss = [ps_rot.next() for _ in range(nch)]
    for c in range(KC):
        sq = sq_rot.next()
        p.op("act", lambda e: e.activation(out=sq[:, 0:nt], in_=z[:, c, 0:nt], func=AF.Square), reads=[z], writes=[sq])
        for n in range(nch):
            p.op("pe", lambda e: e.matmul(pss[n][:, 0:tch], lhsT=ones_bf[:], rhs=sq[:, n * tch:(n + 1) * tch],
                                          start=(c == 0), stop=(c == KC - 1)),
                 reads=[sq, ones_bf], writes=[pss[n]], accum=(c > 0))
    for n in range(nch):
        sl = slice(n * tch, (n + 1) * tch)
        p.op("act", lambda e: e.activation(out=rstd[:, sl], in_=pss[n][:, 0:tch], func=AF.Sqrt, bias=epsc[:, 0:1]),
             reads=[pss[n], epsc], writes=[rstd], accum=(n > 0))
    p.op("dve", lambda e: e.reciprocal(out=rstd[:, 0:nt], in_=rstd[:, 0:nt]), reads=[rstd], writes=[rstd])
    for c in range(KC):
        if hn is not None:
            p.op("dve", lambda e: e.scalar_tensor_tensor(out=hn[:, c, 0:nt], in0=z[:, c, 0:nt], scalar=lnw[:, c:c + 1], in1=rstd[:, 0:nt],
                                                         op0=ALU.mult, op1=ALU.mult), reads=[z, lnw, rstd], writes=[hn])
        if hn32 is not None:
            p.op("dve", lambda e: e.scalar_tensor_tensor(out=hn32[:, c, 0:nt], in0=z[:, c, 0:nt], scalar=lnw[:, c:c + 1], in1=rstd[:, 0:nt],
                                                         op0=ALU.mult, op1=ALU.mult), reads=[z, lnw, rstd], writes=[hn32])


def emit_inproj(p, hn, w_in, projT, wb_rot, ps_rot, st_rot, evac):
    GW = 512
    w_v = w_in.rearrange("(c p) n -> p c n", p=128)
    for g in range(INW // GW):
        wb = wb_rot.next()
        p.dma("pool", wb[:], w_v[:, :, g * GW:(g + 1) * GW], writes=[wb])
        for j in range(GW // 128):
            st = st_rot.next()
            for n in range(NCH):
                ps = ps_rot.next()
                for c in range(KC):
                    p.op("pe", lambda e: e.matmul(ps[:, 0:TCH], lhsT=wb[:, c, j * 128:(j + 1) * 128],
                                                  rhs=hn[:, c, n * TCH:(n + 1) * TCH], start=(c == 0), stop=(c == KC - 1)),
                         reads=[wb, hn], writes=[ps], accum=(c > 0))
                evac(st[:, n * TCH:(n + 1) * TCH], ps[:, 0:TCH], [ps], [st], first=(n == 0))
            r0 = g * GW + j * 128
            p.dma("sp", projT[r0:r0 + 128, :], st[:, 0:NT], reads=[st])


class Evac:
    def __init__(self, p):
        self.p = p
        self.i = 0

    def __call__(self, out, in_, reads, writes, first=True):
        self.i += 1
        p = self.p
        if self.i % 2 == 0:
            p.op("act", lambda e: e.activation(out=out, in_=in_, func=AF.Copy), reads=reads, writes=writes, accum=not first)
        else:
            p.op("dve", lambda e: e.tensor_copy(out=out, in_=in_), reads=reads, writes=writes, accum=not first)


def build_A():
    nc = bass.Bass("TRN2", target_bir_lowering=False)
    p = P(nc)
    zT = nc.dram_tensor("zT", [D, NT], F32, kind="ExternalInput").ap()
    lnw_d = nc.dram_tensor("lnw", [128, KC], F32, kind="ExternalInput").ap()
    w_in = nc.dram_tensor("w_in", [D, INW], F32, kind="ExternalInput").ap()
    projT = nc.dram_tensor("projT", [INW, NT], F32, kind="ExternalOutput").ap()
    z = p.sb([128, KC, NT], F32, "z")
    hn = p.sb([128, KC, NT], BF16, "hn")
    lnw = p.sb([128, KC], F32, "lnw_s")
    rstd = p.sb([128, NT], F32, "rstd")
    ones_bf = p.sb([128, 128], BF16, "ones")
    sq_rot = Rot([p.sb([128, NT], BF16, f"sq{i}") for i in range(2)])
    ps_rot = Rot([p.ps([128, 512], F32, f"ps{i}") for i in range(8)])
    wb_rot = Rot([p.sb([128, KC, 512], BF16, f"wb{i}") for i in range(2)])
    st_rot = Rot([p.sb([128, NT], F32, f"st{i}") for i in range(3)])
    evac = Evac(p)
    zv = zT.rearrange("(c p) t -> p c t", p=128)
    for q in range(4):
        p.dma("sp", z[:, 4 * q:4 * q + 4, :], zv[:, 4 * q:4 * q + 4, :], writes=[z], concurrent=True)
    p.dma("sp", lnw[:], lnw_d[:, :], writes=[lnw])
    p.op("pool", lambda e: e.memset(ones_bf[:], 1.0 / D), writes=[ones_bf])
    epsc = p.sb([128, 1], F32, "epsc")
    p.op("pool", lambda e: e.memset(epsc[:], EPS), writes=[epsc])
    emit_rmsnorm(p, z, lnw, hn, ones_bf, epsc, sq_rot, ps_rot, rstd)
    emit_inproj(p, hn, w_in, projT, wb_rot, ps_rot, st_rot, evac)
    p.finish(st_rot.tiles)
    return nc


HC = 32
LP = 2176
NCK = LP // HC


def build_B1():
    nc = bass.Bass("TRN2", target_bir_lowering=False)
    p = P(nc)
    outs = emit_B1(nc, p)
    p.finish(outs)
    return nc


def emit_B1(nc, p):
    hqT = nc.dram_tensor("hqT", [2, 128, LP], F32, kind="ExternalInput").ap()
    hfT = nc.dram_tensor("hfT", [2, 128, LP], F32, kind="ExternalInput").ap()
    hv = nc.dram_tensor("hv", [2, HC, NCK, 128], F32, kind="ExternalInput").ap()
    hg = nc.dram_tensor("hg", [2, 128, LP // 128, 128], F32, kind="ExternalInput").ap()
    lbraw_d = nc.dram_tensor("lbraw", [128, 2, 2], F32, kind="ExternalInput").ap()
    lsel_d = nc.dram_tensor("lsel", [128, 1], F32, kind="ExternalInput").ap()
    nw_d = nc.dram_tensor("nw", [128, 128], F32, kind="ExternalInput").ap()
    oa = nc.dram_tensor("oa", [2, 128, LP // 128, 128], F32, kind="ExternalOutput").ap()

    lb = p.sb([128, 2], F32, "lb_s")
    oml = p.sb([128, 2], F32, "oml")
    nw = p.sb([128, 128], F32, "nw_s")
    epsc = p.sb([128, 1], F32, "epsc")
    maskU = p.sb([HC, HC], F32, "maskU")
    mm = p.sb([128, LP], F32, "mm")
    ident = p.sb([128, 128], BF16, "ident")
    identf = p.sb([128, 128], F32, "identf")
    lbraw = p.sb([128, 2, 2], F32, "lbraw_s")
    lsel = p.sb([128, 1], F32, "lsel_s")
    p.dma("sp", lbraw[:], lbraw_d[:, :, :], writes=[lbraw])
    p.dma("sp", lsel[:], lsel_d[:, :], writes=[lsel])
    p.op("dve", lambda e: e.tensor_tensor(out=lb[:], in0=lbraw[:, :, 1], in1=lbraw[:, :, 0], op=ALU.subtract), reads=[lbraw], writes=[lb])
    p.op("act", lambda e: e.activation(out=lb[:], in_=lb[:], func=AF.Sigmoid), reads=[lb], writes=[lb])
    p.op("dve", lambda e: e.tensor_scalar(out=lb[:], in0=lb[:], scalar1=lsel[:, 0:1], scalar2=None, op0=ALU.mult), reads=[lb, lsel], writes=[lb])
    p.dma("sp", nw[:], nw_d[:, :], writes=[nw])
    p.op("pool", lambda e: e.memset(epsc[:], EPS), writes=[epsc])
    p.op("pool", lambda e: e.memset(maskU[:], 1.0), writes=[maskU])
    p.op("pool", lambda e: e.affine_select(out=maskU[:], in_=maskU[:], pattern=[[1, HC]], compare_op=ALU.is_ge, fill=0.0,
                                           base=0, channel_multiplier=-1), reads=[maskU], writes=[maskU])
    p.op("pool", lambda e: e.memset(identf[:], 0.0), writes=[identf])
    p.op("pool", lambda e: e.affine_select(out=identf[:], in_=identf[:], pattern=[[-1, 128]], compare_op=ALU.not_equal,
                                           fill=1.0, base=0, channel_multiplier=1), reads=[identf], writes=[identf])
    p.op("pool", lambda e: e.tensor_copy(out=ident[:], in_=identf[:]), reads=[identf], writes=[ident])
    p.op("pool", lambda e: e.memset(mm[:], 1.0), writes=[mm])
    p.op("pool", lambda e: e.memset(mm[:].rearrange("p (n c) -> p n c", c=HC)[:, :, 0:1], 0.0), reads=[mm], writes=[mm])
    p.op("dve", lambda e: e.tensor_scalar(out=oml[:], in0=lb[:], scalar1=-1.0, scalar2=1.0, op0=ALU.mult, op1=ALU.add),
         reads=[lb], writes=[oml])

    H = []
    shared = {nm: p.sb([128, LP], F32, "sh_" + nm) for nm in ("q", "f", "b", "e")}
    for h in range(2):
        d = {}
        for nm in ("q", "f", "b", "e"):
            d[nm] = shared[nm]
        d["qs"] = p.sb([128, LP], BF16, f"qs{h}")
        d["ks"] = p.sb([128, LP], BF16, f"ks{h}")
        d["kh"] = p.sb([128, LP], BF16, f"kh{h}")
        d["ebend"] = p.sb([128, NCK], F32, f"ebend{h}")
        d["v"] = p.sb([HC, NCK, 128], BF16, f"v{h}")
        d["g"] = p.sb([128, LP // 128, 128], F32, f"g{h}")
        d["o"] = p.sb([128, LP // 128, 128], F32, f"o{h}")
        d["S"] = p.sb([128, 128], F32, f"S{h}")
        d["Sb"] = p.sb([128, 128], BF16, f"Sb{h}")
        d["khT"] = Rot([p.sb([HC, 128], BF16, f"khT{h}_{i}") for i in range(4)])
        d["ATm"] = Rot([p.sb([HC, HC], BF16, f"ATm{h}_{i}") for i in range(3)])
        d["ms"] = p.sb([128, LP // 128], F32, f"ms{h}")
        H.append(d)
        p.dma("pool", d["v"][:], hv[h], writes=[d["v"]])
        p.dma("sp", d["g"][:], hg[h], writes=[d["g"]])
    psA = Rot([p.ps([128, 512], F32, f"psA{i}") for i in range(2)])
    psT = Rot([p.ps([128, 512], BF16, f"psT{i}") for i in range(2)])
    psO = Rot([p.ps([128, 512], F32, f"psO{i}") for i in range(2)])
    psS = Rot([p.ps([128, 512], F32, f"psS{i}") for i in range(2)])

    for h in range(2):
        d = H[h]
        q, f, b, e_ = d["q"], d["f"], d["b"], d["e"]
        p.dma("sp", q[:], hqT[h], writes=[q])
        p.dma("sp", f[:], hfT[h], writes=[f])
        p.op("act", lambda e: e.activation(out=f[:], in_=f[:], func=AF.Sigmoid), reads=[f], writes=[f])
        p.op("dve", lambda e: e.tensor_scalar(out=f[:], in0=f[:], scalar1=oml[:, h:h + 1], scalar2=lb[:, h:h + 1],
                                              op0=ALU.mult, op1=ALU.add), reads=[f, oml, lb], writes=[f])
        p.op("act", lambda e: e.activation(out=e_[:], in_=f[:], func=AF.Ln), reads=[f], writes=[e_])
        p.op("dve", lambda e: e.tensor_tensor_scan(out=b[:], data0=mm[:], data1=e_[:], initial=0.0, op0=ALU.mult, op1=ALU.add),
             reads=[mm, e_], writes=[b])
        p.op("dve", lambda e: e.tensor_scalar(out=f[:], in0=f[:], scalar1=-1.0, scalar2=1.0, op0=ALU.mult, op1=ALU.add),
             reads=[f], writes=[f])
        p.op("act", lambda e: e.activation(out=e_[:], in_=b[:], func=AF.Exp), reads=[b], writes=[e_])
        p.op("dve", lambda e: e.tensor_tensor(out=d["qs"][:], in0=q[:], in1=e_[:], op=ALU.mult), reads=[q, e_], writes=[d["qs"]])
        p.op("act", lambda e: e.activation(out=e_[:], in_=b[:], func=AF.Exp, scale=-1.0), reads=[b], writes=[e_])
        p.op("dve", lambda e: e.tensor_tensor(out=d["ks"][:], in0=f[:], in1=e_[:], op=ALU.mult), reads=[f, e_], writes=[d["ks"]])
        b3 = b[:].rearrange("p (n c) -> p n c", c=HC)
        p.op("act", lambda e: e.activation(out=d["ebend"][:], in_=b3[:, :, HC - 1], func=AF.Exp), reads=[b], writes=[d["ebend"]])
        e3 = e_[:].rearrange("p (n c) -> p n c", c=HC)
        p.op("dve", lambda e: e.tensor_tensor(out=e3, in0=b3[:, :, HC - 1:HC].to_broadcast([128, NCK, HC]), in1=b3, op=ALU.subtract),
             reads=[b], writes=[e_])
        p.op("act", lambda e: e.activation(out=e_[:], in_=e_[:], func=AF.Exp), reads=[e_], writes=[e_])
        p.op("dve", lambda e: e.tensor_tensor(out=d["kh"][:], in0=f[:], in1=e_[:], op=ALU.mult), reads=[f, e_], writes=[d["kh"]])
        p.op("pool", lambda e: e.memset(d["S"][:], 0.0), writes=[d["S"]])
        p.op("pool", lambda e: e.memset(d["Sb"][:], 0.0), writes=[d["Sb"]])

    def pre(n, h):
        d = H[h]
        sl = slice(n * HC, (n + 1) * HC)
        qs, ks, kh = d["qs"], d["ks"], d["kh"]
        pa = psA.next()
        p.op("pe", lambda e: e.matmul(pa[0:HC, 0:HC], lhsT=ks[:, sl], rhs=qs[:, sl], start=True, stop=True),
             reads=[ks, qs], writes=[pa])
        atm = d["ATm"].next()
        p.op("dve", lambda e: e.tensor_tensor(out=atm[:], in0=pa[0:HC, 0:HC], in1=maskU[:], op=ALU.mult),
             reads=[pa, maskU], writes=[atm])
        pt = psT.next()
        p.op("pe", lambda e: e.transpose(out=pt[0:HC, 0:128], in_=kh[:, sl], identity=ident[:]), reads=[kh, ident], writes=[pt])
        kht = d["khT"].next()
        p.op("act", lambda e: e.activation(out=kht[:], in_=pt[0:HC, 0:128], func=AF.Copy), reads=[pt], writes=[kht])
        return atm, kht

    def post(n, h, atm, kht):
        d = H[h]
        sl = slice(n * HC, (n + 1) * HC)
        qs, v, S, Sb = d["qs"], d["v"], d["S"], d["Sb"]
        po = psO.next()
        p.op("pe", lambda e: e.matmul(po[0:HC, 0:128], lhsT=atm[:], rhs=v[:, n, :], start=True, stop=False),
             reads=[atm, v], writes=[po])
        p.op("pe", lambda e: e.matmul(po[0:HC, 0:128], lhsT=qs[:, sl], rhs=Sb[:], start=False, stop=True),
             reads=[qs, Sb], writes=[po], accum=True)
        pS = psS.next()
        p.op("pe", lambda e: e.matmul(pS[:, 0:128], lhsT=kht[:], rhs=v[:, n, :], start=True, stop=True),
             reads=[kht, v], writes=[pS])
        p.op("dve", lambda e: e.scalar_tensor_tensor(out=S[:], in0=S[:], scalar=d["ebend"][:, n:n + 1], in1=pS[:, 0:128],
                                                     op0=ALU.mult, op1=ALU.add), reads=[S, d["ebend"], pS], writes=[S])
        p.op("act", lambda e: e.activation(out=Sb[:], in_=S[:], func=AF.Copy), reads=[S], writes=[Sb])
        pj = HC * (n % 4)
        p.op("act", lambda e: e.activation(out=d["o"][pj:pj + HC, n // 4, :], in_=po[0:HC, 0:128], func=AF.Copy), reads=[po],
             writes=[d["o"]], accum=(n > 0))

    nxt = [pre(0, h) for h in range(2)]
    for n in range(NCK):
        cur = nxt
        if n + 1 < NCK:
            nxt = [pre(n + 1, h) for h in range(2)]
        for h in range(2):
            post(n, h, *cur[h])

    for h in range(2):
        d = H[h]
        o, g, ms = d["o"], d["g"], d["ms"]
        sq = shared["q"]
        NT_ = LP // 128
        sq3 = sq[:].rearrange("p (n c) -> p n c", c=128)
        p.op("pool", lambda e: e.tensor_tensor(out=sq3, in0=o[:], in1=o[:], op=ALU.mult), reads=[o], writes=[sq])
        p.op("dve", lambda e: e.tensor_reduce(out=ms[:], in_=sq3, axis=AX.X, op=ALU.add), reads=[sq], writes=[ms])
        p.op("act", lambda e: e.activation(out=ms[:], in_=ms[:], func=AF.Sqrt, bias=epsc[:, 0:1], scale=1.0 / 128),
             reads=[ms, epsc], writes=[ms])
        p.op("dve", lambda e: e.reciprocal(out=ms[:], in_=ms[:]), reads=[ms], writes=[ms])
        p.op("dve", lambda e: e.tensor_tensor(out=o[:], in0=o[:], in1=ms[:].unsqueeze(2).to_broadcast([128, NT_, 128]), op=ALU.mult),
             reads=[o, ms], writes=[o])
        p.op("pool", lambda e: e.tensor_tensor(out=o[:], in0=o[:], in1=nw[:].unsqueeze(1).to_broadcast([128, NT_, 128]), op=ALU.mult),
             reads=[o, nw], writes=[o])
        p.op("act", lambda e: e.activation(out=g[:], in_=g[:], func=AF.Silu), reads=[g], writes=[g])
        p.op("dve", lambda e: e.tensor_tensor(out=o[:], in0=o[:], in1=g[:], op=ALU.mult), reads=[o, g], writes=[o])
        p.dma("sp", oa[h], o[:], reads=[o])
    return [H[0]["o"], H[1]["o"]]


S5N = 6
S5_ENG = ["dve", "dve", "dve", "dve"]
S5C = L // S5N
PI = 3.14159265358979


def build_B2():
    nc = bass.Bass("TRN2", target_bir_lowering=False)
    p = P(nc)
    outs = emit_B2(nc, p)
    p.finish(outs)
    return nc


def emit_B2(nc, p):
    uT = nc.dram_tensor("uT", [2, 128, L], F32, kind="ExternalInput").ap()
    are_d = nc.dram_tensor("a_re", [128, 8], F32, kind="ExternalInput").ap()
    aim_d = nc.dram_tensor("a_im", [128, 8], F32, kind="ExternalInput").ap()
    ldt_d = nc.dram_tensor("ldt", [128, 8], F32, kind="ExternalInput").ap()
    bre_d = nc.dram_tensor("b_re", [128, 8, 16], F32, kind="ExternalInput").ap()
    bim_d = nc.dram_tensor("b_im", [128, 8, 16], F32, kind="ExternalInput").ap()
    cre_d = nc.dram_tensor("cT_re", [128, 8, 16], F32, kind="ExternalInput").ap()
    cim_d = nc.dram_tensor("cT_im", [128, 8, 16], F32, kind="ExternalInput").ap()
    dsk_d = nc.dram_tensor("dsk", [128, 2], F32, kind="ExternalInput").ap()
    yT = nc.dram_tensor("yT", [2, 128, L], F32, kind="ExternalOutput").ap()

    def small(name, shape=(128, 8)):
        return p.sb(list(shape), F32, name)

    a_re, a_im, ldt = small("are_s"), small("aim_s"), small("ldt_s")
    b_re, b_im = small("bre_s", (128, 8, 16)), small("bim_s", (128, 8, 16))
    c_re, c_im = small("cre_s", (128, 8, 16)), small("cim_s", (128, 8, 16))
    dsk = small("dsk_s", (128, 2))
    for t_, d_ in ((a_re, are_d), (a_im, aim_d), (ldt, ldt_d), (dsk, dsk_d)):
        p.dma("sp", t_[:], d_[:, :], writes=[t_])
    for t_, d_ in ((b_re, bre_d), (b_im, bim_d), (c_re, cre_d), (c_im, cim_d)):
        p.dma("sp", t_[:], d_[:, :, :], writes=[t_])
    u = p.sb([128, 2, L], F32, "u")
    ub = p.sb([128, 2, L], BF16, "ub")
    p.dma("sp", u[:], uT.rearrange("k p t -> p k t"), writes=[u])
    p.dma("pool", ub[:], uT.rearrange("k p t -> p k t"), writes=[ub])
    pic = small("pic", (128, 1))
    p.op("pool", lambda e: e.memset(pic[:], PI), writes=[pic])
    identf = p.sb([128, 128], F32, "identf")
    p.op("pool", lambda e: e.memset(identf[:], 0.0), writes=[identf])
    p.op("pool", lambda e: e.affine_select(out=identf[:], in_=identf[:], pattern=[[-1, 128]], compare_op=ALU.not_equal,
                                           fill=1.0, base=0, channel_multiplier=1), reads=[identf], writes=[identf])
    iot = p.sb([128, L], F32, "iot")
    p.op("pool", lambda e: e.iota(iot[:], pattern=[[1, L]], base=0, channel_multiplier=0, allow_small_or_imprecise_dtypes=True), writes=[iot])

    def ts(out, in0, s1, s2, op0, op1=None, eng="dve"):
        rd = [in0] + [x for x in (s1, s2) if isinstance(x, T)]
        a1 = s1[:] if isinstance(s1, T) else s1
        a2 = s2[:] if isinstance(s2, T) else s2
        if op1 is None:
            p.op(eng, lambda e: e.tensor_scalar(out=out[:], in0=in0[:], scalar1=a1, scalar2=None, op0=op0), reads=rd, writes=[out])
        else:
            p.op(eng, lambda e: e.tensor_scalar(out=out[:], in0=in0[:], scalar1=a1, scalar2=a2, op0=op0, op1=op1), reads=rd, writes=[out])

    def tt(out, in0, in1, op, eng="dve"):
        p.op(eng, lambda e: e.tensor_tensor(out=out[:], in0=in0[:], in1=in1[:], op=op), reads=[in0, in1], writes=[out])

    I32 = mybir.dt.int32

    def wrap_sin(out, x, tf, ti, shift=0.0):
        if shift != 0.0:
            ts(tf, x, shift, None, ALU.add, eng="pool")
            x = tf
        p.op("dve", lambda e: e.tensor_scalar(out=ti[:], in0=x[:], scalar1=1.0 / (2 * PI), scalar2=None, op0=ALU.mult), reads=[x], writes=[ti])
        kf = out
        p.op("pool", lambda e: e.tensor_copy(out=kf[:], in_=ti[:]), reads=[ti], writes=[kf])
        p.op("dve", lambda e: e.scalar_tensor_tensor(out=tf[:], in0=kf[:], scalar=-2 * PI, in1=x[:], op0=ALU.mult, op1=ALU.add),
             reads=[kf, x], writes=[tf])
        ts(tf, tf, PI, -PI, ALU.min, ALU.max, eng="pool")
        p.op("act", lambda e: e.activation(out=out[:], in_=tf[:], func=AF.Sin), reads=[tf], writes=[out])

    dt, th, mag, tmp, tmp2 = small("dt"), small("th"), small("mag"), small("tmp"), small("tmp2")
    s1, c1, abr, abi, den, zre, zim = (small(n) for n in ("s1", "c1", "abr", "abi", "den", "zre", "zim"))
    p.op("act", lambda e: e.activation(out=dt[:], in_=ldt[:], func=AF.Exp), reads=[ldt], writes=[dt])
    tt(tmp, a_re, dt, ALU.mult)
    p.op("act", lambda e: e.activation(out=mag[:], in_=tmp[:], func=AF.Exp), reads=[tmp], writes=[mag])
    tt(th, a_im, dt, ALU.mult)
    smi = p.sb([128, 8], I32, "smi")
    p.op("dve", lambda e: e.tensor_scalar(out=smi[:], in0=th[:], scalar1=1.0 / (2 * PI), scalar2=None, op0=ALU.mult), reads=[th], writes=[smi])
    p.op("dve", lambda e: e.tensor_copy(out=tmp[:], in_=smi[:]), reads=[smi], writes=[tmp])
    p.op("dve", lambda e: e.scalar_tensor_tensor(out=th[:], in0=tmp[:], scalar=-2 * PI, in1=th[:], op0=ALU.mult, op1=ALU.add),
         reads=[tmp, th], writes=[th])
    wrap_sin(s1, th, tmp, smi)
    wrap_sin(c1, th, tmp, smi, shift=PI / 2)
    tt(abr, mag, c1, ALU.mult)
    tt(abi, mag, s1, ALU.mult)
    tt(den, a_re, a_re, ALU.mult)
    tt(tmp, a_im, a_im, ALU.mult)
    tt(den, den, tmp, ALU.add)
    p.op("dve", lambda e: e.reciprocal(out=den[:], in_=den[:]), reads=[den], writes=[den])
    ts(tmp, abr, -1.0, None, ALU.add)
    tt(zre, tmp, a_re, ALU.mult)
    tt(tmp2, abi, a_im, ALU.mult)
    tt(zre, zre, tmp2, ALU.add)
    tt(zre, zre, den, ALU.mult)
    tt(zim, abi, a_re, ALU.mult)
    tt(tmp2, tmp, a_im, ALU.mult)
    tt(zim, zim, tmp2, ALU.subtract)
    tt(zim, zim, den, ALU.mult)
    bbr, bbi, t3a, t3b = (small(n, (128, 8, 16)) for n in ("bbr", "bbi", "t3a", "t3b"))

    def bc(x):
        return x[:].unsqueeze(2).to_broadcast([128, 8, 16])

    p.op("dve", lambda e: e.tensor_tensor(out=t3a[:], in0=b_re[:], in1=bc(zre), op=ALU.mult), reads=[b_re, zre], writes=[t3a])
    p.op("dve", lambda e: e.tensor_tensor(out=t3b[:], in0=b_im[:], in1=bc(zim), op=ALU.mult), reads=[b_im, zim], writes=[t3b])
    tt(bbr, t3a, t3b, ALU.subtract)
    p.op("dve", lambda e: e.tensor_tensor(out=t3a[:], in0=b_im[:], in1=bc(zre), op=ALU.mult), reads=[b_im, zre], writes=[t3a])
    p.op("dve", lambda e: e.tensor_tensor(out=t3b[:], in0=b_re[:], in1=bc(zim), op=ALU.mult), reads=[b_re, zim], writes=[t3b])
    tt(bbi, t3a, t3b, ALU.add)
    ts(c_im, c_im, -1.0, None, ALU.mult)

    ps_rot = Rot([p.ps([128, 512], F32, f"ps{i}") for i in range(6)])
    WB = [[p.sb([128, 128], BF16, f"WB{ri}_{j}") for j in range(8)] for ri in range(2)]
    WC = [[p.sb([128, 128], BF16, f"WC{ri}_{j}") for j in range(8)] for ri in range(2)]
    stg = Rot([p.sb([128, 128], F32, f"stg{i}") for i in range(2)])
    for j in range(8):
        j4 = j % 4
        for ri, src in enumerate((bbr, bbi)):
            st = stg.next()
            p.op("pool", lambda e: e.memset(st[:], 0.0), writes=[st])
            p.op("pool", lambda e: e.tensor_copy(out=st[0:64, 32 * j4:32 * j4 + 16], in_=src[0:64, j, :]), reads=[src], writes=[st], accum=True)
            p.op("pool", lambda e: e.tensor_copy(out=st[64:128, 32 * j4 + 16:32 * j4 + 32], in_=src[64:128, j, :]), reads=[src], writes=[st], accum=True)
            ps = ps_rot.next()
            p.op("pe", lambda e: e.transpose(out=ps[:, 0:128], in_=st[:], identity=identf[:]), reads=[st, identf], writes=[ps])
            p.op("act", lambda e: e.activation(out=WB[ri][j][:], in_=ps[:, 0:128], func=AF.Copy), reads=[ps], writes=[WB[ri][j]])
        for ri, src in enumerate((c_re, c_im)):
            w = WC[ri][j]
            p.op("pool", lambda e: e.memset(w[:], 0.0), writes=[w])
            p.op("pool", lambda e: e.tensor_copy(out=w[0:64, 32 * j4:32 * j4 + 16], in_=src[0:64, j, :]), reads=[src], writes=[w], accum=True)
            p.op("pool", lambda e: e.tensor_copy(out=w[64:128, 32 * j4 + 16:32 * j4 + 32], in_=src[64:128, j, :]), reads=[src], writes=[w], accum=True)

    NH = L // 2
    HN = NH // S5C
    big = lambda n, dt_=F32: p.sb([128, L], dt_, n)
    hpic = small("hpic", (128, 1))
    p.op("pool", lambda e: e.memset(hpic[:], PI / 2), writes=[hpic])

    def mkset(i):
        d_ = {nm: p.sb([128, NH], F32, f"{nm}{i}") for nm in ("sinT", "cosT", "arg", "bur", "bui", "wr", "wi", "ta", "tb")}
        d_["argi"] = p.sb([128, NH], I32, f"argi{i}")
        return d_

    sets = [mkset(0), mkset(1)]
    rB_rot = Rot([p.sb([128, NH], F32, f"rB{i}") for i in range(2)])
    xs = [[big(f"x{ri}_{j4}", BF16) for j4 in range(4)] for ri in range(2)]
    yo = Rot([big(f"yo{i}") for i in range(2)])
    evac = Evac(p)

    def wsin(out, x, tf, ti, shift=0.0):
        if shift != 0.0:
            p.op("act", lambda e: e.activation(out=tf[:], in_=x[:], func=AF.Identity, bias=hpic[:, 0:1]), reads=[x, hpic], writes=[tf])
            x = tf
        p.op("dve", lambda e: e.tensor_scalar(out=ti[:], in0=x[:], scalar1=1.0 / (2 * PI), scalar2=None, op0=ALU.mult), reads=[x], writes=[ti])
        p.op("act", lambda e: e.activation(out=out[:], in_=ti[:], func=AF.Copy), reads=[ti], writes=[out])
        p.op("dve", lambda e: e.scalar_tensor_tensor(out=tf[:], in0=out[:], scalar=-2 * PI, in1=x[:], op0=ALU.mult, op1=ALU.add),
             reads=[out, x], writes=[tf])
        ts(tf, tf, PI, -PI, ALU.min, ALU.max, eng="dve")
        p.op("act", lambda e: e.activation(out=out[:], in_=tf[:], func=AF.Sin), reads=[tf], writes=[out])

    def tables(st):
        j, hf = st // 2, st % 2
        S_ = sets[st % 2]
        t0 = hf * NH
        p.op("dve", lambda e: e.tensor_scalar(out=S_["arg"][:], in0=iot[:, t0:t0 + NH], scalar1=th[:, j:j + 1], scalar2=None, op0=ALU.mult),
             reads=[iot, th], writes=[S_["arg"]])
        wsin(S_["sinT"], S_["arg"], S_["tb"], S_["argi"])
        wsin(S_["cosT"], S_["arg"], S_["tb"], S_["argi"], shift=PI / 2)

    tables(0)
    step = 0
    prev = None
    for j in range(8):
        k, j4 = j // 4, j % 4
        rB = rB_rot.next()
        p.op("pool", lambda e: e.memset(rB[:], 1.0), writes=[rB])
        p.op("pool", lambda e: e.tensor_scalar(out=rB[:], in0=rB[:], scalar1=mag[:, j:j + 1], scalar2=None, op0=ALU.mult), reads=[rB, mag], writes=[rB])
        for hf in range(2):
            S_ = sets[step % 2]
            step += 1
            t0 = hf * NH
            sinT, cosT, arg, argi = S_["sinT"], S_["cosT"], S_["arg"], S_["argi"]
            bur, bui, wr, wi, ta, tb = S_["bur"], S_["bui"], S_["wr"], S_["wi"], S_["ta"], S_["tb"]
            for n in range(HN):
                sl = slice(n * S5C, (n + 1) * S5C)
                gl_ = slice(t0 + n * S5C, t0 + (n + 1) * S5C)
                for ri, dst in enumerate((bur, bui)):
                    ps = ps_rot.next()
                    p.op("pe", lambda e: e.matmul(ps[:, 0:S5C], lhsT=WB[ri][j][:], rhs=ub[:, k, gl_], start=True, stop=True),
                         reads=[WB[ri][j], ub], writes=[ps])
                    evac(dst[:, sl], ps[:, 0:S5C], [ps], [dst], first=(n == 0))
            tt(ta, cosT, bur, ALU.mult)
            tt(tb, sinT, bui, ALU.mult, eng=S5_ENG[0])
            tt(wr, ta, tb, ALU.add)
            tt(ta, cosT, bui, ALU.mult)
            tt(tb, sinT, bur, ALU.mult, eng=S5_ENG[1])
            tt(wi, ta, tb, ALU.subtract)
            if step < 16:
                tables(step)
            if hf == 0:
                ir, ii, rd = 0.0, 0.0, []
            else:
                ir, ii, rd = prev["bur"][:, NH - 1:NH], prev["bui"][:, NH - 1:NH], [prev["bur"], prev["bui"]]
            p.op("dve", lambda e: e.tensor_tensor_scan(out=bur[:], data0=rB[:], data1=wr[:], initial=ir, op0=ALU.mult, op1=ALU.add),
                 reads=[rB, wr] + rd, writes=[bur])
            p.op("dve", lambda e: e.tensor_tensor_scan(out=bui[:], data0=rB[:], data1=wi[:], initial=ii, op0=ALU.mult, op1=ALU.add),
                 reads=[rB, wi] + rd, writes=[bui])
            xr, xi = xs[0][j4], xs[1][j4]
            tt(ta, cosT, bur, ALU.mult)
            tt(tb, sinT, bui, ALU.mult, eng=S5_ENG[2])
            p.op("dve", lambda e: e.tensor_tensor(out=xr[:, t0:t0 + NH], in0=ta[:], in1=tb[:], op=ALU.subtract), reads=[ta, tb], writes=[xr], accum=(hf > 0))
            tt(wr, cosT, bui, ALU.mult)
            tt(wi, sinT, bur, ALU.mult, eng=S5_ENG[3])
            p.op("dve", lambda e: e.tensor_tensor(out=xi[:, t0:t0 + NH], in0=wr[:], in1=wi[:], op=ALU.add), reads=[wr, wi], writes=[xi], accum=(hf > 0))
            prev = S_
        if j4 == 3:
            y = yo.next()
            for n in range(S5N):
                sl = slice(n * S5C, (n + 1) * S5C)
                ps = ps_rot.next()
                i = 0
                for jj in range(4):
                    for ri in range(2):
                        p.op("pe", lambda e: e.matmul(ps[:, 0:S5C], lhsT=WC[ri][4 * k + jj][:], rhs=xs[ri][jj][:, sl], start=(i == 0), stop=(i == 7)),
                             reads=[WC[ri][4 * k + jj], xs[ri][jj]], writes=[ps], accum=(i > 0))
                        i += 1
                p.op("dve", lambda e: e.scalar_tensor_tensor(out=y[:, sl], in0=u[:, k, sl], scalar=dsk[:, k:k + 1], in1=ps[:, 0:S5C],
                                                             op0=ALU.mult, op1=ALU.add), reads=[u, dsk, ps], writes=[y], accum=(n > 0))
            for hf in range(2):
                ta = sets[hf]["wr"]
                ysl = y[:, hf * NH:(hf + 1) * NH]
                p.op("pool", lambda e: e.tensor_tensor(out=ta[:], in0=ysl, in1=ysl, op=ALU.mult), reads=[y], writes=[ta])
                p.op("pool", lambda e: e.tensor_tensor(out=ta[:], in0=ta[:], in1=ysl, op=ALU.mult), reads=[y, ta], writes=[ta])
                p.op("dve", lambda e: e.scalar_tensor_tensor(out=ta[:], in0=ta[:], scalar=0.044715, in1=ysl, op0=ALU.mult, op1=ALU.add),
                     reads=[ta, y], writes=[ta])
                p.op("act", lambda e: e.activation(out=ta[:], in_=ta[:], func=AF.Sigmoid, scale=1.5957691216), reads=[ta], writes=[ta])
                p.op("dve", lambda e: e.tensor_tensor(out=ysl, in0=ysl, in1=ta[:], op=ALU.mult), reads=[y, ta], writes=[y], accum=(hf > 0))
            prev = None
            p.dma("sp", yT[k], y[:], reads=[y])
    return yo.tiles


def build_B():
    nc = bass.Bass("TRN2", target_bir_lowering=False)
    p = P(nc)
    outs = []
    for pre, fn in (("b1_", emit_B1), ("b2_", emit_B2), ("b3_", emit_B3)):
        p.prefix = pre
        m = p.mark()
        outs += fn(nc, p)
        p.release(m)
    p.finish(outs)
    return nc


NQT = 17
DH = 128
DV = 256


def build_B3():
    nc = bass.Bass("TRN2", target_bir_lowering=False)
    p = P(nc)
    outs = emit_B3(nc, p)
    p.finish(outs)
    return nc


def emit_B3(nc, p):
    qT_d = nc.dram_tensor("qT", [4, 128, L], F32, kind="ExternalInput").ap()
    kT_d = nc.dram_tensor("kT", [4, 128, L], F32, kind="ExternalInput").ap()
    v_d = nc.dram_tensor("v", [128, 2, NQT, DV], F32, kind="ExternalInput").ap()
    lam_d = nc.dram_tensor("lamv", [128, 4, DH], F32, kind="ExternalInput").ap()
    sw_d = nc.dram_tensor("subw", [128, DV], F32, kind="ExternalInput").ap()
    li_d = nc.dram_tensor("linit", [128, 2], F32, kind="ExternalInput").ap()
    oc = nc.dram_tensor("oc", [128, 2, NQT, DV], F32, kind="ExternalOutput").ap()

    qT = p.sb([128, 4, L], BF16, "qT_s")
    kT = p.sb([128, 4, L], BF16, "kT_s")
    V = p.sb([128, 2, NQT, DV + 1], BF16, "V")
    lamv = p.sb([128, 4, DH], F32, "lamv_s")
    subw = p.sb([128, DV], F32, "subw_s")
    linit = p.sb([128, 2], F32, "linit_s")
    epsc = p.sb([128, 1], F32, "epsc")
    p.op("pool", lambda e: e.memset(epsc[:], EPS), writes=[epsc])
    p.op("pool", lambda e: e.memset(V[:], 1.0), writes=[V])
    p.dma("pool", V[:, :, :, 0:DV], v_d[:, :, :, :], writes=[V])
    for i in range(4):
        p.dma("pool", qT[:, i, :], qT_d[i], writes=[qT], concurrent=True)
        p.dma("pool", kT[:, i, :], kT_d[i], writes=[kT], concurrent=True)
    p.dma("sp", lamv[:], lam_d[:, :, :], writes=[lamv])
    p.dma("sp", subw[:], sw_d[:, :], writes=[subw])
    p.dma("sp", linit[:], li_d[:, :], writes=[linit])
    lt = p.sb([128, 2, DH], F32, "lt")
    ls = p.sb([128, 2], F32, "ls")
    nlam = p.sb([128, 1], F32, "nlam")
    p.op("dve", lambda e: e.tensor_tensor(out=lt[:, 0, :], in0=lamv[:, 0, :], in1=lamv[:, 1, :], op=ALU.mult), reads=[lamv], writes=[lt])
    p.op("dve", lambda e: e.tensor_tensor(out=lt[:, 1, :], in0=lamv[:, 2, :], in1=lamv[:, 3, :], op=ALU.mult), reads=[lamv], writes=[lt], accum=True)
    p.op("dve", lambda e: e.tensor_reduce(out=ls[:], in_=lt[:], axis=AX.X, op=ALU.add), reads=[lt], writes=[ls])
    p.op("act", lambda e: e.activation(out=ls[:], in_=ls[:], func=AF.Exp), reads=[ls], writes=[ls])
    p.op("dve", lambda e: e.tensor_tensor(out=nlam[:], in0=ls[:, 1:2], in1=ls[:, 0:1], op=ALU.subtract), reads=[ls], writes=[nlam])
    p.op("dve", lambda e: e.tensor_tensor(out=nlam[:], in0=nlam[:], in1=linit[:, 0:1], op=ALU.subtract), reads=[nlam, linit], writes=[nlam])

    acc = [p.sb([128, NQT, DV], F32, f"acc{h}") for h in range(2)]
    psS = Rot([p.ps([128, 512], F32, f"psS{i}") for i in range(3)])
    psO = [p.ps([128, 512], F32, f"psO{i}") for i in range(4)]
    PT = Rot([p.sb([128, 512], BF16, f"PT{i}") for i in range(4)])
    rl = Rot([p.sb([128, 1], F32, f"rl{i}") for i in range(4)])
    on = Rot([p.sb([128, DV], F32, f"on{i}") for i in range(2)])
    scale = DH ** -0.5

    def tok0(i):
        return (0, 16) if i == 0 else (16 + 128 * (i - 1), 128)

    def finish_tile(h, s, i, po, nq):
        r = rl.next()
        p.op("dve", lambda e: e.reciprocal(out=r[0:nq, :], in_=po[0:nq, DV:DV + 1]), reads=[po], writes=[r])
        if s == 0:
            p.op("act", lambda e: e.activation(out=acc[h][0:nq, i, :], in_=po[0:nq, 0:DV], func=AF.Copy, scale=r[0:nq, 0:1]),
                 reads=[po, r], writes=[acc[h]], accum=True)
        else:
            o_ = on.next()
            p.op("act", lambda e: e.activation(out=o_[0:nq, :], in_=po[0:nq, 0:DV], func=AF.Copy, scale=r[0:nq, 0:1]),
                 reads=[po, r], writes=[o_])
            p.op("dve", lambda e: e.scalar_tensor_tensor(out=acc[h][0:nq, i, :], in0=o_[0:nq, :], scalar=nlam[0:nq, 0:1],
                                                         in1=acc[h][0:nq, i, :], op0=ALU.mult, op1=ALU.add),
                 reads=[o_, nlam, acc[h]], writes=[acc[h]])

    for h in range(2):
        p.op("pool", lambda e: e.memset(acc[h][:], 0.0), writes=[acc[h]])
    for h in range(2):
        for s in range(2):
            hs = 2 * h + s
            ps = psS.next()
            p.op("pe", lambda e: e.matmul(ps[0:16, 0:16], lhsT=kT[:, hs, 0:16], rhs=qT[:, hs, 0:16], start=True, stop=True),
                 reads=[kT, qT], writes=[ps])
            pt = PT.next()
            p.op("act", lambda e: e.activation(out=pt[0:16, 0:16], in_=ps[0:16, 0:16], func=AF.Exp, scale=scale), reads=[ps], writes=[pt])
            po = psO[0]
            p.op("pe", lambda e: e.matmul(po[0:16, 0:DV + 1], lhsT=pt[0:16, 0:16], rhs=V[0:16, h, 0, :], start=True, stop=True),
                 reads=[pt, V], writes=[po])
            finish_tile(h, s, 0, po, 16)
            for g in range(4):
                q0 = 16 + 512 * g
                tiles = [4 * g + 1 + a for a in range(4)]
                js = list(range(0, 4 * g + 5))

                def scores(j):
                    k0, nk = tok0(j)
                    ps = psS.next()
                    p.op("pe", lambda e: e.matmul(ps[0:nk, 0:512], lhsT=kT[:, hs, k0:k0 + nk], rhs=qT[:, hs, q0:q0 + 512], start=True, stop=True),
                         reads=[kT, qT], writes=[ps])
                    pt = PT.next()
                    p.op("act", lambda e: e.activation(out=pt[0:nk, :], in_=ps[0:nk, 0:512], func=AF.Exp, scale=scale), reads=[ps], writes=[pt])
                    if j in tiles:
                        a = tiles.index(j)
                        p.op("pool", lambda e: e.memset(pt[64:128, 128 * a:128 * a + 64], 0.0), reads=[pt], writes=[pt])
                    return pt

                nxt = scores(js[0])
                for idx, j in enumerate(js):
                    pt = nxt
                    if idx + 1 < len(js):
                        nxt = scores(js[idx + 1])
                    k0, nk = tok0(j)
                    for a, i in enumerate(tiles):
                        if i < j:
                            continue
                        p.op("pe", lambda e: e.matmul(psO[a][:, 0:DV + 1], lhsT=pt[0:nk, 128 * a:128 * (a + 1)], rhs=V[0:nk, h, j, :],
                                                      start=(j == 0), stop=(j == i)), reads=[pt, V], writes=[psO[a]], accum=(j > 0))
                        if j == i:
                            finish_tile(h, s, i, psO[a], 128)
    sq = p.sb([128, NQT, DV], F32, "sq")
    ms = p.sb([128, NQT], F32, "ms")
    for h in range(2):
        a = acc[h]
        p.op("pool", lambda e: e.tensor_tensor(out=sq[:], in0=a[:], in1=a[:], op=ALU.mult), reads=[a], writes=[sq])
        p.op("dve", lambda e: e.tensor_reduce(out=ms[:], in_=sq[:], axis=AX.X, op=ALU.add), reads=[sq], writes=[ms])
        p.op("act", lambda e: e.activation(out=ms[:], in_=ms[:], func=AF.Sqrt, bias=epsc[:, 0:1], scale=1.0 / DV), reads=[ms, epsc], writes=[ms])
        p.op("dve", lambda e: e.reciprocal(out=ms[:], in_=ms[:]), reads=[ms], writes=[ms])
        p.op("dve", lambda e: e.tensor_scalar(out=ms[:], in0=ms[:], scalar1=linit[:, 1:2], scalar2=None, op0=ALU.mult), reads=[ms, linit], writes=[ms])
        p.op("dve", lambda e: e.tensor_tensor(out=a[:], in0=a[:], in1=ms[:].unsqueeze(2).to_broadcast([128, NQT, DV]), op=ALU.mult),
             reads=[a, ms], writes=[a])
        p.op("pool", lambda e: e.tensor_tensor(out=a[:], in0=a[:], in1=subw[:].unsqueeze(1).to_broadcast([128, NQT, DV]), op=ALU.mult),
             reads=[a, subw], writes=[a])
        p.dma("sp", oc[:, h, :, :], a[:], reads=[a])
    return acc


NTP = 1152
NTT = NTP // 128
TCP = NTP // 3
NE = 64
NR = 72
BIGNEG = -1.0e30


def build_C1(gather=True):
    nc = bass.Bass("TRN2", target_bir_lowering=False)
    p = P(nc)
    zT = nc.dram_tensor("zT", [D, NT], F32, kind="ExternalInput").ap()
    oaT = nc.dram_tensor("oaT", [512, NT], F32, kind="ExternalInput").ap()
    ysT = nc.dram_tensor("ysT", [512, NT], F32, kind="ExternalInput").ap()
    ocT = nc.dram_tensor("ocT", [1024, NT], F32, kind="ExternalInput").ap()
    wglu_d = nc.dram_tensor("w_glu", [512, 1024], F32, kind="ExternalInput").ap()
    bglu_d = nc.dram_tensor("b_glu", [128, 8], F32, kind="ExternalInput").ap()
    wout_d = nc.dram_tensor("w_out", [D, D], F32, kind="ExternalInput").ap()
    ln2_d = nc.dram_tensor("ln2", [128, KC], F32, kind="ExternalInput").ap()
    wr_d = nc.dram_tensor("w_r", [D, NR], F32, kind="ExternalInput").ap()
    br_d = nc.dram_tensor("b_r", [128, NR], F32, kind="ExternalInput").ap()
    z1T = nc.dram_tensor("z1T", [D, NT], F32, kind="ExternalOutput").ap()
    if gather:
        xe_d = nc.dram_tensor("xeT", [NE, D, NSL], BF16, kind="ExternalOutput").ap()
    else:
        xtm_d = nc.dram_tensor("xtm", [128, NTT, D], BF16, kind="ExternalOutput").ap()
        rankp_d = nc.dram_tensor("rankp", [128, NTT, NE], F32, kind="ExternalOutput").ap()
    slotT_d = nc.dram_tensor("slotT", [NE, NTP], F32, kind="ExternalOutput").ap()
    gtT_d = nc.dram_tensor("gtT", [NE, NTP], F32, kind="ExternalOutput").ap()

    xtm = p.sb([128, NTT, D], BF16, "xtm_s")
    rankp = p.sb([128, NTT, NE], F32, "rankp_s")
    slotT = p.sb([NE, NTP], F32, "slotT_s")
    gtT = p.sb([NE, NTP], F32, "gtT_s")
    iota_s = p.sb([128, 128], F32, "iota_s")
    ps_rot = Rot([p.ps([128, 512], F32, f"ps{i}") for i in range(6)])
    psb_rot = Rot([p.ps([128, 512], BF16, f"psb{i}") for i in range(2)])
    mk = p.mark()
    z = p.sb([128, KC, NTP], F32, "z")
    mx = p.sb([128, KC, NTP], BF16, "mx")
    bglu = p.sb([128, 8], F32, "bglu_s")
    ln2 = p.sb([128, KC], F32, "ln2_s")
    wr = p.sb([128, KC, NR], F32, "wr_s")
    br = p.sb([128, NR], F32, "br_s")
    rstd = p.sb([128, NTP], F32, "rstd")
    ones_bf = p.sb([128, 128], BF16, "ones_bf")
    onesD = p.sb([128, 128], BF16, "onesD")
    epsc = p.sb([128, 1], F32, "epsc")
    identf = p.sb([128, 128], F32, "identf")
    identb = p.sb([128, 128], BF16, "identb")
    ustr = p.sb([128, 128], BF16, "ustr")
    vmask = p.sb([128, NTT], F32, "vmask")
    evac = Evac(p)
    ys = p.sb([128, 4, NTP], BF16, "ys")
    wg = p.sb([128, 4, 1024], BF16, "wg")
    sg_rot = Rot([p.sb([128, TCP], F32, f"sg{i}") for i in range(2)])
    wb_rot = Rot([p.sb([128, KC, 512], BF16, f"wb{i}") for i in range(1)])

    p.op("pool", lambda e: e.memset(z[:, :, NT:NTP], 0.0), writes=[z])
    p.op("pool", lambda e: e.memset(mx[:, :, NT:NTP], 0.0), writes=[mx])
    p.op("pool", lambda e: e.memset(ys[:, :, NT:NTP], 0.0), writes=[ys])
    zv = zT.rearrange("(c p) t -> p c t", p=128)
    for q in range(4):
        p.dma("sp", z[:, 4 * q:4 * q + 4, 0:NT], zv[:, 4 * q:4 * q + 4, :], writes=[z], concurrent=True)
    p.dma("pool", ys[:, :, 0:NT], ysT.rearrange("(c p) t -> p c t", p=128), writes=[ys])
    p.dma("pool", wg[:], wglu_d.rearrange("(c p) n -> p c n", p=128), writes=[wg])
    p.dma("pool", mx[:, 0:4, 0:NT], oaT.rearrange("(c p) t -> p c t", p=128), writes=[mx], concurrent=True)
    p.dma("pool", mx[:, 8:16, 0:NT], ocT.rearrange("(c p) t -> p c t", p=128), writes=[mx], concurrent=True)
    for t_, d_ in ((bglu, bglu_d), (ln2, ln2_d), (br, br_d)):
        p.dma("sp", t_[:], d_[:, :], writes=[t_])
    p.dma("sp", wr[:], wr_d.rearrange("(c p) n -> p c n", p=128), writes=[wr])
    p.op("pool", lambda e: e.memset(ones_bf[:], 1.0), writes=[ones_bf])
    p.op("pool", lambda e: e.memset(onesD[:], 1.0 / D), writes=[onesD])
    p.op("pool", lambda e: e.memset(epsc[:], EPS), writes=[epsc])
    p.op("pool", lambda e: e.memset(identf[:], 0.0), writes=[identf])
    p.op("pool", lambda e: e.affine_select(out=identf[:], in_=identf[:], pattern=[[-1, 128]], compare_op=ALU.not_equal,
                                           fill=1.0, base=0, channel_multiplier=1), reads=[identf], writes=[identf])
    p.op("pool", lambda e: e.tensor_copy(out=identb[:], in_=identf[:]), reads=[identf], writes=[identb])
    p.op("pool", lambda e: e.memset(ustr[:], 1.0), writes=[ustr])
    p.op("pool", lambda e: e.affine_select(out=ustr[:], in_=ustr[:], pattern=[[1, 128]], compare_op=ALU.is_ge, fill=0.0,
                                           base=-1, channel_multiplier=-1), reads=[ustr], writes=[ustr])
    p.op("pool", lambda e: e.memset(vmask[:], 1.0), writes=[vmask])
    p.op("pool", lambda e: e.affine_select(out=vmask[:], in_=vmask[:], pattern=[[-128, NTT]], compare_op=ALU.is_ge, fill=0.0,
                                           base=NT - 1, channel_multiplier=-1), reads=[vmask], writes=[vmask])

    for m in range(4):
        for n in range(3):
            sl = slice(n * TCP, (n + 1) * TCP)
            pv, pg = ps_rot.next(), ps_rot.next()
            for c in range(4):
                p.op("pe", lambda e: e.matmul(pv[:, 0:TCP], lhsT=wg[:, c, m * 128:(m + 1) * 128], rhs=ys[:, c, sl], start=(c == 0), stop=(c == 3)),
                     reads=[wg, ys], writes=[pv], accum=(c > 0))
            for c in range(4):
                p.op("pe", lambda e: e.matmul(pg[:, 0:TCP], lhsT=wg[:, c, 512 + m * 128:512 + (m + 1) * 128], rhs=ys[:, c, sl], start=(c == 0), stop=(c == 3)),
                     reads=[wg, ys], writes=[pg], accum=(c > 0))
            sg = sg_rot.next()
            p.op("act", lambda e: e.activation(out=sg[:], in_=pg[:, 0:TCP], func=AF.Sigmoid, bias=bglu[:, 4 + m:5 + m]), reads=[pg, bglu], writes=[sg])
            p.op("dve", lambda e: e.scalar_tensor_tensor(out=mx[:, 4 + m, sl], in0=pv[:, 0:TCP], scalar=bglu[:, m:m + 1], in1=sg[:],
                                                         op0=ALU.add, op1=ALU.mult), reads=[pv, bglu, sg], writes=[mx], accum=True)
    wo_v = wout_d.rearrange("(c p) n -> p c n", p=128)
    for g in range(4):
        wb = wb_rot.next()
        p.dma("pool", wb[:], wo_v[:, :, g * 512:(g + 1) * 512], writes=[wb])
        for j in range(4):
            ct = g * 4 + j
            for n in range(3):
                sl = slice(n * TCP, (n + 1) * TCP)
                ps = ps_rot.next()
                for c in range(KC):
                    p.op("pe", lambda e: e.matmul(ps[:, 0:TCP], lhsT=wb[:, c, j * 128:(j + 1) * 128], rhs=mx[:, c, sl], start=(c == 0), stop=(c == KC - 1)),
                         reads=[wb, mx], writes=[ps], accum=(c > 0))
                p.op("dve", lambda e: e.tensor_tensor(out=z[:, ct, sl], in0=z[:, ct, sl], in1=ps[:, 0:TCP], op=ALU.add), reads=[z, ps], writes=[z])
    z1v = z1T.rearrange("(c p) t -> p c t", p=128)
    for q in range(4):
        p.dma("sp", z1v[:, 4 * q:4 * q + 4, :], z[:, 4 * q:4 * q + 4, 0:NT], reads=[z])
    p.barrier()
    p.free([ys, wg] + sg_rot.tiles + wb_rot.tiles)
    hn2 = mx
    sq_rot = Rot([p.sb([128, NTP], BF16, f"sq{i}") for i in range(2)])
    emit_rmsnorm(p, z, ln2, hn2, onesD, epsc, sq_rot, ps_rot, rstd, nt=NTP, tch=TCP)
    for i in range(NTT):
        for cq in range(4):
            pb = psb_rot.next()
            for cc in range(4):
                c = cq * 4 + cc
                p.op("pe", lambda e: e.transpose(out=pb[:, cc * 128:(cc + 1) * 128], in_=hn2[:, c, i * 128:(i + 1) * 128], identity=identb[:]),
                     reads=[hn2, identb], writes=[pb], accum=(cc > 0))
            evac(xtm[:, i, cq * 512:(cq + 1) * 512], pb[:, 0:512], [pb], [xtm], first=False)
    if not gather:
        p.dma("sp", xtm_d[:, :, :], xtm[:], reads=[xtm])
    p.op("dve", lambda e: e.tensor_tensor(out=wr[:], in0=wr[:], in1=ln2[:].unsqueeze(2).to_broadcast([128, KC, NR]), op=ALU.mult),
         reads=[wr, ln2], writes=[wr])
    rstd_tm = p.sb([128, NTT], F32, "rstd_tm")
    lg = p.sb([128, NTT, NR], F32, "lg")
    for i in range(NTT):
        pt = ps_rot.next()
        p.op("pe", lambda e: e.transpose(out=pt[:, 0:128], in_=rstd[:, i * 128:(i + 1) * 128], identity=identf[:]), reads=[rstd, identf], writes=[pt])
        p.op("act", lambda e: e.activation(out=rstd_tm[:, i:i + 1], in_=pt[:, 0:1], func=AF.Copy), reads=[pt], writes=[rstd_tm], accum=True)
        ps = ps_rot.next()
        for c in range(KC):
            p.op("pe", lambda e: e.matmul(ps[:, 0:NR], lhsT=z[:, c, i * 128:(i + 1) * 128], rhs=wr[:, c, :], start=(c == 0), stop=(c == KC - 1)),
                 reads=[z, wr], writes=[ps], accum=(c > 0))
        p.op("dve", lambda e: e.scalar_tensor_tensor(out=lg[:, i, :], in0=ps[:, 0:NR], scalar=rstd_tm[:, i:i + 1], in1=br[:],
                                                     op0=ALU.mult, op1=ALU.add), reads=[ps, rstd_tm, br], writes=[lg], accum=True)

    def sbf(name, shape):
        return p.sb(list(shape), F32, name)

    def tt(out_ap, in0_ap, in1_ap, op, reads, writes, eng="dve"):
        p.op(eng, lambda e: e.tensor_tensor(out=out_ap, in0=in0_ap, in1=in1_ap, op=op), reads=reads, writes=writes)

    def red(out, in_ap, op, reads):
        p.op("dve", lambda e: e.tensor_reduce(out=out[:], in_=in_ap, axis=AX.X, op=op), reads=reads, writes=[out])

    gl = lg[:, :, 0:8]
    gmax, gsum, pg_ = sbf("gmax", (128, NTT)), sbf("gsum", (128, NTT)), sbf("pgrp", (128, NTT))
    ge, oh = sbf("ge", (128, NTT, 8)), sbf("oh", (128, NTT, 8))
    red(gmax, gl, ALU.max, [lg])
    b8 = lambda t_: t_[:].unsqueeze(2).to_broadcast([128, NTT, 8])
    tt(ge[:], gl, b8(gmax), ALU.subtract, [lg, gmax], [ge])
    tt(oh[:], gl, b8(gmax), ALU.is_equal, [lg, gmax], [oh])
    p.op("act", lambda e: e.activation(out=ge[:], in_=ge[:], func=AF.Exp), reads=[ge], writes=[ge])
    red(gsum, ge[:], ALU.add, [ge])
    p.op("dve", lambda e: e.reciprocal(out=pg_[:], in_=gsum[:]), reads=[gsum], writes=[pg_])
    el4 = sbf("el4", (128, NTT, 8, 8))
    esel = sbf("esel", (128, NTT, 8))
    lg4 = lg[:, :, 8:NR].rearrange("p t (g j) -> p t g j", j=8)
    tt(el4[:], lg4, oh[:].unsqueeze(3).to_broadcast([128, NTT, 8, 8]), ALU.mult, [lg, oh], [el4])
    red(esel, el4[:].rearrange("p t g j -> p t j g"), ALU.add, [el4])
    m1, m2 = sbf("m1", (128, NTT)), sbf("m2", (128, NTT))
    k1, k2, e2 = sbf("k1", (128, NTT, 8)), sbf("k2", (128, NTT, 8)), sbf("e2", (128, NTT, 8))
    red(m1, esel[:], ALU.max, [esel])
    tt(k1[:], esel[:], b8(m1), ALU.is_equal, [esel, m1], [k1])
    p.op("dve", lambda e: e.scalar_tensor_tensor(out=e2[:], in0=k1[:], scalar=BIGNEG, in1=esel[:], op0=ALU.mult, op1=ALU.add),
         reads=[k1, esel], writes=[e2])
    red(m2, e2[:], ALU.max, [e2])
    tt(k2[:], e2[:], b8(m2), ALU.is_equal, [e2, m2], [k2])
    dd, g1, g2 = sbf("dd", (128, NTT)), sbf("g1", (128, NTT)), sbf("g2", (128, NTT))
    tt(dd[:], m2[:], m1[:], ALU.subtract, [m2, m1], [dd])
    p.op("act", lambda e: e.activation(out=dd[:], in_=dd[:], func=AF.Exp), reads=[dd], writes=[dd])
    p.op("dve", lambda e: e.tensor_scalar(out=dd[:], in0=dd[:], scalar1=1.0, scalar2=None, op0=ALU.add), reads=[dd], writes=[dd])
    p.op("dve", lambda e: e.reciprocal(out=dd[:], in_=dd[:]), reads=[dd], writes=[dd])
    tt(g1[:], pg_[:], dd[:], ALU.mult, [pg_, dd], [g1])
    tt(g2[:], pg_[:], g1[:], ALU.subtract, [pg_, g1], [g2])
    tt(oh[:], oh[:], b8(vmask), ALU.mult, [oh, vmask], [oh])
    gj, aj = sbf("gj", (128, NTT, 8)), sbf("aj", (128, NTT, 8))
    tt(gj[:], k1[:], b8(g1), ALU.mult, [k1, g1], [gj])
    tt(e2[:], k2[:], b8(g2), ALU.mult, [k2, g2], [e2])
    tt(gj[:], gj[:], e2[:], ALU.add, [gj, e2], [gj])
    tt(aj[:], k1[:], k2[:], ALU.add, [k1, k2], [aj])
    Gt, Af = sbf("Gt", (128, NTT, 8, 8)), sbf("Af", (128, NTT, 8, 8))
    Ab = p.sb([128, NTT, NE], BF16, "Ab")
    ohb = oh[:].unsqueeze(3).to_broadcast([128, NTT, 8, 8])
    tt(Gt[:], ohb, gj[:].unsqueeze(2).to_broadcast([128, NTT, 8, 8]), ALU.mult, [oh, gj], [Gt])
    tt(Af[:], ohb, aj[:].unsqueeze(2).to_broadcast([128, NTT, 8, 8]), ALU.mult, [oh, aj], [Af])
    Af2 = Af[:].rearrange("p t g j -> p t (g j)")
    Gt2 = Gt[:].rearrange("p t g j -> p t (g j)")
    p.op("dve", lambda e: e.tensor_copy(out=Ab[:], in_=Af2), reads=[Af], writes=[Ab])
    AT = sbf("AT_s", (NE, NTP))
    for i in range(NTT):
        ps = ps_rot.next()
        for i2 in range(i + 1):
            p.op("pe", lambda e: e.matmul(ps[:, 0:NE], lhsT=(ustr[:] if i2 == i else ones_bf[:]), rhs=Ab[:, i2, :], start=(i2 == 0), stop=(i2 == i)),
                 reads=[ustr, ones_bf, Ab], writes=[ps], accum=(i2 > 0))
        p.op("dve", lambda e: e.scalar_tensor_tensor(out=rankp[:, i, :], in0=ps[:, 0:NE], scalar=1.0, in1=Af2[:, i, :], op0=ALU.add, op1=ALU.mult),
             reads=[ps, Af], writes=[rankp], accum=True)
        ps2 = ps_rot.next()
        for i2 in range(i + 1):
            p.op("pe", lambda e: e.matmul(ps2[0:NE, 0:128], lhsT=Ab[:, i2, :], rhs=(ustr[:] if i2 == i else ones_bf[:]), start=(i2 == 0), stop=(i2 == i)),
                 reads=[ustr, ones_bf, Ab], writes=[ps2], accum=(i2 > 0))
        p.op("act", lambda e: e.activation(out=slotT[:, i * 128:(i + 1) * 128], in_=ps2[0:NE, 0:128], func=AF.Copy), reads=[ps2], writes=[slotT], accum=True)
        ps3 = ps_rot.next()
        p.op("pe", lambda e: e.transpose(out=ps3[0:NE, 0:128], in_=Af2[:, i, :], identity=identf[:]), reads=[Af, identf], writes=[ps3])
        p.op("act", lambda e: e.activation(out=AT[:, i * 128:(i + 1) * 128], in_=ps3[0:NE, 0:128], func=AF.Copy), reads=[ps3], writes=[AT], accum=True)
        ps4 = ps_rot.next()
        p.op("pe", lambda e: e.transpose(out=ps4[0:NE, 0:128], in_=Gt2[:, i, :], identity=identf[:]), reads=[Gt, identf], writes=[ps4])
        p.op("act", lambda e: e.activation(out=gtT[:, i * 128:(i + 1) * 128], in_=ps4[0:NE, 0:128], func=AF.Copy), reads=[ps4], writes=[gtT], accum=True)
    p.op("dve", lambda e: e.tensor_scalar(out=rankp[:], in0=rankp[:], scalar1=-1.0, scalar2=None, op0=ALU.add), reads=[rankp], writes=[rankp])
    p.op("dve", lambda e: e.scalar_tensor_tensor(out=slotT[:], in0=slotT[:], scalar=1.0, in1=AT[:], op0=ALU.add, op1=ALU.mult),
         reads=[slotT, AT], writes=[slotT])
    p.op("dve", lambda e: e.tensor_scalar(out=slotT[:], in0=slotT[:], scalar1=-1.0, scalar2=None, op0=ALU.add), reads=[slotT], writes=[slotT])
    p.dma("sp", slotT_d[:, :], slotT[:], reads=[slotT])
    p.dma("sp", gtT_d[:, :], gtT[:], reads=[gtT])
    if not gather:
        p.dma("sp", rankp_d[:, :, :], rankp[:], reads=[rankp])
        p.finish([z, xtm, rankp, slotT, gtT])
        return nc
    p.release(mk)
    p.op("pool", lambda e: e.iota(iota_s[:], pattern=[[1, 128]], base=0, channel_multiplier=0, allow_small_or_imprecise_dtypes=True), writes=[iota_s])
    S_rot = Rot([p.sb([128, NTT, 4, 128], BF16, f"S{i}") for i in range(2)])
    XeT_rot = Rot([p.sb([128, KC, 512], BF16, f"XeT{i}") for i in range(2)])
    for qd in range(NE // 4):
        e0 = 4 * qd
        S = S_rot.next()
        p.op("dve", lambda e: e.tensor_tensor(out=S[:], in0=iota_s[:].unsqueeze(1).unsqueeze(1).to_broadcast([128, NTT, 4, 128]),
                                              in1=rankp[:, :, e0:e0 + 4].unsqueeze(3).to_broadcast([128, NTT, 4, 128]), op=ALU.is_equal),
             reads=[iota_s, rankp], writes=[S])
        S2 = S[:].rearrange("p t e s -> p t (e s)")
        XeT = XeT_rot.next()
        for c in range(KC):
            ps = ps_rot.next()
            for i in range(NTT):
                p.op("pe", lambda e: e.matmul(ps[:, 0:512], lhsT=xtm[:, i, c * 128:(c + 1) * 128], rhs=S2[:, i, :], start=(i == 0), stop=(i == NTT - 1)),
                     reads=[xtm, S], writes=[ps], accum=(i > 0))
            evac(XeT[:, c, :], ps[:, 0:512], [ps], [XeT], first=(c == 0))
        for ee in range(4):
            p.dma("sp", xe_d[e0 + ee].rearrange("(c p) s -> p c s", p=128), XeT[:, :, ee * 128:(ee + 1) * 128], reads=[XeT])
    p.finish([z, slotT, gtT] + XeT_rot.tiles)
    return nc


DF = 512
SC = NT // 3


NSL = 128
NSRC = 8
ESL = NSRC * NSL
EPC = NE // 8


def build_C2a():
    nc = bass.Bass("TRN2", target_bir_lowering=False)
    p = P(nc)
    xtm_d = nc.dram_tensor("xtm", [128, NTT, D], BF16, kind="ExternalInput").ap()
    rankp_d = nc.dram_tensor("rankp", [128, NTT, NE], F32, kind="ExternalInput").ap()
    xe_d = nc.dram_tensor("xeT", [NE, D, NSL], BF16, kind="ExternalOutput").ap()
    X = p.sb([128, NTT, D], BF16, "X")
    rankp = p.sb([128, NTT, NE], F32, "rankp_s")
    iota_s = p.sb([128, 128], F32, "iota_s")
    p.dma("sp", X[:], xtm_d[:, :, :], writes=[X])
    p.dma("sp", rankp[:], rankp_d[:, :, :], writes=[rankp])
    p.op("pool", lambda e: e.iota(iota_s[:], pattern=[[1, 128]], base=0, channel_multiplier=0, allow_small_or_imprecise_dtypes=True), writes=[iota_s])
    S_rot = Rot([p.sb([128, NTT, 4, 128], BF16, f"S{i}") for i in range(2)])
    XeT_rot = Rot([p.sb([128, KC, 512], BF16, f"XeT{i}") for i in range(2)])
    ps_rot = Rot([p.ps([128, 512], F32, f"ps{i}") for i in range(8)])
    evac = Evac(p)
    for qd in range(NE // 4):
        e0 = 4 * qd
        S = S_rot.next()
        p.op("dve", lambda e: e.tensor_tensor(out=S[:], in0=iota_s[:].unsqueeze(1).unsqueeze(1).to_broadcast([128, NTT, 4, 128]),
                                              in1=rankp[:, :, e0:e0 + 4].unsqueeze(3).to_broadcast([128, NTT, 4, 128]), op=ALU.is_equal),
             reads=[iota_s, rankp], writes=[S])
        S2 = S[:].rearrange("p t e s -> p t (e s)")
        XeT = XeT_rot.next()
        for c in range(KC):
            ps = ps_rot.next()
            for i in range(NTT):
                p.op("pe", lambda e: e.matmul(ps[:, 0:512], lhsT=X[:, i, c * 128:(c + 1) * 128], rhs=S2[:, i, :], start=(i == 0), stop=(i == NTT - 1)),
                     reads=[X, S], writes=[ps], accum=(i > 0))
            evac(XeT[:, c, :], ps[:, 0:512], [ps], [XeT], first=(c == 0))
        for ee in range(4):
            p.dma("sp", xe_d[e0 + ee].rearrange("(c p) s -> p c s", p=128), XeT[:, :, ee * 128:(ee + 1) * 128], reads=[XeT])
    p.finish(XeT_rot.tiles)
    return nc


def build_C2b():
    nc = bass.Bass("TRN2", target_bir_lowering=False)
    p = P(nc)
    xe_d = nc.dram_tensor("xe", [EPC, D, ESL], BF16, kind="ExternalInput").ap()
    w1_d = nc.dram_tensor("w1", [EPC, D, DF], F32, kind="ExternalInput").ap()
    w3_d = nc.dram_tensor("w3", [EPC, D, DF], F32, kind="ExternalInput").ap()
    w2_d = nc.dram_tensor("w2", [EPC, DF, D], F32, kind="ExternalInput").ap()
    ye_d = nc.dram_tensor("ye", [EPC, ESL, D], BF16, kind="ExternalOutput").ap()
    W1 = Rot([p.sb([128, KC, DF], BF16, f"W1_{i}") for i in range(2)])
    W3 = Rot([p.sb([128, KC, DF], BF16, f"W3_{i}") for i in range(2)])
    W2 = Rot([p.sb([128, 4, D], BF16, f"W2_{i}") for i in range(2)])
    Xe = Rot([p.sb([128, KC, ESL], BF16, f"Xe{i}") for i in range(2)])
    Hg = Rot([p.sb([128, 4, 512], BF16, f"Hg{i}") for i in range(2)])
    h1 = Rot([p.sb([128, 512], F32, f"h1_{i}") for i in range(2)])
    yo = Rot([p.sb([128, D], BF16, f"yo{i}") for i in range(3)])
    ps_rot = Rot([p.ps([128, 512], F32, f"ps{i}") for i in range(8)])
    evac = Evac(p)
    for ex in range(EPC):
        w1, w3, w2, xe = W1.next(), W3.next(), W2.next(), Xe.next()
        p.dma("sp", xe[:], xe_d[ex].rearrange("(c p) s -> p c s", p=128), writes=[xe])
        p.dma("pool", w1[:], w1_d[ex].rearrange("(c p) f -> p c f", p=128), writes=[w1])
        p.dma("pool", w3[:], w3_d[ex].rearrange("(c p) f -> p c f", p=128), writes=[w3])
        p.dma("pool", w2[:], w2_d[ex].rearrange("(m p) d -> p m d", p=128), writes=[w2])
        for half in range(ESL // 512):
            hs = slice(half * 512, (half + 1) * 512)
            hg = Hg.next()
            for m in range(4):
                p1, p3 = ps_rot.next(), ps_rot.next()
                for c in range(KC):
                    p.op("pe", lambda e: e.matmul(p1[:, 0:512], lhsT=w1[:, c, m * 128:(m + 1) * 128], rhs=xe[:, c, hs], start=(c == 0), stop=(c == KC - 1)),
                         reads=[w1, xe], writes=[p1], accum=(c > 0))
                for c in range(KC):
                    p.op("pe", lambda e: e.matmul(p3[:, 0:512], lhsT=w3[:, c, m * 128:(m + 1) * 128], rhs=xe[:, c, hs], start=(c == 0), stop=(c == KC - 1)),
                         reads=[w3, xe], writes=[p3], accum=(c > 0))
                h = h1.next()
                p.op("act", lambda e: e.activation(out=h[:], in_=p1[:, 0:512], func=AF.Silu), reads=[p1], writes=[h])
                p.op("dve", lambda e: e.tensor_tensor(out=hg[:, m, :], in0=h[:], in1=p3[:, 0:512], op=ALU.mult), reads=[h, p3], writes=[hg], accum=(m > 0))
            for st in range(4):
                y = yo.next()
                for n in range(4):
                    ps = ps_rot.next()
                    for m in range(4):
                        p.op("pe", lambda e: e.matmul(ps[:, 0:512], lhsT=hg[:, m, st * 128:(st + 1) * 128], rhs=w2[:, m, n * 512:(n + 1) * 512],
                                                      start=(m == 0), stop=(m == 3)), reads=[hg, w2], writes=[ps], accum=(m > 0))
                    evac(y[:, n * 512:(n + 1) * 512], ps[:, 0:512], [ps], [y], first=(n == 0))
                r0 = half * 512 + st * 128
                p.dma("sp", ye_d[ex, r0:r0 + 128, :], y[:], reads=[y])
    p.finish(yo.tiles)
    return nc


def build_C2c(last):
    nc = bass.Bass("TRN2", target_bir_lowering=False)
    p = P(nc)
    zT = nc.dram_tensor("zT", [D, NT], F32, kind="ExternalInput").ap()
    slotT_d = nc.dram_tensor("slotT", [NE, NTP], F32, kind="ExternalInput").ap()
    gtT_d = nc.dram_tensor("gtT", [NE, NTP], F32, kind="ExternalInput").ap()
    ye_d = nc.dram_tensor("ye", [NE, NSL, D], BF16, kind="ExternalInput").ap()
    fw_d = nc.dram_tensor("fnw", [128, KC], F32, kind="ExternalInput").ap()
    outT = nc.dram_tensor("outT", [D, NT], F32, kind="ExternalOutput").ap()
    if not last:
        lnw_d = nc.dram_tensor("lnw", [128, KC], F32, kind="ExternalInput").ap()
        w_in = nc.dram_tensor("w_in", [D, INW], F32, kind="ExternalInput").ap()
        projT = nc.dram_tensor("projT", [INW, NT], F32, kind="ExternalOutput").ap()
    z = p.sb([128, KC, NT], F32, "z")
    slotT = p.sb([NE, NTP], BF16, "slotT_s")
    gtT = p.sb([NE, NTP], BF16, "gtT_s")
    fnw = p.sb([128, KC], F32, "fnw_s")
    identb = p.sb([128, 128], BF16, "identb")
    identf = p.sb([128, 128], F32, "identf")
    iota_p = p.sb([128, 1], F32, "iota_p")
    zv = zT.rearrange("(c p) t -> p c t", p=128)
    for q in range(4):
        p.dma("sp", z[:, 4 * q:4 * q + 4, :], zv[:, 4 * q:4 * q + 4, :], writes=[z], concurrent=True)
    p.dma("sp", fnw[:], fw_d[:, :], writes=[fnw])
    p.dma("pool", slotT[:], slotT_d[:, :], writes=[slotT])
    p.dma("pool", gtT[:], gtT_d[:, :], writes=[gtT])
    p.op("pool", lambda e: e.memset(identf[:], 0.0), writes=[identf])
    p.op("pool", lambda e: e.affine_select(out=identf[:], in_=identf[:], pattern=[[-1, 128]], compare_op=ALU.not_equal,
                                           fill=1.0, base=0, channel_multiplier=1), reads=[identf], writes=[identf])
    p.op("pool", lambda e: e.tensor_copy(out=identb[:], in_=identf[:]), reads=[identf], writes=[identb])
    p.op("pool", lambda e: e.iota(iota_p[:], pattern=[[0, 1]], base=0, channel_multiplier=1, allow_small_or_imprecise_dtypes=True), writes=[iota_p])
    ps_rot = Rot([p.ps([128, 512], F32, f"ps{i}") for i in range(8)])
    mk = p.mark()
    NG = 4
    Ye_rot = Rot([p.sb([128, NG, D], BF16, f"Ye{i}") for i in range(2)])
    SgT_rot = Rot([p.sb([128, NG, NT], BF16, f"SgT{i}") for i in range(2)])
    sel_rot = Rot([p.sb([NE, 128], BF16, f"sel{i}") for i in range(2)])
    gB_rot = Rot([p.sb([128, SC], F32, f"gB{i}") for i in range(2)])
    for qd in range(NE // NG):
        e0 = NG * qd
        Ye, SgT = Ye_rot.next(), SgT_rot.next()
        p.dma("sp", Ye[:], ye_d[e0:e0 + NG].rearrange("e s d -> s e d"), writes=[Ye])
        for ee in range(NG):
            ex = e0 + ee
            sel = sel_rot.next()
            p.op("dve", lambda e: e.tensor_copy(out=sel[:], in_=identb[0:NE, ex:ex + 1].to_broadcast([NE, 128])), reads=[identb], writes=[sel])
            for n in range(3):
                sl = slice(n * SC, (n + 1) * SC)
                pa, pg = ps_rot.next(), ps_rot.next()
                p.op("pe", lambda e: e.matmul(pa[:, 0:SC], lhsT=sel[:], rhs=slotT[:, sl], start=True, stop=True), reads=[sel, slotT], writes=[pa])
                p.op("pe", lambda e: e.matmul(pg[:, 0:SC], lhsT=sel[:], rhs=gtT[:, sl], start=True, stop=True), reads=[sel, gtT], writes=[pg])
                gB = gB_rot.next()
                p.op("act", lambda e: e.activation(out=gB[:], in_=pg[:, 0:SC], func=AF.Copy), reads=[pg], writes=[gB])
                p.op("dve", lambda e: e.scalar_tensor_tensor(out=SgT[:, ee, sl], in0=pa[:, 0:SC], scalar=iota_p[:, 0:1], in1=gB[:],
                                                             op0=ALU.is_equal, op1=ALU.mult), reads=[pa, iota_p, gB], writes=[SgT], accum=(ee + n > 0))
        for c in range(KC):
            for n in range(3):
                sl = slice(n * SC, (n + 1) * SC)
                ps = ps_rot.next()
                for ee in range(NG):
                    p.op("pe", lambda e: e.matmul(ps[:, 0:SC], lhsT=Ye[:, ee, c * 128:(c + 1) * 128], rhs=SgT[:, ee, sl], start=(ee == 0), stop=(ee == NG - 1)),
                         reads=[Ye, SgT], writes=[ps], accum=(ee > 0))
                p.op("dve", lambda e: e.tensor_tensor(out=z[:, c, sl], in0=z[:, c, sl], in1=ps[:, 0:SC], op=ALU.add), reads=[z, ps], writes=[z])
    ov = outT.rearrange("(c p) t -> p c t", p=128)
    p.release(mk)
    rstd = p.sb([128, NT], F32, "rstd")
    onesD = p.sb([128, 128], BF16, "onesD")
    epsc = p.sb([128, 1], F32, "epsc")
    p.op("pool", lambda e: e.memset(onesD[:], 1.0 / D), writes=[onesD])
    p.op("pool", lambda e: e.memset(epsc[:], EPS), writes=[epsc])
    sq_rot = Rot([p.sb([128, NT], BF16, f"sq{i}") for i in range(2)])
    if last:
        emit_rmsnorm(p, z, fnw, None, onesD, epsc, sq_rot, ps_rot, rstd, hn32=z)
        for q in range(4):
            p.dma("sp", ov[:, 4 * q:4 * q + 4, :], z[:, 4 * q:4 * q + 4, :], reads=[z])
        p.finish([z])
        return nc
    for q in range(4):
        p.dma("sp", ov[:, 4 * q:4 * q + 4, :], z[:, 4 * q:4 * q + 4, :], reads=[z])
    lnw = p.sb([128, KC], F32, "lnw_s")
    p.dma("sp", lnw[:], lnw_d[:, :], writes=[lnw])
    hn = p.sb([128, KC, NT], BF16, "hn")
    wb_rot = Rot([p.sb([128, KC, 512], BF16, f"wb{i}") for i in range(2)])
    st_rot = Rot([p.sb([128, NT], F32, f"st{i}") for i in range(3)])
    emit_rmsnorm(p, z, lnw, hn, onesD, epsc, sq_rot, ps_rot, rstd)
    emit_inproj(p, hn, w_in, projT, wb_rot, ps_rot, st_rot, Evac(p))
    p.finish([z] + st_rot.tiles)
    return nc


_PROGS = {}


def _prog(name, fn, *args):
    key = (name,) + args
    if key not in _PROGS:
        _PROGS[key] = fn(*args)
    return _PROGS[key]


def _run(nc, in_maps):
    res = run_bass_kernel_spmd(nc, in_maps, core_ids=list(range(8)))
    return res.results


def _colT(v, n):
    return np.ascontiguousarray(np.asarray(v, np.float32).reshape(n, 128).T)


def _rep(v, n=128):
    v = np.asarray(v, np.float32)
    return np.ascontiguousarray(np.broadcast_to(v[None], (n,) + v.shape))


def _pairs(a):
    a = np.asarray(a, np.float32)
    sh = a.shape[2:]
    nd = len(sh)
    return np.ascontiguousarray(a.reshape(8, 2, 64, *sh).transpose(1, 2, 0, *range(3, 3 + nd)).reshape(128, 8, *sh))


def _tok_tiles(a):
    o = np.zeros((NQT, 128, a.shape[-1]), np.float32)
    o[0, :NMETA] = a[:NMETA]
    o[1:] = a[NMETA:].reshape(16, 128, -1)
    return o.transpose(1, 0, 2)


def _untok_tiles(o):
    o = o.transpose(1, 0, 2)
    return np.concatenate([o[0, :NMETA], o[1:].reshape(SEQ, -1)], axis=0)


def kernel(x, meta_tokens, ln1_w, w_in, hgrn_lower_bounds, hgrn_norm_w, s5_a_re, s5_a_im,
           s5_b_re, s5_b_im, s5_c_re, s5_c_im, s5_d, s5_log_dt, s5_w_glu, s5_b_glu,
           diff_lambda_q1, diff_lambda_k1, diff_lambda_q2, diff_lambda_k2, diff_subln_w,
           w_out, ln2_w, router_group_w, router_group_b, router_expert_w, router_expert_b,
           expert_w1, expert_w3, expert_w2, final_norm_w):
    import math
    f32 = lambda a: np.asarray(a, np.float32)
    x, meta_tokens = f32(x), f32(meta_tokens)
    z0 = np.concatenate([np.broadcast_to(meta_tokens[None], (NB, NMETA, D)), x], axis=1)
    zTs = [np.ascontiguousarray(z0[c // 2, (c % 2) * NT:(c % 2 + 1) * NT].T) for c in range(8)]
    depth = f32(ln1_w).shape[0]
    hlb = f32(hgrn_lower_bounds)
    fnw = _colT(final_norm_w, KC)
    for layer in range(depth):
        if layer == 0:
            r = _run(_prog("A", build_A), [{"zT": zTs[c], "lnw": _colT(f32(ln1_w)[0], KC), "w_in": f32(w_in)[0]} for c in range(8)])
            projTs = [r[c]["projT"] for c in range(8)]
            del r
        proj = np.stack([np.concatenate([projTs[2 * b].T, projTs[2 * b + 1].T], axis=0) for b in range(NB)])
        del projTs
        lsel = np.full((128, 1), 1.0 if layer > 0 else 0.0, np.float32)
        nw = _rep(f32(hgrn_norm_w)[layer])
        li = 0.8 - 0.6 * math.exp(-0.3 * layer)
        linit = _rep(np.array([li, 1.0 - li], np.float32))
        lamv = _rep(np.stack([f32(diff_lambda_q1)[layer], f32(diff_lambda_k1)[layer], f32(diff_lambda_q2)[layer], f32(diff_lambda_k2)[layer]]))
        subw = _rep(f32(diff_subln_w)[layer])
        ins = []
        for c in range(8):
            b, hh = c // 2, c % 2
            hs = [2 * hh, 2 * hh + 1]

            def padT(col0):
                o = np.zeros((2, 128, LP), np.float32)
                for i, h in enumerate(hs):
                    o[i, :, :L] = proj[b, :, col0 + h * 128:col0 + (h + 1) * 128].T
                return o

            def padded(col0):
                o = np.zeros((2, LP, 128), np.float32)
                for i, h in enumerate(hs):
                    o[i, :L] = proj[b, :, col0 + h * 128:col0 + (h + 1) * 128]
                return o

            hv = np.ascontiguousarray(padded(1024).reshape(2, NCK, HC, 128).transpose(0, 2, 1, 3))
            hg = np.ascontiguousarray(padded(1536).reshape(2, LP // 128, 128, 128).transpose(0, 2, 1, 3))
            lbraw = np.ascontiguousarray(np.stack([hlb[0:2, h * 128:(h + 1) * 128].T for h in hs], axis=1))
            d_ = {"hqT": padT(0), "hfT": padT(512), "hv": hv, "hg": hg, "lbraw": lbraw, "lsel": lsel, "nw": nw}
            gs = slice(16 * hh, 16 * hh + 16)
            u = proj[b, :, 2048 + 256 * hh:2048 + 256 * (hh + 1)]
            d_.update({"uT": np.ascontiguousarray(u.T.reshape(2, 128, L)),
                       "a_re": _pairs(f32(s5_a_re)[layer, gs]), "a_im": _pairs(f32(s5_a_im)[layer, gs]),
                       "ldt": _pairs(np.broadcast_to(f32(s5_log_dt)[layer, gs][:, None], (16, 64))),
                       "b_re": _pairs(f32(s5_b_re)[layer, gs]), "b_im": _pairs(f32(s5_b_im)[layer, gs]),
                       "cT_re": _pairs(f32(s5_c_re)[layer, gs].transpose(0, 2, 1)), "cT_im": _pairs(f32(s5_c_im)[layer, gs].transpose(0, 2, 1)),
                       "dsk": np.ascontiguousarray(f32(s5_d)[layer, 256 * hh:256 * (hh + 1)].reshape(2, 128).T)})
            dq = proj[b, :, 2560:3584].reshape(L, 4, 2, 128)
            dk = proj[b, :, 3584:4608].reshape(L, 4, 2, 128)
            dv = proj[b, :, 4608:5632].reshape(L, 4, 256)
            d_.update({"qT": np.ascontiguousarray(dq[:, 2 * hh:2 * hh + 2].reshape(L, 4, 128).transpose(1, 2, 0)),
                       "kT": np.ascontiguousarray(dk[:, 2 * hh:2 * hh + 2].reshape(L, 4, 128).transpose(1, 2, 0)),
                       "v": np.ascontiguousarray(np.stack([_tok_tiles(dv[:, 2 * hh + i]) for i in range(2)], axis=1)),
                       "lamv": lamv, "subw": subw, "linit": linit})
            ins.append(d_)
        r = _run(_prog("B", build_B), ins)
        o_a = np.zeros((NB, L, 512), np.float32)
        ys = np.zeros((NB, L, 512), np.float32)
        o_c = np.zeros((NB, L, 1024), np.float32)
        for c in range(8):
            b, hh = c // 2, c % 2
            for i in range(2):
                h = 2 * hh + i
                o_a[b, :, h * 128:(h + 1) * 128] = r[c]["oa"][i].transpose(1, 0, 2).reshape(LP, 128)[:L]
                o_c[b, :, h * 256:(h + 1) * 256] = _untok_tiles(r[c]["oc"][:, i])
            ys[b, :, 256 * hh:256 * (hh + 1)] = r[c]["yT"].reshape(256, L).T
        del r
        del proj
        w_r = np.ascontiguousarray(np.concatenate([f32(router_group_w)[layer], f32(router_expert_w)[layer]], axis=1))
        b_r = _rep(np.concatenate([f32(router_group_b)[layer], f32(router_expert_b)[layer]]))
        ins = []
        for c in range(8):
            b, tk = c // 2, slice((c % 2) * NT, (c % 2 + 1) * NT)
            ins.append({"zT": zTs[c], "oaT": np.ascontiguousarray(o_a[b, tk].T), "ysT": np.ascontiguousarray(ys[b, tk].T),
                        "ocT": np.ascontiguousarray(o_c[b, tk].T), "w_glu": f32(s5_w_glu)[layer], "b_glu": _colT(f32(s5_b_glu)[layer], 8),
                        "w_out": f32(w_out)[layer], "ln2": _colT(f32(ln2_w)[layer], KC), "w_r": w_r, "b_r": b_r})
        r2 = r1 = _run(_prog("C1", build_C1), ins)
        ins = []
        for g in range(8):
            es = slice(EPC * g, EPC * (g + 1))
            xe = np.ascontiguousarray(np.concatenate([r2[c]["xeT"][es] for c in range(8)], axis=2))
            ins.append({"xe": xe, "w1": f32(expert_w1)[layer, es], "w3": f32(expert_w3)[layer, es], "w2": f32(expert_w2)[layer, es]})
        del r2
        r3 = _run(_prog("C2b", build_C2b), ins)
        last = layer == depth - 1
        ins = []
        for c in range(8):
            ye = np.ascontiguousarray(np.concatenate([r3[g]["ye"][:, c * NSL:(c + 1) * NSL] for g in range(8)], axis=0))
            d_ = {"zT": r1[c]["z1T"], "slotT": r1[c]["slotT"], "gtT": r1[c]["gtT"], "ye": ye, "fnw": fnw}
            if not last:
                d_.update({"lnw": _colT(f32(ln1_w)[layer + 1], KC), "w_in": f32(w_in)[layer + 1]})
            ins.append(d_)
        del r3
        r4 = _run(_prog("C2c", build_C2c, last), ins)
        zTs = [r4[c]["outT"] for c in range(8)]
        if not last:
            projTs = [r4[c]["projT"] for c in range(8)]
        del r4
    out = np.stack([np.concatenate([zTs[2 * b].T, zTs[2 * b + 1].T], axis=0) for b in range(NB)])
    return np.ascontiguousarray(out[:, NMETA:]).astype(np.float32)
```
